# Optimizing a Trainium2 kernel written in Bass

```python
import jax, jax.numpy as jnp
from jax import lax
import numpy as np

D_MODEL = 1024
BATCH = 4
SEQ = 8192
DEPTH = 2

CHUNK = 64
HEAD_DIM = 64
ROT_DIM = HEAD_DIM // 4
ROPE_THETA = 500000.0
N_HEADS_A = 6
N_HEADS_B = 5
N_HEADS_C = 5
WIDTH_A = N_HEADS_A * HEAD_DIM
WIDTH_B = N_HEADS_B * HEAD_DIM
WIDTH_C = N_HEADS_C * HEAD_DIM
MIX_WIDTH = WIDTH_A + WIDTH_B + WIDTH_C
IDX_HEADS = 8
IDX_DIM = 64
TOPK_MAX = 256
PREV_CHUNKS = 8
BAND = (PREV_CHUNKS + 1) * CHUNK
REL_CLIP = 128
Q_BLOCK = 128
N_BRANCH = 3
FFN_HIDDEN = -(-8 * D_MODEL // (3 * 256)) * 256
RMS_EPS = 1e-6
FORGET_BIAS_LO = 2.0
FORGET_BIAS_HI = 6.0
SPLIT_SIZES = (WIDTH_A, WIDTH_A, WIDTH_A, IDX_HEADS * IDX_DIM, IDX_DIM, IDX_HEADS,
               3 * WIDTH_B, 3 * WIDTH_C, N_HEADS_C, N_BRANCH * D_MODEL)
IN_WIDTH = sum(SPLIT_SIZES)

kernel_name = 'hybrid_gated_streaming_encoder'


def rmsnorm(x, g):
    xf = x.astype(jnp.float32)
    y = xf * lax.rsqrt(jnp.mean(xf * xf, axis=-1, keepdims=True) + RMS_EPS)
    return (y * g.astype(jnp.float32)).astype(x.dtype)


def rope_tables(positions):
    inv = ROPE_THETA ** (-jnp.arange(0, ROT_DIM, 2, dtype=jnp.float32) / ROT_DIM)
    ang = positions.astype(jnp.float32)[..., None] * inv
    return jnp.cos(ang), jnp.sin(ang)


def partial_rope(t, cos, sin):
    half = ROT_DIM // 2
    cs = cos[:, :, None, :].astype(t.dtype)
    sn = sin[:, :, None, :].astype(t.dtype)
    x1 = t[..., :half]
    x2 = t[..., half:ROT_DIM]
    return jnp.concatenate([x1 * cs - x2 * sn, x2 * cs + x1 * sn, t[..., ROT_DIM:]], axis=-1)


def dsa_attention(q, k, v, q_idx, k_idx, w_idx):
    B, S = q.shape[0], q.shape[1]
    topk = min(TOPK_MAX, S // 4)
    n_blk = S // Q_BLOCK
    key_chunk = jnp.arange(S) // CHUNK
    scale = HEAD_DIM ** -0.5
    kf = k_idx.astype(jnp.float32)
    gather = jax.vmap(lambda arr, ix: arr[ix])

    def block(i):
        start = i * Q_BLOCK
        qs = lax.dynamic_slice_in_dim(q, start, Q_BLOCK, axis=1)
        qis = lax.dynamic_slice_in_dim(q_idx, start, Q_BLOCK, axis=1).astype(jnp.float32)
        ws = lax.dynamic_slice_in_dim(w_idx, start, Q_BLOCK, axis=1).astype(jnp.float32)
        q_chunk = (start + jnp.arange(Q_BLOCK)) // CHUNK
        idx_scores = jnp.einsum('bqhs,bqh->bqs',
                                jax.nn.relu(jnp.einsum('bqhd,bsd->bqhs', qis, kf)), ws)
        adm = key_chunk[None, :] <= q_chunk[:, None]
        idx_scores = jnp.where(adm[None], idx_scores, -jnp.inf)
        _, sel = lax.top_k(idx_scores, topk)
        valid = (sel // CHUNK) <= q_chunk[None, :, None]
        k_sel = gather(k, sel)
        v_sel = gather(v, sel)
        logits = jnp.einsum('bqhd,bqkhd->bhqk', qs, k_sel).astype(jnp.float32) * scale
        logits = jnp.where(valid[:, None], logits, -jnp.inf)
        p = jax.nn.softmax(logits, axis=-1).astype(v.dtype)
        return jnp.einsum('bhqk,bqkhd->bqhd', p, v_sel)

    out = lax.map(block, jnp.arange(n_blk))
    return jnp.moveaxis(out, 0, 1).reshape(B, S, -1)


def chunk_band_attention(q, k, v, rel_bias):
    B, S, H, Dh = q.shape
    n_c = S // CHUNK
    pad = PREV_CHUNKS * CHUNK
    qc = q.reshape(B, n_c, CHUNK, H, Dh)
    kp = jnp.pad(k, ((0, 0), (pad, 0), (0, 0), (0, 0))).reshape(B, n_c + PREV_CHUNKS, CHUNK, H, Dh)
    vp = jnp.pad(v, ((0, 0), (pad, 0), (0, 0), (0, 0))).reshape(B, n_c + PREV_CHUNKS, CHUNK, H, Dh)
    kband = jnp.concatenate([kp[:, j:j + n_c] for j in range(PREV_CHUNKS + 1)], axis=2)
    vband = jnp.concatenate([vp[:, j:j + n_c] for j in range(PREV_CHUNKS + 1)], axis=2)
    logits = jnp.einsum('bcqhd,bckhd->bchqk', qc, kband).astype(jnp.float32) * (Dh ** -0.5)
    qi = jnp.arange(CHUNK)
    kj = jnp.arange(BAND)
    dist = jnp.clip(qi[:, None] + pad - kj[None, :], -REL_CLIP, REL_CLIP) + REL_CLIP
    bias = rel_bias[:, dist].astype(jnp.float32)
    band_chunk = jnp.arange(n_c)[:, None] - PREV_CHUNKS + kj[None, :] // CHUNK
    valid = band_chunk >= 0
    logits = jnp.where(valid[None, :, None, None, :], logits + bias[None, None], -jnp.inf)
    p = jax.nn.softmax(logits, axis=-1).astype(v.dtype)
    out = jnp.einsum('bchqk,bckhd->bcqhd', p, vband)
    return out.reshape(B, S, H * Dh)


def forgetting_attention(q, k, v, f_logit):
    B, S, H, Dh = q.shape
    n_blk = S // Q_BLOCK
    log_f = jax.nn.log_sigmoid(f_logit.astype(jnp.float32))
    F = jnp.moveaxis(jnp.cumsum(log_f, axis=1), 1, 2)
    key_pos = jnp.arange(S)
    scale = Dh ** -0.5

    def block(i):
        start = i * Q_BLOCK
        qs = lax.dynamic_slice_in_dim(q, start, Q_BLOCK, axis=1)
        Fq = lax.dynamic_slice_in_dim(F, start, Q_BLOCK, axis=2)
        logits = (jnp.einsum('bqhd,bshd->bhqs', qs, k).astype(jnp.float32) * scale
                  + Fq[..., :, None] - F[:, :, None, :])
        qpos = start + jnp.arange(Q_BLOCK)
        logits = jnp.where((key_pos[None, :] <= qpos[:, None])[None, None], logits, -jnp.inf)
        p = jax.nn.softmax(logits, axis=-1).astype(v.dtype)
        return jnp.einsum('bhqs,bshd->bqhd', p, v)

    out = lax.map(block, jnp.arange(n_blk))
    return jnp.moveaxis(out, 0, 1).reshape(B, S, H * Dh)


def hybrid_mixer(h, cos, sin, w_in, b_in, rel_bias, w_branch, w_out):
    B, S, D = h.shape
    proj = h @ w_in + b_in
    parts = []
    off = 0
    for size in SPLIT_SIZES:
        parts.append(proj[..., off:off + size])
        off += size
    qa, ka, va, qi, ki, wi, qkv_b, qkv_c, f_c, gate_logits = parts
    qa = partial_rope(qa.reshape(B, S, N_HEADS_A, HEAD_DIM), cos, sin)
    ka = partial_rope(ka.reshape(B, S, N_HEADS_A, HEAD_DIM), cos, sin)
    va = va.reshape(B, S, N_HEADS_A, HEAD_DIM)
    qi = partial_rope(qi.reshape(B, S, IDX_HEADS, IDX_DIM), cos, sin)
    ki = partial_rope(ki[:, :, None, :], cos, sin)[:, :, 0, :]
    wi = wi * (IDX_HEADS ** -0.5 * IDX_DIM ** -0.5)
    o_a = dsa_attention(qa, ka, va, qi, ki, wi)
    qkv_b = qkv_b.reshape(B, S, 3, N_HEADS_B, HEAD_DIM)
    o_b = chunk_band_attention(qkv_b[:, :, 0], qkv_b[:, :, 1], qkv_b[:, :, 2], rel_bias)
    qkv_c = qkv_c.reshape(B, S, 3, N_HEADS_C, HEAD_DIM)
    o_c = forgetting_attention(qkv_c[:, :, 0], qkv_c[:, :, 1], qkv_c[:, :, 2], f_c)
    gates = jax.nn.sigmoid(gate_logits.reshape(B, S, N_BRANCH, D))
    y = (gates[:, :, 0] * (o_a @ w_branch[:WIDTH_A])
         + gates[:, :, 1] * (o_b @ w_branch[WIDTH_A:WIDTH_A + WIDTH_B])
         + gates[:, :, 2] * (o_c @ w_branch[WIDTH_A + WIDTH_B:]))
    return y @ w_out


def swiglu(h, w_ffn_in, w_ffn_out):
    gu = h @ w_ffn_in
    g, u = gu[..., :FFN_HIDDEN], gu[..., FFN_HIDDEN:]
    return (jax.nn.silu(g) * u) @ w_ffn_out


def setup_inputs(seed: int = 0) -> dict:
    key = jax.random.key(seed)
    ks = jax.random.split(key, 16)
    f32 = jnp.float32
    D = D_MODEL
    nrm = jax.random.normal
    x = nrm(ks[0], (BATCH, SEQ, D), f32)
    c = nrm(ks[1], (BATCH, D), f32)
    offset = jax.random.randint(ks[2], (BATCH, 1), 0, 4096, dtype=jnp.int32)
    positions = offset + jnp.arange(SEQ, dtype=jnp.int32)[None, :]
    ada_w = 0.5 * D ** -0.5 * nrm(ks[3], (DEPTH, D, 6 * D), f32)
    ada_b = 0.01 * nrm(ks[4], (DEPTH, 6 * D), f32)
    norm1_g = 1.0 + 0.05 * nrm(ks[5], (DEPTH, D), f32)
    w_in = D ** -0.5 * nrm(ks[6], (DEPTH, D, IN_WIDTH), f32)
    f_off = sum(SPLIT_SIZES[:8])
    forget_b = jax.random.uniform(ks[7], (DEPTH, N_HEADS_C), f32, FORGET_BIAS_LO, FORGET_BIAS_HI)
    b_in = (0.01 * nrm(ks[8], (DEPTH, IN_WIDTH), f32)).at[:, f_off:f_off + N_HEADS_C].add(forget_b)
    rel_bias = 0.5 * nrm(ks[9], (DEPTH, N_HEADS_B, 2 * REL_CLIP + 1), f32)
    w_branch = WIDTH_B ** -0.5 * nrm(ks[10], (DEPTH, MIX_WIDTH, D), f32)
    w_out = D ** -0.5 * nrm(ks[11], (DEPTH, D, D), f32)
    norm2_g = 1.0 + 0.05 * nrm(ks[12], (DEPTH, D), f32)
    w_ffn_in = D ** -0.5 * nrm(ks[13], (DEPTH, D, 2 * FFN_HIDDEN), f32)
    w_ffn_out = FFN_HIDDEN ** -0.5 * nrm(ks[14], (DEPTH, FFN_HIDDEN, D), f32)
    final_g = 1.0 + 0.05 * nrm(ks[15], (D,), f32)
    return {'x': x, 'c': c, 'positions': positions, 'ada_w': ada_w, 'ada_b': ada_b,
            'norm1_g': norm1_g, 'w_in': w_in, 'b_in': b_in, 'rel_bias': rel_bias,
            'w_branch': w_branch, 'w_out': w_out, 'norm2_g': norm2_g,
            'w_ffn_in': w_ffn_in, 'w_ffn_out': w_ffn_out, 'final_g': final_g}


def reference(x, c, positions, ada_w, ada_b, norm1_g, w_in, b_in, rel_bias,
              w_branch, w_out, norm2_g, w_ffn_in, w_ffn_out, final_g):
    cos, sin = rope_tables(positions)
    cond = jax.nn.silu(c)
    for l in range(DEPTH):
        mod = (cond @ ada_w[l] + ada_b[l])[:, None, :]
        sh1, sc1, g1, sh2, sc2, g2 = jnp.split(mod, 6, axis=-1)
        h = rmsnorm(x, norm1_g[l]) * (1 + sc1) + sh1
        x = x + g1 * hybrid_mixer(h, cos, sin, w_in[l], b_in[l], rel_bias[l], w_branch[l], w_out[l])
        h = rmsnorm(x, norm2_g[l]) * (1 + sc2) + sh2
        x = x + g2 * swiglu(h, w_ffn_in[l], w_ffn_out[l])
    return rmsnorm(x, final_g)
```

```python
import contextlib
import numpy as np
import concourse.bass as bass
import concourse.mybir as mybir
from concourse.bass_utils import run_bass_kernel_spmd

F32 = mybir.dt.float32
BF16 = mybir.dt.bfloat16
I32 = mybir.dt.int32
AF = mybir.ActivationFunctionType
ALU = mybir.AluOpType
AX = mybir.AxisListType

ENGS = ("pe", "act", "dve", "pool", "sp")
DMA_K = 24
EPOCH = 12000

D = 1024
DEPTH = 2
HD = 64
NHA, NHB, NHC = 6, 5, 5
WA, WB_, WC = NHA * HD, NHB * HD, NHC * HD
IDXH = 8
TOPK = 256
FFN = 2816
EPS = 1e-6
SPLIT = (WA, WA, WA, IDXH * HD, HD, IDXH, 3 * WB_, 3 * WC, NHC, 3 * D)
INW = sum(SPLIT)
OFF = np.cumsum((0,) + SPLIT)
O_QA, O_KA, O_VA, O_QI, O_KI, O_WI, O_QKVB, O_QKVC, O_FC, O_GATE = [int(v) for v in OFF[:10]]
BIG = 30000.0
NEG = -1.0e30
NITER = 13
TWO_PI = 2.0 * np.pi
CW1 = 6.28125
CW2 = float(TWO_PI - 6.28125)
MAGIC = 12582912.0

R_QA, R_KA, R_QI, R_KI = 0, 384, 768, 1280
R_QB, R_KB, R_QC, R_KC = 1344, 1664, 1984, 2304
R_G = 2624
FM_ROWS = R_G + 3 * D


class Buf:
    def __init__(self, name, t):
        self.name = name
        self.t = t
        self.last_writer = None
        self.readers = []

    def __getitem__(self, idx):
        return self.t[idx]


class Rec:
    __slots__ = ("eng", "fn", "deps", "marked", "count", "epoch", "is_dma", "dsem", "dval",
                 "prewait")

    def __init__(self, eng, fn, is_dma=False):
        self.eng = eng
        self.fn = fn
        self.deps = []
        self.marked = False
        self.count = None
        self.epoch = None
        self.is_dma = is_dma
        self.dsem = None
        self.dval = None
        self.prewait = None


class Ctx:
    def __init__(self, nc):
        self.nc = nc
        self.stack = contextlib.ExitStack()
        self.recs = []
        self.last = {e: None for e in ENGS}
        self.dma_recs = {e: [] for e in ENGS}
        self.nbuf = 0

    def sbuf(self, name, shape, dtype, stack=None):
        st = stack or self.stack
        self.nbuf += 1
        t = st.enter_context(self.nc.sbuf_tensor(f"{name}_{self.nbuf}", list(shape), dtype))
        return Buf(name, t)

    def psum(self, name, shape, dtype, stack=None):
        st = stack or self.stack
        self.nbuf += 1
        t = st.enter_context(self.nc.psum_tensor(f"{name}_{self.nbuf}", list(shape), dtype))
        return Buf(name, t)

    def dram(self, name, shape, dtype, kind="Internal"):
        t = self.nc.dram_tensor(name, list(shape), dtype, kind=kind)
        return Buf(name, t.ap())

    def _track(self, rec, reads, writes):
        for b in reads:
            w = b.last_writer
            if w is not None and w is not rec:
                if not (w.eng == rec.eng and rec.eng == "pe" and not w.is_dma and not rec.is_dma):
                    rec.deps.append(w)
        for b in writes:
            w = b.last_writer
            if w is not None and w is not rec:
                if w.is_dma or rec.is_dma or w.eng != rec.eng or rec.eng != "pe":
                    rec.deps.append(w)
            for r in b.readers:
                if r is rec:
                    continue
                if r.is_dma or rec.is_dma or r.eng != rec.eng or rec.eng != "pe":
                    rec.deps.append(r)
        for b in reads:
            b.readers.append(rec)
        for b in writes:
            b.last_writer = rec
            b.readers = []

    def op(self, eng, fn, reads=(), writes=()):
        rec = Rec(eng, fn)
        self._track(rec, reads, writes)
        self.recs.append(rec)
        self.last[eng] = rec
        return rec

    def dma(self, eng, fn, reads=(), writes=()):
        rec = Rec(eng, fn, is_dma=True)
        self._track(rec, reads, writes)
        self.recs.append(rec)
        self.dma_recs[eng].append(rec)
        return rec

    def barrier(self):
        pend = [self.last[e] for e in ENGS if self.last[e] is not None]
        dmas = []
        for e in ENGS:
            dmas += self.dma_recs[e][-DMA_K:]
        for e in ENGS:
            rec = Rec(e, None)
            rec.deps = [p for p in pend if p.eng != e] + dmas
            self.recs.append(rec)

    def finalize(self):
        nc = self.nc
        for r in self.recs:
            for d in r.deps:
                if not d.is_dma:
                    d.marked = True
        cnt = {e: 0 for e in ENGS}
        for r in self.recs:
            if r.is_dma or r.fn is None:
                continue
            if r.marked:
                cnt[r.eng] += 1
                r.epoch = (cnt[r.eng] - 1) // EPOCH
                r.count = (cnt[r.eng] - 1) % EPOCH + 1
        self.esem = {}
        for e in ENGS:
            for ep in range((cnt[e] + EPOCH - 1) // EPOCH):
                self.esem[(e, ep)] = self.stack.enter_context(nc.semaphore(f"s_{e}_{ep}"))
        self.dsem = {}
        for e in ENGS:
            if self.dma_recs[e]:
                for k in range(DMA_K):
                    self.dsem[(e, k)] = self.stack.enter_context(nc.semaphore(f"d_{e}_{k}"))
            for j, r in enumerate(self.dma_recs[e]):
                r.dsem = self.dsem[(e, j % DMA_K)]
                r.dval = 16 * (j // DMA_K + 1)
                if j >= DMA_K:
                    r.prewait = (r.dsem, 16 * (j // DMA_K))

    def replay(self):
        nc = self.nc
        by_eng = {e: [] for e in ENGS}
        for r in self.recs:
            by_eng[r.eng].append(r)
        self.nwaits = 0
        self.ninstr = {e: len(by_eng[e]) for e in ENGS}
        with nc.Block() as block:
            deco = {"pe": block.tensor, "act": block.scalar, "dve": block.vector,
                    "pool": block.gpsimd, "sp": block.sync}

            def make(e):
                def body(eng):
                    seen = {}
                    for r in by_eng[e]:
                        waits = []
                        for d in r.deps:
                            if d.is_dma:
                                waits.append((d.dsem, d.dval))
                            else:
                                waits.append((self.esem[(d.eng, d.epoch)], d.count))
                        if r.prewait is not None:
                            waits.append(r.prewait)
                        best = {}
                        for s, v in waits:
                            k = id(s)
                            if seen.get(k, 0) >= v:
                                continue
                            if k not in best or best[k][1] < v:
                                best[k] = (s, v)
                        for k, (s, v) in best.items():
                            eng.wait_ge(s, v)
                            seen[k] = v
                            self.nwaits += 1
                        if r.fn is None:
                            continue
                        ins = r.fn(eng)
                        if r.is_dma:
                            ins.then_inc(r.dsem, 16)
                        elif r.marked:
                            ins.then_inc(self.esem[(r.eng, r.epoch)], 1)
                return body

            for e in ENGS:
                if by_eng[e]:
                    deco[e](make(e))


def make_plan():
    roped = []
    for h in range(NHA):
        roped.append(("qa", h, O_QA + h * HD, R_QA + h * HD))
    for h in range(NHA):
        roped.append(("ka", h, O_KA + h * HD, R_KA + h * HD))
    for h in range(IDXH):
        roped.append(("qi", h, O_QI + h * HD, R_QI + h * HD))
    roped.append(("ki", 0, O_KI, R_KI))
    tiles = []

    def newtile(kind):
        t = dict(cols=np.full(128, -1, np.int64), scale=np.ones(128, np.float32), kind=kind, segs=[])
        tiles.append(t)
        return t

    for g0 in range(0, len(roped), 8):
        grp = roped[g0:g0 + 8]
        tR = newtile("ropeR")
        tS = newtile("ropeS")
        for j, (nm, h, cb, rb) in enumerate(grp):
            sc = 0.125 if nm == "qa" else 1.0
            for i in range(16):
                tR["cols"][16 * j + i] = cb + i
                tS["cols"][16 * j + i] = cb + ((i + 8) % 16)
                tR["scale"][16 * j + i] = sc
                tS["scale"][16 * j + i] = sc
            tR["segs"].append((16 * j, 16, rb))
    npass = len(roped) * 48
    ptiles = [newtile("plain") for _ in range((npass + 127) // 128)]
    for hi, (nm, h, cb, rb) in enumerate(roped):
        sc = 0.125 if nm == "qa" else 1.0
        for d in range(48):
            rg = hi * 48 + d
            t = ptiles[rg // 128]
            t["cols"][rg % 128] = cb + 16 + d
            t["scale"][rg % 128] = sc
        r0 = hi * 48
        r1 = r0 + 48
        while r0 < r1:
            ti = r0 // 128
            n = min(r1, (ti + 1) * 128) - r0
            ptiles[ti]["segs"].append((r0 % 128, n, rb + 16 + (r0 - hi * 48)))
            r0 += n
    last = ptiles[-1]
    assert npass % 128 <= 112
    for h in range(NHC):
        last["cols"][112 + h] = O_FC + h
    last["kind"] = "plain_fc"
    plain = []
    for h in range(NHB):
        plain.append((O_QKVB + h * HD, R_QB + h * HD, 0.125))
    for h in range(NHB):
        plain.append((O_QKVB + WB_ + h * HD, R_KB + h * HD, 1.0))
    for h in range(NHC):
        plain.append((O_QKVC + h * HD, R_QC + h * HD, 0.125))
    for h in range(NHC):
        plain.append((O_QKVC + WC + h * HD, R_KC + h * HD, 1.0))
    for i in range(0, len(plain), 2):
        t = newtile("plain")
        for j, (cb, rb, sc) in enumerate(plain[i:i + 2]):
            t["cols"][64 * j:64 * j + 64] = np.arange(cb, cb + 64)
            t["scale"][64 * j:64 * j + 64] = sc
            t["segs"].append((64 * j, 64, rb))
    for j in range(24):
        t = newtile("gate")
        t["cols"][:] = np.arange(O_GATE + j * 128, O_GATE + (j + 1) * 128)
        t["segs"].append((0, 128, R_G + j * 128))
    tm_cols = np.concatenate([
        np.arange(O_VA, O_VA + WA),
        np.arange(O_QKVB + 2 * WB_, O_QKVB + 3 * WB_),
        np.arange(O_QKVC + 2 * WC, O_QKVC + 3 * WC),
        np.arange(O_WI, O_WI + IDXH)])
    return dict(tiles=tiles, tm_cols=tm_cols)


PLAN = make_plan()
NFM = len(PLAN["tiles"])
NTM = len(PLAN["tm_cols"])


def host_layer_inputs(l, w_in, b_in, ada_w, ada_b, norm1_g, norm2_g, rel_bias, w_branch, w_out,
                      w_ffn_in, w_ffn_out):
    out = {}
    W = np.asarray(w_in[l], np.float32)
    B = np.asarray(b_in[l], np.float32)
    wfm = np.zeros((D, NFM * 128), np.float32)
    bfm = np.zeros((128, NFM), np.float32)
    for j, t in enumerate(PLAN["tiles"]):
        m = t["cols"] >= 0
        wfm[:, j * 128:(j + 1) * 128][:, m] = W[:, t["cols"][m]]
        bfm[m, j] = B[t["cols"][m]]
    out["wfm"] = wfm
    out["bfm"] = bfm
    out["wtm"] = np.ascontiguousarray(W[:, PLAN["tm_cols"]])
    out["btm"] = np.ascontiguousarray(np.broadcast_to(B[PLAN["tm_cols"]][None, :], (128, NTM)))
    out["adaw"] = np.asarray(ada_w[l], np.float32)
    ab = np.asarray(ada_b[l], np.float32)
    out["adab_col"] = np.ascontiguousarray(ab.reshape(48, 128).T)
    grow = np.concatenate([ab[2 * D:3 * D], ab[5 * D:6 * D]])
    out["adab_grow"] = np.ascontiguousarray(np.broadcast_to(grow[None, :], (128, 2 * D)))
    out["n1g"] = np.ascontiguousarray(np.asarray(norm1_g[l], np.float32).reshape(8, 128).T)
    out["n2g"] = np.ascontiguousarray(np.asarray(norm2_g[l], np.float32).reshape(8, 128).T)
    rb = np.asarray(rel_bias[l], np.float32)
    q = np.arange(512)[None, :]
    bt = np.zeros((NHB, 8, 128, 512), np.float32)
    for r in range(8):
        k = (-512 + 128 * r + np.arange(128))[:, None]
        dist = np.clip(q - k, -128, 128) + 128
        bt[:, r] = rb[:, dist]
    out["bt"] = bt
    out["wbr"] = np.asarray(w_branch[l], np.float32)
    out["wout"] = np.asarray(w_out[l], np.float32)
    out["wfi"] = np.asarray(w_ffn_in[l], np.float32)
    out["wfo"] = np.asarray(w_ffn_out[l], np.float32)
    return out


def host_consts():
    c = {}
    c["ident"] = np.eye(128, dtype=np.float32)
    c["bigi"] = (BIG * np.eye(128)).astype(np.float32)
    c["negones"] = -np.ones((128, 128), np.float32)
    c["ones"] = np.ones((128, 128), np.float32)
    sc = np.zeros((128, NFM), np.float32)
    for j, t in enumerate(PLAN["tiles"]):
        sc[:, j] = t["scale"]
    c["sctab"] = sc
    i = np.arange(128) % 16
    inv = (500000.0 ** (-(np.arange(0, 16, 2, dtype=np.float32)) / 16.0)).astype(np.float32)
    c["ropec"] = np.stack([inv[i % 8], np.where(i < 8, -1.0, 1.0).astype(np.float32)], 1).astype(np.float32)
    q = np.arange(512)[None, :]
    mb = np.zeros((8, 128, 512), np.float32)
    for r in range(8):
        k = (-512 + 128 * r + np.arange(128))[:, None]
        qc = q // 64
        kc = np.floor_divide(k, 64)
        valid = (kc >= qc - 8) & (kc <= qc)
        mb[r] = np.where(valid, 0.0, -BIG)
    c["maskb"] = mb
    cb = np.zeros((4, 128, 512), np.float32)
    for j in range(4):
        k = (128 * j + np.arange(128))[:, None]
        cb[j] = np.where(k <= q, 0.0, -BIG)
    c["maskc"] = cb
    oh = np.zeros((NHC, NHC, 128), np.float32)
    for h in range(NHC):
        oh[h, h, :] = 1.0
    c["onehot"] = np.ascontiguousarray(oh.transpose(1, 0, 2))
    c["pw"] = np.ascontiguousarray(np.broadcast_to((2.0 ** -np.arange(NITER + 1))[None, :], (128, NITER + 1))).astype(np.float32)
    return c


class Builder:
    def __init__(self, S, debug=False, stages=99):
        self.S = S
        self.debug = debug
        self.stages = stages
        self.nc = bass.Bass("TRN2", target_bir_lowering=False)
        self.c = Ctx(self.nc)
        self.inputs = {}
        self.outputs = {}
        self.qrr = 0

    def inp(self, name, shape, dtype=F32):
        b = self.c.dram(name, shape, dtype, kind="ExternalInput")
        self.inputs[name] = b
        return b

    def outp(self, name, shape, dtype=F32):
        b = self.c.dram(name, shape, dtype, kind="ExternalOutput")
        self.outputs[name] = b
        return b

    def scratch(self, name, shape, dtype):
        if self.debug:
            return self.outp(name, shape, dtype)
        return self.c.dram(name, shape, dtype, kind="Internal")

    def ld(self, out_ap, in_ap, reads, writes, q=None):
        if q is None:
            q = "sp"
        return self.c.dma(q, lambda e: e.dma_start(out=out_ap, in_=in_ap), reads=reads, writes=writes)

    def ldcast(self, out_ap, in_ap, reads, writes):
        return self.c.dma("pool", lambda e: e.dma_start(out=out_ap, in_=in_ap), reads=reads, writes=writes)

    def build(self):
        S = self.S
        c = self.c
        NT = S // 128
        NG = S // 512
        x_in = self.inp("x", [S, D])
        ccol = self.inp("ccol", [128, 8])
        pos = self.inp("pos", [128, S], I32)
        fing = self.inp("fing", [128, D])
        K = {}
        for nm, shp in [("ident", [128, 128]), ("bigi", [128, 128]), ("negones", [128, 128]),
                        ("ones", [128, 128]), ("sctab", [128, NFM]), ("ropec", [128, 2]),
                        ("maskb", [8, 128, 512]), ("maskc", [4, 128, 512]), ("onehot", [NHC, NHC, 128]),
                        ("pw", [128, NITER + 1])]:
            K[nm] = self.inp(nm, shp)
        L = []
        for l in range(DEPTH):
            d = {}
            for nm, shp in [("wfm", [D, NFM * 128]), ("bfm", [128, NFM]), ("wtm", [D, NTM]),
                            ("btm", [128, NTM]), ("adaw", [D, 6 * D]), ("adab_col", [128, 48]),
                            ("adab_grow", [128, 2 * D]), ("n1g", [128, 8]), ("n2g", [128, 8]),
                            ("bt", [NHB, 8, 128, 512]), ("wbr", [D, D]), ("wout", [D, D]),
                            ("wfi", [D, 2 * FFN]), ("wfo", [FFN, D])]:
                d[nm] = self.inp(f"{nm}{l}", shp)
            L.append(d)
        out = self.outp("out", [S, D])
        self.fm = self.scratch("fm", [FM_ROWS, S], BF16)
        self.vO = self.scratch("vO", [S, 16 * 65], BF16)
        self.fcT = self.scratch("fcT", [NHC, S], F32)
        self.FT = self.scratch("FT", [NHC, S], F32)
        self.ctab = self.scratch("ctab", [128, S], F32)
        self.stab = self.scratch("stab", [128, S], F32)
        self.mbd = self.scratch("mbd", [NT, 128, S], BF16)
        self.oT = self.scratch("oT", [D, S], BF16)
        self.xa = self.scratch("xa", [S, D], F32)
        self.xb = self.scratch("xb", [S, D], F32)
        self.wtokd = self.scratch("wtokd", [S, IDXH], F32)
        self.K = K
        self.ident = c.sbuf("ident", [128, 128], F32)
        self.identb = c.sbuf("identb", [128, 128], BF16)
        self.bigi = c.sbuf("bigi", [128, 128], BF16)
        self.negones = c.sbuf("negones", [128, 128], BF16)
        self.onesf = c.sbuf("onesf", [128, 128], F32)
        self.sctab = c.sbuf("sctab", [128, NFM], F32)
        self.ropec = c.sbuf("ropec", [128, 2], F32)
        self.ld(self.ident[:], K["ident"][:], [K["ident"]], [self.ident])
        self.ld(self.onesf[:], K["ones"][:], [K["ones"]], [self.onesf])
        self.ld(self.sctab[:], K["sctab"][:], [K["sctab"]], [self.sctab])
        self.ld(self.ropec[:], K["ropec"][:], [K["ropec"]], [self.ropec])
        self.ldcast(self.identb[:], K["ident"][:], [K["ident"]], [self.identb])
        self.ldcast(self.bigi[:], K["bigi"][:], [K["bigi"]], [self.bigi])
        self.ldcast(self.negones[:], K["negones"][:], [K["negones"]], [self.negones])
        self.modc = c.sbuf("modc", [128, 48], F32)
        self.g1c = c.sbuf("g1c", [128, 8], F32)
        self.g2c = c.sbuf("g2c", [128, 8], F32)
        self.bfe = c.sbuf("bfe", [128, NFM], F32)
        self.grow = c.sbuf("grow", [128, 2 * D], F32)
        self.cond2 = c.sbuf("cond2", [128, 8, 2], F32)
        self.condrep = c.sbuf("condrep", [128, 8, 128], F32)
        self.negF = c.sbuf("negF", [128, NT, NHC], F32)
        self.fgbc = c.sbuf("fgbc", [128, NHC, max(NG, 2)], F32)

        self.phase_setup(pos, ccol)
        xcur = x_in
        for l in range(DEPTH):
            if self.stages < 1:
                break
            self.phase_mod(L[l])
            self.phase_inproj(L[l], xcur)
            if self.stages < 2:
                break
            self.phase_F()
            self.phase_A1()
            if self.stages < 3:
                break
            self.phase_A2()
            self.phase_B(L[l])
            self.phase_C()
            if self.stages < 4:
                break
            self.phase_merge(L[l], xcur, self.xa)
            self.phase_ffn(L[l], self.xa, self.xb)
            xcur = self.xb
            if self.stages < 5:
                break
        if self.stages >= 5:
            self.phase_final(xcur, fing, out)
        else:
            pass
        c.barrier()
        c.finalize()
        c.replay()
        return self.nc

    def phase_setup(self, pos, ccol):
        c = self.c
        S = self.S
        with contextlib.ExitStack() as st:
            ct = c.sbuf("ct", [128, 8], F32, st)
            self.ld(ct[:], ccol[:], [ccol], [ct])
            cs = c.sbuf("cs", [128, 8], F32, st)
            c.op("act", lambda e: e.activation(out=cs[:], in_=ct[:], func=AF.Silu), [ct], [cs])
            c.op("dve", lambda e: e.tensor_copy(out=self.cond2[:, :, 0], in_=cs[:]), [cs], [self.cond2])
            c.op("dve", lambda e: e.tensor_copy(out=self.cond2[:, :, 1], in_=cs[:]), [cs], [self.cond2])
            for k in range(8):
                c.op("dve", lambda e, k=k: e.tensor_scalar(out=self.condrep[:, k, :], in0=self.onesf[:],
                                                           scalar1=cs[:, k:k + 1], scalar2=None, op0=ALU.mult),
                     [self.onesf, cs], [self.condrep])
            NB = 2
            pi_ = [c.sbuf("pi", [128, 512], I32, st) for _ in range(NB)]
            ang = [c.sbuf("ang", [128, 512], F32, st) for _ in range(NB)]
            t1 = [c.sbuf("t1", [128, 512], F32, st) for _ in range(NB)]
            t2 = [c.sbuf("t2", [128, 512], F32, st) for _ in range(NB)]
            res = [[c.sbuf("res", [128, 512], F32, st) for _ in range(2)] for _ in range(NB)]
            for g in range(S // 512):
                b = g % NB
                sl = slice(g * 512, (g + 1) * 512)
                self.ld(pi_[b][:], pos[:, sl], [pos], [pi_[b]])
                c.op("dve", lambda e, b=b: e.tensor_copy(out=ang[b][:], in_=pi_[b][:]), [pi_[b]], [ang[b]])
                c.op("dve", lambda e, b=b: e.tensor_scalar(out=ang[b][:], in0=ang[b][:], scalar1=self.ropec[:, 0:1],
                                                           scalar2=None, op0=ALU.mult), [ang[b], self.ropec], [ang[b]])
                for which in range(2):
                    shift = (np.pi / 2) if which == 0 else 0.0
                    if which == 0:
                        c.op("dve", lambda e, b=b: e.tensor_scalar(
                            out=t1[b][:], in0=ang[b][:], scalar1=float(1.0 / TWO_PI), scalar2=0.25,
                            op0=ALU.mult, op1=ALU.add), [ang[b]], [t1[b]])
                        c.op("dve", lambda e, b=b: e.tensor_scalar(
                            out=t1[b][:], in0=t1[b][:], scalar1=float(MAGIC), scalar2=None,
                            op0=ALU.add), [t1[b]], [t1[b]])
                    else:
                        c.op("dve", lambda e, b=b: e.tensor_scalar(
                            out=t1[b][:], in0=ang[b][:], scalar1=float(1.0 / TWO_PI), scalar2=float(MAGIC),
                            op0=ALU.mult, op1=ALU.add), [ang[b]], [t1[b]])
                    c.op("dve", lambda e, b=b: e.tensor_scalar(out=t1[b][:], in0=t1[b][:], scalar1=float(-MAGIC),
                                                               scalar2=None, op0=ALU.add), [t1[b]], [t1[b]])
                    c.op("dve", lambda e, b=b: e.scalar_tensor_tensor(out=t2[b][:], in0=t1[b][:], scalar=float(-CW1),
                                                                      in1=ang[b][:], op0=ALU.mult, op1=ALU.add),
                         [t1[b], ang[b]], [t2[b]])
                    c.op("dve", lambda e, b=b: e.scalar_tensor_tensor(out=t2[b][:], in0=t1[b][:], scalar=float(-CW2),
                                                                      in1=t2[b][:], op0=ALU.mult, op1=ALU.add),
                         [t1[b], t2[b]], [t2[b]])
                    c.op("dve", lambda e, b=b, shift=shift: e.tensor_scalar(
                        out=t2[b][:], in0=t2[b][:], scalar1=float(shift), scalar2=float(3.1415925),
                        op0=ALU.add, op1=ALU.min), [t2[b]], [t2[b]])
                    c.op("dve", lambda e, b=b: e.tensor_scalar(out=t2[b][:], in0=t2[b][:], scalar1=float(-3.1415925),
                                                               scalar2=None, op0=ALU.max), [t2[b]], [t2[b]])
                    if which == 0:
                        c.op("act", lambda e, b=b: e.activation(out=res[b][0][:], in_=t2[b][:], func=AF.Sin),
                             [t2[b]], [res[b][0]])
                        self.ld(self.ctab[:, sl], res[b][0][:], [res[b][0]], [self.ctab])
                    else:
                        c.op("act", lambda e, b=b: e.activation(out=res[b][1][:], in_=t2[b][:], func=AF.Sin,
                                                                scale=self.ropec[:, 1:2]),
                             [t2[b], self.ropec], [res[b][1]])
                        self.ld(self.stab[:, sl], res[b][1][:], [res[b][1]], [self.stab])
        c.barrier()

    def phase_mod(self, Ld):
        c = self.c
        with contextlib.ExitStack() as st:
            aw = [c.sbuf("aw", [128, 8, 512], F32, st) for _ in range(2)]
            pcol = c.psum("pcol", [128, 512], F32, st)
            prow = [c.psum("prow", [128, 512], F32, st) for _ in range(2)]
            abc = c.sbuf("abc", [128, 48], F32, st)
            abg = c.sbuf("abg", [128, 2 * D], F32, st)
            n1 = c.sbuf("n1", [128, 8], F32, st)
            n2 = c.sbuf("n2", [128, 8], F32, st)
            bfm = c.sbuf("bfm", [128, NFM], F32, st)
            self.ld(abc[:], Ld["adab_col"][:], [Ld["adab_col"]], [abc])
            self.ld(abg[:], Ld["adab_grow"][:], [Ld["adab_grow"]], [abg])
            self.ld(n1[:], Ld["n1g"][:], [Ld["n1g"]], [n1])
            self.ld(n2[:], Ld["n2g"][:], [Ld["n2g"]], [n2])
            self.ld(bfm[:], Ld["bfm"][:], [Ld["bfm"]], [bfm])
            c.op("dve", lambda e: e.tensor_tensor(out=self.bfe[:], in0=bfm[:], in1=self.sctab[:], op=ALU.mult),
                 [bfm, self.sctab], [self.bfe])
            adaw = Ld["adaw"]
            for j in range(12):
                b = j % 2
                src = adaw[:, j * 512:(j + 1) * 512].rearrange("(k p) n -> p k n", p=128)
                self.ld(aw[b][:], src, [adaw], [aw[b]], q=("sp" if j % 2 == 0 else "act"))
                for jj in range(4):
                    col = j * 4 + jj
                    for k in range(8):
                        c.op("pe", lambda e, b=b, jj=jj, k=k, col=col: e.matmul(
                            pcol[:, 2 * col:2 * col + 2], lhsT=aw[b][:, k, jj * 128:(jj + 1) * 128],
                            rhs=self.cond2[:, k, :], start=(k == 0), stop=(k == 7)),
                            [aw[b], self.cond2], [pcol])
                gi = {4: 0, 5: 1, 10: 2, 11: 3}.get(j)
                if gi is not None:
                    pr = prow[gi % 2]
                    for k in range(8):
                        c.op("pe", lambda e, b=b, k=k, pr=pr: e.matmul(
                            pr[:, :], lhsT=self.condrep[:, k, :], rhs=aw[b][:, k, :], start=(k == 0), stop=(k == 7)),
                            [aw[b], self.condrep], [pr])
                    c.op("dve", lambda e, pr=pr, gi=gi: e.tensor_tensor(
                        out=self.grow[:, gi * 512:(gi + 1) * 512], in0=pr[:, :], in1=abg[:, gi * 512:(gi + 1) * 512],
                        op=ALU.add), [pr, abg], [self.grow])
            pv = pcol[:, 0:96].rearrange("p (c t) -> p c t", t=2)[:, :, 0]
            c.op("dve", lambda e: e.tensor_tensor(out=self.modc[:], in0=pv, in1=abc[:], op=ALU.add),
                 [pcol, abc], [self.modc])
            c.op("dve", lambda e: e.scalar_tensor_tensor(out=self.g1c[:], in0=self.modc[:, 8:16], scalar=1.0,
                                                         in1=n1[:], op0=ALU.add, op1=ALU.mult),
                 [self.modc, n1], [self.g1c])
            c.op("dve", lambda e: e.scalar_tensor_tensor(out=self.g2c[:], in0=self.modc[:, 32:40], scalar=1.0,
                                                         in1=n2[:], op0=ALU.add, op1=ALU.mult),
                 [self.modc, n2], [self.g2c])
        c.barrier()

    def norm_transpose(self, st, xsrc, t0, hT, hslot, gcol, bcol0, xt, sq, ss, xs, tp):
        c = self.c
        self.ld(xt[:], xsrc[t0:t0 + 128, :], [xsrc], [xt])
        c.op("act", lambda e: e.activation(out=sq[:], in_=xt[:], func=AF.Square, accum_out=ss[:, 0:1]),
             [xt], [sq, ss])
        c.op("act", lambda e: e.activation(out=ss[:, 1:2], in_=ss[:, 0:1], func=AF.Sqrt, scale=float(1.0 / D),
                                           bias=self.epsc[:, 0:1]), [ss, self.epsc], [ss])
        c.op("dve", lambda e: e.reciprocal(out=ss[:, 2:3], in_=ss[:, 1:2]), [ss], [ss])
        c.op("dve", lambda e: e.tensor_scalar(out=xs[:], in0=xt[:], scalar1=ss[:, 2:3], scalar2=None, op0=ALU.mult),
             [xt, ss], [xs])
        for k in range(8):
            c.op("pe", lambda e, k=k: e.transpose(out=tp[k // 4][:, (k % 4) * 128:(k % 4 + 1) * 128],
                                                  in_=xs[:, k * 128:(k + 1) * 128], identity=self.ident[:]),
                 [xs, self.ident], [tp[k // 4]])
        for k in range(8):
            src = tp[k // 4][:, (k % 4) * 128:(k % 4 + 1) * 128]
            dst = hT[:, k, hslot * 128:(hslot + 1) * 128]
            if k % 2 == 0:
                c.op("act", lambda e, src=src, dst=dst, k=k: e.activation(
                    out=dst, in_=src, func=AF.Identity, scale=gcol[:, k:k + 1],
                    bias=self.modc[:, bcol0 + k:bcol0 + k + 1]), [tp[k // 4], gcol, self.modc], [hT])
            else:
                c.op("dve", lambda e, src=src, dst=dst, k=k: e.tensor_scalar(
                    out=dst, in0=src, scalar1=gcol[:, k:k + 1], scalar2=self.modc[:, bcol0 + k:bcol0 + k + 1],
                    op0=ALU.mult, op1=ALU.add), [tp[k // 4], gcol, self.modc], [hT])

    def phase_inproj(self, Ld, xsrc):
        c = self.c
        S = self.S
        NG = S // 512
        with contextlib.ExitStack() as st:
            self.epsc = c.sbuf("epsc", [128, 1], F32, st)
            c.op("dve", lambda e: e.memset(self.epsc[:], EPS), [], [self.epsc])
            wfm = c.sbuf("wfm", [128, 8, NFM * 128], BF16, st)
            wtm = c.sbuf("wtm", [128, 8, NTM], BF16, st)
            btm = c.sbuf("btm", [128, NTM], F32, st)
            self.ld(btm[:], Ld["btm"][:], [Ld["btm"]], [btm])
            for k in range(8):
                for j0 in range(0, NFM * 128, 2048):
                    j1 = min(NFM * 128, j0 + 2048)
                    self.ldcast(wfm[:, k, j0:j1], Ld["wfm"][k * 128:(k + 1) * 128, j0:j1], [Ld["wfm"]], [wfm])
                self.ldcast(wtm[:, k, :], Ld["wtm"][k * 128:(k + 1) * 128, :], [Ld["wtm"]], [wtm])
            hT = [c.sbuf("hT", [128, 8, 512], BF16, st) for _ in range(2)]
            xt = [c.sbuf("xt", [128, D], F32, st) for _ in range(2)]
            sq = c.sbuf("sq", [128, D], BF16, st)
            xs = [c.sbuf("xs", [128, D], F32, st) for _ in range(2)]
            ss = [c.sbuf("ss", [128, 4], F32, st) for _ in range(2)]
            tp = [[c.psum("tp", [128, 512], F32, st) for _ in range(2)] for _ in range(1)]
            pm = [c.psum("pm", [128, 512], F32, st) for _ in range(4)]
            ptm = [c.psum("ptm", [128, 512], F32, st) for _ in range(2)]
            NE = 10
            ev = [c.sbuf("ev", [128, 512], BF16, st) for _ in range(NE)]
            evf = [c.sbuf("evf", [128, 512], F32, st) for _ in range(2)]
            rR = [c.sbuf("rR", [128, 512], F32, st) for _ in range(2)]
            rS = [c.sbuf("rS", [128, 512], F32, st) for _ in range(2)]
            ctb = [c.sbuf("ctb", [128, 512], F32, st) for _ in range(2)]
            stb = [c.sbuf("stb", [128, 512], F32, st) for _ in range(2)]
            vo = [c.sbuf("vo", [128, 16, 65], BF16, st) for _ in range(2)]
            wt = [c.sbuf("wt", [128, IDXH], F32, st) for _ in range(2)]
            for b in range(2):
                c.op("pool", lambda e, b=b: e.memset(vo[b][:], 1.0), [], [vo[b]])
            tiles = PLAN["tiles"]
            iev = 0
            ipm = 0
            def prep(g):
                sl_ = slice(g * 512, (g + 1) * 512)
                self.ld(ctb[g % 2][:], self.ctab[:, sl_], [self.ctab], [ctb[g % 2]], q="sp")
                self.ld(stb[g % 2][:], self.stab[:, sl_], [self.stab], [stb[g % 2]], q="sp")
                for i in range(4):
                    tt = g * 4 + i
                    self.norm_transpose(st, xsrc, tt * 128, hT[g % 2], i, self.g1c, 0, xt[tt % 2], sq, ss[tt % 2],
                                        xs[tt % 2], tp[0])

            prep(0)
            for g in range(NG):
                hb = hT[g % 2]
                sl = slice(g * 512, (g + 1) * 512)
                for i in range(4):
                    tt = g * 4 + i
                    vb_ = vo[tt % 2]
                    for ci, (c0, c1, h0, nh) in enumerate([(0, 384, 0, 6), (384, 704, 6, 5), (704, 1024, 11, 5),
                                                            (1024, 1032, 0, 0)]):
                        pt = ptm[ci % 2]
                        n = c1 - c0
                        for k in range(8):
                            c.op("pe", lambda e, k=k, pt=pt, n=n, c0=c0, c1=c1, i=i, hb=hb: e.matmul(
                                pt[:, 0:n], lhsT=hb[:, k, i * 128:(i + 1) * 128], rhs=wtm[:, k, c0:c1],
                                start=(k == 0), stop=(k == 7)), [hb, wtm], [pt])
                        if nh > 0:
                            c.op("dve", lambda e, pt=pt, n=n, c0=c0, c1=c1, h0=h0, nh=nh, vb_=vb_: e.tensor_tensor(
                                out=vb_[:, h0:h0 + nh, 0:64], in0=pt[:, 0:n].rearrange("p (h d) -> p h d", d=64),
                                in1=btm[:, c0:c1].rearrange("p (h d) -> p h d", d=64), op=ALU.add),
                                [pt, btm], [vb_])
                        else:
                            wb_ = wt[tt % 2]
                            c.op("dve", lambda e, pt=pt, c0=c0, c1=c1, wb_=wb_: e.tensor_tensor(
                                out=wb_[:], in0=pt[:, 0:IDXH], in1=btm[:, c0:c1], op=ALU.add), [pt, btm], [wb_])
                            self.ld(self.wtokd[tt * 128:(tt + 1) * 128, :], wb_[:], [wb_], [self.wtokd])
                    self.ld(self.vO[tt * 128:(tt + 1) * 128, :], vb_[:].rearrange("p h d -> p (h d)"),
                            [vb_], [self.vO], q="pool")
                for j, t in enumerate(tiles):
                    if j == 28 and g + 1 < NG:
                        prep(g + 1)
                    ps = pm[ipm % 4]
                    ipm += 1
                    for k in range(8):
                        c.op("pe", lambda e, k=k, ps=ps, j=j, hb=hb: e.matmul(
                            ps[:, :], lhsT=wfm[:, k, j * 128:(j + 1) * 128], rhs=hb[:, k, :],
                            start=(k == 0), stop=(k == 7)), [wfm, hb], [ps])
                    kind = t["kind"]
                    if kind in ("ropeR", "ropeS"):
                        dst = (rR if kind == "ropeR" else rS)[(j // 2) % 2]
                        c.op("act", lambda e, ps=ps, dst=dst, j=j: e.activation(
                            out=dst[:], in_=ps[:, :], func=AF.Identity, scale=self.sctab[:, j:j + 1],
                            bias=self.bfe[:, j:j + 1]), [ps, self.sctab, self.bfe], [dst])
                        if kind == "ropeS":
                            a = rR[(j // 2) % 2]
                            b2 = rS[(j // 2) % 2]
                            o = ev[iev % NE]
                            iev += 1
                            c.op("dve", lambda e, a=a, g=g: e.tensor_tensor(out=a[:], in0=a[:], in1=ctb[g % 2][:],
                                                                            op=ALU.mult), [a, ctb[g % 2]], [a])
                            c.op("pool", lambda e, b2=b2, g=g: e.tensor_tensor(out=b2[:], in0=b2[:], in1=stb[g % 2][:],
                                                                              op=ALU.mult), [b2, stb[g % 2]], [b2])
                            c.op("dve", lambda e, a=a, b2=b2, o=o: e.tensor_tensor(out=o[:], in0=a[:], in1=b2[:],
                                                                                   op=ALU.add), [a, b2], [o])
                            for (r0, n, fr) in tiles[j - 1]["segs"]:
                                self.ld(self.fm[fr:fr + n, sl], o[r0:r0 + n, :], [o], [self.fm],
                                        q=("sp" if (r0 // 16) % 2 == 0 else "pool"))
                    else:
                        o = ev[iev % NE]
                        iev += 1
                        func = AF.Sigmoid if kind == "gate" else AF.Identity
                        c.op("act", lambda e, ps=ps, o=o, j=j, func=func: e.activation(
                            out=o[:], in_=ps[:, :], func=func, scale=self.sctab[:, j:j + 1],
                            bias=self.bfe[:, j:j + 1]), [ps, self.sctab, self.bfe], [o])
                        if kind == "plain_fc":
                            of = evf[g % 2]
                            c.op("dve", lambda e, ps=ps, of=of, j=j: e.tensor_scalar(
                                out=of[:], in0=ps[:, :], scalar1=self.bfe[:, j:j + 1], scalar2=None, op0=ALU.add),
                                [ps, self.bfe], [of])
                            self.ld(self.fcT[:, sl], of[112:112 + NHC, :], [of], [self.fcT])
                        for si, (r0, n, fr) in enumerate(t["segs"]):
                            self.ld(self.fm[fr:fr + n, sl], o[r0:r0 + n, :], [o], [self.fm],
                                    q=("sp" if si % 2 == 0 else "pool"))
        c.barrier()

    def phase_F(self):
        c = self.c
        S = self.S
        NT = S // 128
        NG = S // 512
        with contextlib.ExitStack() as st:
            fc = c.sbuf("fc", [NHC, S], F32, st)
            e1 = c.sbuf("e1", [NHC, S], F32, st)
            on = c.sbuf("on", [NHC, S], F32, st)
            G = c.sbuf("G", [NHC, S], F32, st)
            oh = c.sbuf("oh", [NHC, NHC, 128], F32, st)
            gs = c.sbuf("gs", [NHC, NG], F32, st)
            onec = c.sbuf("onec", [NHC, 1], F32, st)
            psT = c.psum("psT", [128, 512], F32, st)
            psb = c.psum("psb", [128, 512], F32, st)
            self.ld(fc[:], self.fcT[:], [self.fcT], [fc])
            self.ld(oh[:], self.K["onehot"][:], [self.K["onehot"]], [oh])
            c.op("pool", lambda e: e.memset(on[:], 1.0), [], [on])
            c.op("dve", lambda e: e.memset(onec[:], 1.0), [], [onec])
            c.op("act", lambda e: e.activation(out=e1[:], in_=fc[:], func=AF.Exp, scale=-1.0), [fc], [e1])
            c.op("act", lambda e: e.activation(out=e1[:], in_=e1[:], func=AF.Ln, bias=onec[:, 0:1]), [e1, onec], [e1])
            c.op("dve", lambda e: e.tensor_tensor_scan(out=G[:], data0=on[:], data1=e1[:], initial=0.0,
                                                       op0=ALU.mult, op1=ALU.add), [on, e1], [G])
            self.ld(self.FT[:], G[:], [G], [self.FT])
            for tt in range(NT):
                c.op("pe", lambda e, tt=tt: e.transpose(out=psT[:, tt * NHC:(tt + 1) * NHC],
                                                        in_=G[:, tt * 128:(tt + 1) * 128],
                                                        identity=self.ident[0:NHC, 0:NHC]), [G, self.ident], [psT])
            c.op("dve", lambda e: e.tensor_copy(out=self.negF[:].rearrange("p n h -> p (n h)"),
                                                in_=psT[:, 0:NT * NHC]), [psT], [self.negF])
            gview = G[:].rearrange("h (g t) -> h g t", t=512)[:, :, 511]
            c.op("dve", lambda e: e.tensor_copy(out=gs[:], in_=gview), [G], [gs])
            for h in range(NHC):
                c.op("pe", lambda e, h=h: e.matmul(psb[:, h * NG:(h + 1) * NG], lhsT=oh[:, h, :], rhs=gs[:],
                                                   start=True, stop=True), [oh, gs], [psb])
            c.op("dve", lambda e: e.tensor_copy(out=self.fgbc[:, :, 0:NG],
                                                in_=psb[:, 0:NHC * NG].rearrange("p (h g) -> p h g", g=NG)),
                 [psb], [self.fgbc])
        c.barrier()

    def phase_A1(self):
        c = self.c
        S = self.S
        NT = S // 128
        FA = 0.0
        with contextlib.ExitStack() as st:
            kiT = c.sbuf("kiT", [64, S], BF16, st)
            self.ld(kiT[:], self.fm[R_KI:R_KI + 64, :], [self.fm], [kiT])
            pw = c.sbuf("pw", [128, NITER + 1], F32, st)
            self.ld(pw[:], self.K["pw"][:], [self.K["pw"]], [pw])
            qib = [c.sbuf("qib", [64, IDXH, 128], BF16, st) for _ in range(2)]
            wtb = [c.sbuf("wtb", [128, IDXH], F32, st) for _ in range(2)]
            Dm = [c.sbuf("Dm", [128, IDXH, 128], BF16, st) for _ in range(2)]
            sc = [c.sbuf("sc", [128, S], F32, st) for _ in range(2)]
            junk = c.sbuf("junk", [128, S], BF16, st)
            junkA = c.sbuf("junkA", [128, S], BF16, st)
            mbq = [c.sbuf("mbq", [128, S], BF16, st) for _ in range(2)]
            Rb = [c.sbuf("Rb", [128, 512], BF16, st) for _ in range(4)]
            sm = [c.sbuf("sm", [128, 8], F32, st) for _ in range(2)]
            sa = [c.sbuf("sa", [128, 2], F32, st) for _ in range(2)]
            WT = [c.sbuf("WT", [128, NITER + 1], F32, st) for _ in range(2)]
            psz = [c.psum("psz", [128, 512], F32, st) for _ in range(4)]
            pss = [c.psum("pss", [128, 512], F32, st) for _ in range(2)]
            cnt = dict(iss=0, iz=0)

            def indexer(qb):
                p = qb % 2
                t0 = qb * 128
                nk = (qb + 1) * 128
                src = self.fm[R_QI:R_QI + IDXH * 64, t0:t0 + 128].rearrange("(h d) t -> d h t", d=64)
                self.ld(qib[p][:], src, [self.fm], [qib[p]])
                self.ld(wtb[p][:], self.wtokd[t0:t0 + 128, :], [self.wtokd], [wtb[p]])
                for h in range(IDXH):
                    c.op("pool", lambda e, h=h, p=p: e.tensor_scalar(
                        out=Dm[p][:, h, :], in0=self.identb[:], scalar1=wtb[p][:, h:h + 1], scalar2=None,
                        op0=ALU.mult), [self.identb, wtb[p]], [Dm[p]])
                scb = sc[p]
                for ks in range((nk + 511) // 512):
                    n = min(512, nk - ks * 512)
                    k0 = ks * 512
                    pacc = pss[cnt["iss"] % 2]
                    cnt["iss"] += 1

                    def zmm(h, n=n, k0=k0, p=p):
                        pz = psz[cnt["iz"] % 4]
                        rb = Rb[cnt["iz"] % 4]
                        cnt["iz"] += 1
                        c.op("pe", lambda e: e.matmul(pz[:, 0:n], lhsT=qib[p][:, h, :], rhs=kiT[:, k0:k0 + n],
                                                      start=True, stop=True), [qib[p], kiT], [pz])
                        c.op("act", lambda e: e.activation(out=rb[:, 0:n], in_=pz[:, 0:n], func=AF.Relu), [pz], [rb])
                        return rb

                    def wsm(h, rb, n=n, p=p, pacc=pacc):
                        c.op("pe", lambda e: e.matmul(pacc[:, 0:n], lhsT=Dm[p][:, h, :], rhs=rb[:, 0:n],
                                                      start=(h == 0), stop=(h == IDXH - 1)), [Dm[p], rb], [pacc])

                    rbs = {}
                    for h in range(IDXH + 2):
                        if h < IDXH:
                            rbs[h] = zmm(h)
                        if h >= 2:
                            wsm(h - 2, rbs[h - 2])
                    c.op("act", lambda e, pacc=pacc, scb=scb, n=n, k0=k0: e.copy(out=scb[:, k0:k0 + n],
                                                                                  in_=pacc[:, 0:n]), [pacc], [scb])

            def bisect(qb):
                p = qb % 2
                nk = (qb + 1) * 128
                scb = sc[p]
                s_ = sm[p]
                a_ = sa[p]
                wt_ = WT[p]
                nA = int(nk * FA) // 64 * 64 if nk >= 1024 else 0
                n1 = nk - nA
                c.op("dve", lambda e: e.tensor_reduce(out=s_[:, 5:6], in_=scb[:, 0:nk], axis=AX.X, op=ALU.max),
                     [scb], [s_])
                c.op("dve", lambda e: e.tensor_reduce(out=s_[:, 6:7], in_=scb[:, 0:nk], axis=AX.X, op=ALU.min),
                     [scb], [s_])
                c.op("dve", lambda e: e.memset(scb[0:64, nk - 64:nk], NEG), [], [scb])
                c.op("dve", lambda e: e.tensor_scalar(out=s_[:, 0:1], in0=s_[:, 5:6], scalar1=s_[:, 6:7],
                                                      scalar2=0.5, op0=ALU.add, op1=ALU.mult), [s_], [s_])
                c.op("dve", lambda e: e.tensor_scalar(out=s_[:, 1:2], in0=s_[:, 5:6], scalar1=s_[:, 6:7],
                                                      scalar2=0.5005, op0=ALU.subtract, op1=ALU.mult), [s_], [s_])
                c.op("dve", lambda e: e.tensor_scalar(out=s_[:, 1:2], in0=s_[:, 1:2], scalar1=1e-20,
                                                      scalar2=None, op0=ALU.add), [s_], [s_])
                c.op("dve", lambda e: e.tensor_scalar(out=wt_[:], in0=pw[:], scalar1=s_[:, 1:2],
                                                      scalar2=None, op0=ALU.mult), [pw, s_], [wt_])
                for it in range(NITER):
                    if nA > 0:
                        c.op("dve", lambda e: e.tensor_scalar(out=a_[:, 0:1], in0=s_[:, 0:1], scalar1=-1.0,
                                                              scalar2=None, op0=ALU.mult), [s_], [a_])
                        c.op("act", lambda e: e.activation(out=junkA[:, n1:nk], in_=scb[:, n1:nk], func=AF.Sign,
                                                           bias=a_[:, 0:1], accum_out=a_[:, 1:2]),
                             [scb, a_], [junkA, a_])
                    c.op("dve", lambda e, it=it: e.tensor_tensor(
                        out=s_[:, 2:3], in0=s_[:, 0:1], in1=wt_[:, it + 1:it + 2], op=ALU.subtract), [s_, wt_], [s_])
                    c.op("dve", lambda e: e.tensor_scalar(
                        out=junk[:, 0:n1], in0=scb[:, 0:n1], scalar1=s_[:, 0:1], scalar2=None, op0=ALU.is_ge,
                        op1=ALU.add, accum_out=s_[:, 3:4]), [scb, s_], [junk, s_])
                    if nA > 0:
                        c.op("dve", lambda e: e.scalar_tensor_tensor(
                            out=s_[:, 3:4], in0=a_[:, 1:2], scalar=0.5, in1=s_[:, 3:4], op0=ALU.mult, op1=ALU.add),
                            [a_, s_], [s_])
                    c.op("dve", lambda e: e.tensor_scalar(out=s_[:, 4:5], in0=s_[:, 3:4],
                                                          scalar1=float(TOPK - 0.5 * nA), scalar2=None,
                                                          op0=ALU.is_ge), [s_], [s_])
                    c.op("dve", lambda e, it=it: e.scalar_tensor_tensor(
                        out=s_[:, 0:1], in0=s_[:, 4:5], scalar=wt_[:, it:it + 1], in1=s_[:, 2:3],
                        op0=ALU.mult, op1=ALU.add), [s_, wt_], [s_])
                c.op("dve", lambda e: e.tensor_tensor(
                    out=s_[:, 7:8], in0=s_[:, 0:1], in1=wt_[:, NITER:NITER + 1], op=ALU.subtract), [s_, wt_], [s_])
                mq = mbq[p]
                c.op("dve", lambda e: e.tensor_scalar(
                    out=mq[:, 0:nk], in0=scb[:, 0:nk], scalar1=s_[:, 7:8], scalar2=-1.0, op0=ALU.is_ge, op1=ALU.add),
                    [scb, s_], [mq])
                self.ld(self.mbd[qb, :, 0:nk], mq[:, 0:nk], [mq], [self.mbd], q="sp")

            for qb in range(NT + 1):
                if qb < NT:
                    indexer(qb)
                if qb >= 1:
                    bisect(qb - 1)
        c.barrier()

    def normalize_store(self, oacc, pbc, osb, rec, on_, row0, g):
        c = self.c
        c.op("act", lambda e: e.copy(out=osb[0:65, :], in_=oacc[0:65, :]), [oacc], [osb])
        c.op("dve", lambda e: e.reciprocal(out=rec[64:65, :], in_=osb[64:65, :]), [osb], [rec])
        c.op("pe", lambda e: e.matmul(pbc[0:64, :], lhsT=self.onesf[64:65, 0:64], rhs=rec[64:65, :],
                                      start=True, stop=True), [self.onesf, rec], [pbc])
        c.op("dve", lambda e: e.tensor_tensor(out=on_[0:64, :], in0=osb[0:64, :], in1=pbc[0:64, :], op=ALU.mult),
             [osb, pbc], [on_])
        self.ld(self.oT[row0:row0 + 64, g * 512:(g + 1) * 512], on_[0:64, :], [on_], [self.oT])

    def load_kT(self, st, row0, nheads, name):
        c = self.c
        S = self.S
        kT = c.sbuf(name, [128, (nheads + 1) // 2, S], BF16, st)
        for pr in range((nheads + 1) // 2):
            n = min(128, nheads * 64 - pr * 128)
            self.ld(kT[0:n, pr, :], self.fm[row0 + pr * 128:row0 + pr * 128 + n, :], [self.fm], [kT],
                    q=("sp" if pr % 2 == 0 else "act"))
        return kT

    def load_q(self, qg, row0, nheads, g):
        for pr in range((nheads + 1) // 2):
            n = min(128, nheads * 64 - pr * 128)
            self.ld(qg[0:n, pr, :], self.fm[row0 + pr * 128:row0 + pr * 128 + n, g * 512:(g + 1) * 512],
                    [self.fm], [qg])

    @staticmethod
    def pipeline(blocks, first, rest, la):
        n = len(blocks)
        for idx in range(n + la):
            if idx < n:
                first(idx, blocks[idx])
            if idx - la >= 0:
                rest(idx - la, blocks[idx - la])

    def phase_A2(self):
        c = self.c
        S = self.S
        NT = S // 128
        NG = S // 512
        with contextlib.ExitStack() as st:
            kT = self.load_kT(st, R_KA, NHA, "kTa")
            vO = c.sbuf("vOa", [128, NT, NHA * 65], BF16, st)
            self.ld(vO[:], self.vO[:, 0:NHA * 65].rearrange("(n p) c -> p n c", p=128), [self.vO], [vO], q="act")
            onec = c.sbuf("onecA", [128, 1], F32, st)
            c.op("dve", lambda e: e.memset(onec[:], 1.0), [], [onec])
            qg = [c.sbuf("qga", [128, 3, 512], BF16, st) for _ in range(2)]
            mbc = [c.sbuf("mbc", [128, 4, 512], BF16, st) for _ in range(2)]
            m01 = [c.sbuf("m01", [128, 512], BF16, st) for _ in range(4)]
            pt = [c.sbuf("pta", [128, 512], BF16, st) for _ in range(3)]
            pm = [c.sbuf("ptm", [128, 512], BF16, st) for _ in range(3)]
            osb = [c.sbuf("osb", [128, 512], F32, st) for _ in range(2)]
            rec = [c.sbuf("rec", [128, 512], F32, st) for _ in range(2)]
            on_ = [c.sbuf("on_", [64, 512], BF16, st) for _ in range(2)]
            oacc = [c.psum("oacc", [128, 512], F32, st) for _ in range(3)]
            pst = [c.psum("pst", [128, 512], F32, st) for _ in range(3)]
            pmk = [c.psum("pmk", [128, 512], F32, st) for _ in range(2)]
            blocks = []
            for g in range(NG):
                for hp in range(2):
                    for kta in range(4 * (g + 1)):
                        for hh in range(3):
                            blocks.append((g, hp, kta, hh))
            state = dict(imb=0, imk=0, mb_=None, q_=None)
            ctx = {}
            masks = {}

            def build_mask(g, hp, kta):
                ksup, kt = kta // 4, kta % 4
                if kt == 0:
                    state["mb_"] = mbc[state["imb"] % 2]
                    state["imb"] += 1
                    self.ld(state["mb_"][:], self.mbd[4 * g:4 * g + 4, :, ksup * 512:(ksup + 1) * 512].rearrange(
                        "i p k -> p i k"), [self.mbd], [state["mb_"]], q="sp")
                mb_ = state["mb_"]
                pk = pmk[state["imk"] % 2]
                m_ = m01[state["imk"] % 4]
                state["imk"] += 1
                for i in range(4):
                    vis = kta <= 4 * g + i
                    lhs = mb_[:, i, kt * 128:(kt + 1) * 128] if vis else self.negones[:]
                    rd = [mb_, self.identb] if vis else [self.negones, self.identb]
                    c.op("pe", lambda e, pk=pk, lhs=lhs, i=i: e.matmul(
                        pk[:, i * 128:(i + 1) * 128], lhsT=lhs, rhs=self.identb[:], start=True, stop=True),
                        rd, [pk])
                c.op("dve", lambda e, pk=pk, m_=m_: e.tensor_scalar(out=m_[:], in0=pk[:, :], scalar1=1.0,
                                                                    scalar2=None, op0=ALU.add), [pk], [m_])
                masks[(g, hp, kta)] = m_

            def first(idx, blk):
                g, hp, kta, hh = blk
                h = hp * 3 + hh
                nkt = 4 * (g + 1)
                if kta == 0 and hh == 0 and hp == 0:
                    state["q_"] = qg[g % 2]
                    self.load_q(state["q_"], R_QA, NHA, g)
                if hh == 0:
                    if kta == 0:
                        build_mask(g, hp, 0)
                    if kta + 1 < nkt:
                        build_mask(g, hp, kta + 1)
                q_ = state["q_"]
                ps = pst[idx % 3]
                pb = 64 * (h % 2)
                k0 = kta * 128
                c.op("pe", lambda e: e.matmul(ps[:, :], lhsT=kT[pb:pb + 64, h // 2, k0:k0 + 128],
                                              rhs=q_[pb:pb + 64, h // 2, :], start=True, stop=True), [kT, q_], [ps])
                ctx[idx] = (ps, masks[(g, hp, kta)])

            def rest(idx, blk):
                g, hp, kta, hh = blk
                h = hp * 3 + hh
                ps, m_ = ctx.pop(idx)
                pt_ = pt[idx % 3]
                pm_ = pm[idx % 3]
                nkt = 4 * (g + 1)
                c.op("act", lambda e: e.activation(out=pt_[:], in_=ps[:, :], func=AF.Exp), [ps], [pt_])
                c.op("dve", lambda e: e.tensor_tensor(out=pm_[:], in0=pt_[:], in1=m_[:], op=ALU.mult), [pt_, m_], [pm_])
                c.op("pe", lambda e: e.matmul(oacc[hh][0:65, :], lhsT=vO[:, kta, h * 65:(h + 1) * 65], rhs=pm_[:],
                                              start=(kta == 0), stop=(kta == nkt - 1)), [vO, pm_], [oacc[hh]])
                if kta == nkt - 1:
                    self.normalize_store(oacc[hh], pmk[hh % 2], osb[h % 2], rec[h % 2], on_[h % 2], h * 64, g)

            self.pipeline(blocks, first, rest, 2)
        c.barrier()

    def phase_B(self, Ld):
        c = self.c
        S = self.S
        NT = S // 128
        NG = S // 512
        with contextlib.ExitStack() as st:
            kT = self.load_kT(st, R_KB, NHB, "kTb")
            vO = c.sbuf("vOb", [128, NT, NHB * 65], BF16, st)
            self.ld(vO[:], self.vO[:, NHA * 65:(NHA + NHB) * 65].rearrange("(n p) c -> p n c", p=128),
                    [self.vO], [vO], q="act")
            btm = c.sbuf("btmB", [128, NHB, 8, 512], BF16, st)
            mk = c.sbuf("mk", [128, 8, 512], F32, st)
            btf = [c.sbuf("btf", [128, 512], F32, st) for _ in range(2)]
            self.ld(mk[:], self.K["maskb"][:].rearrange("r p q -> p r q"), [self.K["maskb"]], [mk])
            for h in range(NHB):
                for r in range(8):
                    bf = btf[(h * 8 + r) % 2]
                    self.ld(bf[:], Ld["bt"][h, r], [Ld["bt"]], [bf])
                    c.op("pool", lambda e, bf=bf, h=h, r=r: e.tensor_tensor(out=btm[:, h, r, :], in0=bf[:],
                                                                          in1=mk[:, r, :], op=ALU.add),
                         [bf, mk], [btm])
            qg = [c.sbuf("qgb", [128, 3, 512], BF16, st) for _ in range(2)]
            pt = [c.sbuf("ptb", [128, 512], BF16, st) for _ in range(3)]
            osb = [c.sbuf("osb", [128, 512], F32, st) for _ in range(2)]
            rec = [c.sbuf("rec", [128, 512], F32, st) for _ in range(2)]
            on_ = [c.sbuf("on_", [64, 512], BF16, st) for _ in range(2)]
            oacc = [c.psum("oacc", [128, 512], F32, st) for _ in range(NHB)]
            pst = [c.psum("pst", [128, 512], F32, st) for _ in range(3)]
            blocks = []
            for g in range(NG):
                rs = [r for r in range(8) if 4 * g - 4 + r >= 0]
                for r in rs:
                    for h in range(NHB):
                        blocks.append((g, r, h, rs[0], rs[-1]))
            state = dict(q_=None)
            ctx = {}

            def first(idx, blk):
                g, r, h, r0, r1 = blk
                if r == r0 and h == 0:
                    state["q_"] = qg[g % 2]
                    self.load_q(state["q_"], R_QB, NHB, g)
                q_ = state["q_"]
                ps = pst[idx % 3]
                pb = 64 * (h % 2)
                k0 = (4 * g - 4 + r) * 128
                c.op("pe", lambda e: e.matmul(ps[:, :], lhsT=kT[pb:pb + 64, h // 2, k0:k0 + 128],
                                              rhs=q_[pb:pb + 64, h // 2, :], start=True, stop=False), [kT, q_], [ps])
                c.op("pe", lambda e: e.matmul(ps[:, :], lhsT=self.identb[:], rhs=btm[:, h, r, :], start=False,
                                              stop=True), [self.identb, btm], [ps])
                ctx[idx] = ps

            def rest(idx, blk):
                g, r, h, r0, r1 = blk
                ps = ctx.pop(idx)
                pt_ = pt[idx % 3]
                kta = 4 * g - 4 + r
                c.op("act", lambda e: e.activation(out=pt_[:], in_=ps[:, :], func=AF.Exp), [ps], [pt_])
                c.op("pe", lambda e: e.matmul(oacc[h][0:65, :], lhsT=vO[:, kta, h * 65:(h + 1) * 65], rhs=pt_[:],
                                              start=(r == r0), stop=(r == r1)), [vO, pt_], [oacc[h]])
                if r == r1:
                    self.normalize_store(oacc[h], pst[(idx + 2) % 3], osb[h % 2], rec[h % 2], on_[h % 2],
                                         WA + h * 64, g)

            self.pipeline(blocks, first, rest, 1)
        c.barrier()

    def phase_C(self):
        c = self.c
        S = self.S
        NT = S // 128
        NG = S // 512
        with contextlib.ExitStack() as st:
            kA = c.sbuf("kA", [65, NHC, S], BF16, st)
            for h in range(NHC):
                self.ld(kA[0:64, h, :], self.fm[R_KC + h * 64:R_KC + (h + 1) * 64, :], [self.fm], [kA],
                        q=("sp" if h % 2 == 0 else "act"))
            c.op("dve", lambda e: e.memset(kA[64:65, :, :], 1.0), [], [kA])
            vO = c.sbuf("vOc", [128, NT, NHC * 65], BF16, st)
            self.ld(vO[:], self.vO[:, (NHA + NHB) * 65:16 * 65].rearrange("(n p) c -> p n c", p=128),
                    [self.vO], [vO], q="act")
            cbm = c.sbuf("cbm", [128, 4, 512], BF16, st)
            self.ldcast(cbm[:], self.K["maskc"][:].rearrange("r p q -> p r q"), [self.K["maskc"]], [cbm])
            qa_ = [c.sbuf("qaug", [65, NHC, 512], BF16, st) for _ in range(2)]
            gq = [c.sbuf("gq", [65, NHC, 512], F32, st) for _ in range(2)]
            kb = [c.sbuf("kb", [128, NHC, NT], F32, st) for _ in range(2)]
            pt = [c.sbuf("ptc", [128, 512], BF16, st) for _ in range(3)]
            osb = [c.sbuf("osb", [128, 512], F32, st) for _ in range(2)]
            rec = [c.sbuf("rec", [128, 512], F32, st) for _ in range(2)]
            on_ = [c.sbuf("on_", [64, 512], BF16, st) for _ in range(2)]
            oacc = [c.psum("oacc", [128, 512], F32, st) for _ in range(NHC)]
            pst = [c.psum("pst", [128, 512], F32, st) for _ in range(3)]
            blocks = []
            for g in range(NG):
                for kta in range(4 * (g + 1)):
                    for h in range(NHC):
                        blocks.append((g, kta, h))
            state = dict(q_=None, kb_=None)
            ctx = {}

            def first(idx, blk):
                g, kta, h = blk
                nkt = 4 * (g + 1)
                if kta == 0 and h == 0:
                    q_ = qa_[g % 2]
                    gq_ = gq[g % 2]
                    kb_ = kb[g % 2]
                    state["q_"] = q_
                    state["kb_"] = kb_
                    for hh in range(NHC):
                        self.ld(q_[0:64, hh, :], self.fm[R_QC + hh * 64:R_QC + (hh + 1) * 64, g * 512:(g + 1) * 512],
                                [self.fm], [q_])
                    self.ld(gq_[64:65, :, :], self.FT[:, g * 512:(g + 1) * 512].rearrange("(o h) t -> o h t", o=1),
                            [self.FT], [gq_], q="sp")
                    for hh in range(NHC):
                        c.op("dve", lambda e, hh=hh: e.tensor_scalar(
                            out=q_[64:65, hh, :], in0=gq_[64:65, hh, :], scalar1=self.fgbc[64:65, hh, g:g + 1],
                            scalar2=-1.0, op0=ALU.subtract, op1=ALU.mult), [gq_, self.fgbc], [q_])
                        c.op("dve", lambda e, hh=hh: e.tensor_scalar(
                            out=kb_[:, hh, 0:nkt], in0=self.negF[:, 0:nkt, hh], scalar1=self.fgbc[:, hh, g:g + 1],
                            scalar2=None, op0=ALU.subtract), [self.negF, self.fgbc], [kb_])
                q_ = state["q_"]
                ps = pst[idx % 3]
                k0 = kta * 128
                diag = kta >= 4 * g
                c.op("pe", lambda e: e.matmul(ps[:, :], lhsT=kA[0:65, h, k0:k0 + 128], rhs=q_[0:65, h, :],
                                              start=True, stop=(not diag)), [kA, q_], [ps])
                if diag:
                    c.op("pe", lambda e: e.matmul(ps[:, :], lhsT=self.identb[:], rhs=cbm[:, kta - 4 * g, :],
                                                  start=False, stop=True), [self.identb, cbm], [ps])
                ctx[idx] = (ps, state["kb_"])

            def rest(idx, blk):
                g, kta, h = blk
                nkt = 4 * (g + 1)
                ps, kb_ = ctx.pop(idx)
                pt_ = pt[idx % 3]
                c.op("act", lambda e: e.activation(out=pt_[:], in_=ps[:, :], func=AF.Exp,
                                                   bias=kb_[:, h, kta:kta + 1]), [ps, kb_], [pt_])
                c.op("pe", lambda e: e.matmul(oacc[h][0:65, :], lhsT=vO[:, kta, h * 65:(h + 1) * 65], rhs=pt_[:],
                                              start=(kta == 0), stop=(kta == nkt - 1)), [vO, pt_], [oacc[h]])
                if kta == nkt - 1:
                    self.normalize_store(oacc[h], pst[(idx + 2) % 3], osb[h % 2], rec[h % 2], on_[h % 2],
                                         WA + WB_ + h * 64, g)

            self.pipeline(blocks, first, rest, 1)
        c.barrier()

    def load_folded(self, st, wsrc, nk, gcol0, name):
        c = self.c
        w = c.sbuf(name, [128, nk, D], BF16, st)
        with contextlib.ExitStack() as st2:
            stg = [c.sbuf("stg", [128, D], F32, st2) for _ in range(2)]
            for k in range(nk):
                sg = stg[k % 2]
                self.ld(sg[:], wsrc[k * 128:(k + 1) * 128, :], [wsrc], [sg], q=("sp" if k % 2 == 0 else "act"))
                eng = "dve" if k % 2 == 0 else "pool"
                c.op(eng, lambda e, sg=sg, k=k: e.tensor_tensor(out=w[:, k, :], in0=sg[:],
                                                               in1=self.grow[:, gcol0:gcol0 + D], op=ALU.mult),
                     [sg, self.grow], [w])
            c.barrier()
        return w

    def phase_merge(self, Ld, xsrc, xdst):
        c = self.c
        S = self.S
        NG = S // 512
        with contextlib.ExitStack() as st:
            wbr = c.sbuf("wbr", [128, 8, D], BF16, st)
            for k in range(8):
                self.ldcast(wbr[:, k, :], Ld["wbr"][k * 128:(k + 1) * 128, :], [Ld["wbr"]], [wbr])
            wout = self.load_folded(st, Ld["wout"], 8, 0, "wout")
            oTs = [c.sbuf("oTs", [128, 8, 512], BF16, st) for _ in range(2)]
            gts = [c.sbuf("gts", [128, 24, 512], BF16, st) for _ in range(2)]
            yT = [c.sbuf("yT", [128, 8, 512], BF16, st) for _ in range(2)]
            ta = [c.sbuf("ta", [128, 512], F32, st) for _ in range(2)]
            tb = [c.sbuf("tb", [128, 512], F32, st) for _ in range(2)]
            tc = [c.sbuf("tc", [128, 512], F32, st) for _ in range(2)]
            xt = [c.sbuf("xtm", [128, D], F32, st) for _ in range(2)]
            xn = [c.sbuf("xnm", [128, D], F32, st) for _ in range(2)]
            pbr = [[c.psum("pbr", [128, 512], F32, st) for _ in range(3)] for _ in range(2)]
            pso = [c.psum("pso", [128, 512], F32, st) for _ in range(2)]
            pieces = [[(0, 0, 128), (1, 0, 128), (2, 0, 128)],
                      [(3, 0, 128), (4, 0, 128), (5, 0, 64)],
                      [(5, 64, 128), (6, 0, 128), (7, 0, 128)]]
            ipo = 0
            for g in range(NG):
                sl = slice(g * 512, (g + 1) * 512)
                o_ = oTs[g % 2]
                g_ = gts[g % 2]
                y_ = yT[g % 2]
                self.ld(o_[:], self.oT[:, sl].rearrange("(k p) t -> p k t", p=128), [self.oT], [o_])
                self.ld(g_[:], self.fm[R_G:R_G + 3 * D, sl].rearrange("(j p) t -> p j t", p=128), [self.fm], [g_],
                        q="pool")
                for dc in range(8):
                    pb = pbr[dc % 2]
                    for br in range(3):
                        for pi, (k, p0, p1) in enumerate(pieces[br]):
                            c.op("pe", lambda e, pb=pb, br=br, k=k, p0=p0, p1=p1, dc=dc, pi=pi, o_=o_: e.matmul(
                                pb[br][:, :], lhsT=wbr[p0:p1, k, dc * 128:(dc + 1) * 128], rhs=o_[p0:p1, k, :],
                                start=(pi == 0), stop=(pi == 2)), [wbr, o_], [pb[br]])
                    a_, b_, c_ = ta[dc % 2], tb[dc % 2], tc[dc % 2]
                    for br, dst in enumerate([a_, b_, c_]):
                        c.op("dve", lambda e, pb=pb, br=br, dst=dst, g_=g_, dc=dc: e.tensor_tensor(
                            out=dst[:], in0=pb[br][:, :], in1=g_[:, br * 8 + dc, :], op=ALU.mult), [pb[br], g_], [dst])
                    c.op("pool", lambda e, a_=a_, b_=b_: e.tensor_tensor(out=a_[:], in0=a_[:], in1=b_[:], op=ALU.add),
                         [a_, b_], [a_])
                    c.op("pool", lambda e, a_=a_, c_=c_, y_=y_, dc=dc: e.tensor_tensor(out=y_[:, dc, :], in0=a_[:],
                                                                                       in1=c_[:], op=ALU.add),
                         [a_, c_], [y_])
                for i in range(4):
                    tt = g * 4 + i
                    x_ = xt[tt % 2]
                    n_ = xn[tt % 2]
                    self.ld(x_[:], xsrc[tt * 128:(tt + 1) * 128, :], [xsrc], [x_])
                    for half in range(2):
                        po = pso[ipo % 2]
                        ipo += 1
                        for k in range(8):
                            c.op("pe", lambda e, po=po, k=k, i=i, half=half, y_=y_: e.matmul(
                                po[:, :], lhsT=y_[:, k, i * 128:(i + 1) * 128], rhs=wout[:, k, half * 512:(half + 1) * 512],
                                start=(k == 0), stop=(k == 7)), [y_, wout], [po])
                        c.op("dve", lambda e, po=po, x_=x_, n_=n_, half=half: e.tensor_tensor(
                            out=n_[:, half * 512:(half + 1) * 512], in0=po[:, :], in1=x_[:, half * 512:(half + 1) * 512],
                            op=ALU.add), [po, x_], [n_])
                    self.ld(xdst[tt * 128:(tt + 1) * 128, :], n_[:], [n_], [xdst], q="pool")
        c.barrier()

    def phase_ffn(self, Ld, xsrc, xdst):
        c = self.c
        S = self.S
        NG = S // 512
        NF = FFN // 128
        with contextlib.ExitStack() as st:
            self.epsc = c.sbuf("epsc", [128, 1], F32, st)
            c.op("dve", lambda e: e.memset(self.epsc[:], EPS), [], [self.epsc])
            wfi = c.sbuf("wfi", [128, 8, 2 * FFN], BF16, st)
            for k in range(8):
                for j0 in range(0, 2 * FFN, 2048):
                    j1 = min(2 * FFN, j0 + 2048)
                    self.ldcast(wfi[:, k, j0:j1], Ld["wfi"][k * 128:(k + 1) * 128, j0:j1], [Ld["wfi"]], [wfi])
            wfo = self.load_folded(st, Ld["wfo"], NF, D, "wfo")
            hT = [c.sbuf("hTf", [128, 8, 512], BF16, st)]
            aT = c.sbuf("aT", [128, NF, 512], BF16, st)
            xt = [c.sbuf("xtf", [128, D], F32, st) for _ in range(2)]
            sq = c.sbuf("sqf", [128, D], BF16, st)
            xs = [c.sbuf("xsf", [128, D], F32, st)]
            ss = [c.sbuf("ssf", [128, 4], F32, st) for _ in range(2)]
            sg = [c.sbuf("sgf", [128, 512], F32, st) for _ in range(2)]
            xn = [c.sbuf("xnf", [128, D], F32, st)]
            tp = [c.psum("tpf", [128, 512], F32, st) for _ in range(2)]
            pg = [c.psum("pg", [128, 512], F32, st) for _ in range(2)]
            pu = [c.psum("pu", [128, 512], F32, st) for _ in range(2)]
            pso = [c.psum("psof", [128, 512], F32, st) for _ in range(2)]

            def prep_tile(g, i):
                tt = g * 4 + i
                self.norm_transpose(st, xsrc, tt * 128, hT[0], i, self.g2c, 24, xt[tt % 2], sq, ss[tt % 2],
                                    xs[0], tp)

            for i in range(4):
                prep_tile(0, i)
            ipo = 0
            for g in range(NG):
                hb = hT[0]
                for f in range(NF):
                    pg_ = pg[f % 2]
                    pu_ = pu[f % 2]
                    for k in range(8):
                        c.op("pe", lambda e, pg_=pg_, k=k, f=f: e.matmul(
                            pg_[:, :], lhsT=wfi[:, k, f * 128:(f + 1) * 128], rhs=hb[:, k, :], start=(k == 0),
                            stop=(k == 7)), [wfi, hb], [pg_])
                    for k in range(8):
                        c.op("pe", lambda e, pu_=pu_, k=k, f=f: e.matmul(
                            pu_[:, :], lhsT=wfi[:, k, FFN + f * 128:FFN + (f + 1) * 128], rhs=hb[:, k, :],
                            start=(k == 0), stop=(k == 7)), [wfi, hb], [pu_])
                    s_ = sg[f % 2]
                    c.op("act", lambda e, pg_=pg_, s_=s_: e.activation(out=s_[:], in_=pg_[:, :], func=AF.Silu),
                         [pg_], [s_])
                    c.op("dve", lambda e, pu_=pu_, s_=s_, f=f: e.tensor_tensor(out=aT[:, f, :], in0=pu_[:, :],
                                                                              in1=s_[:], op=ALU.mult),
                         [pu_, s_], [aT])
                for i in range(4):
                    tt = g * 4 + i
                    n_ = xn[0]
                    x_ = xt[tt % 2]
                    self.ld(x_[:], xsrc[tt * 128:(tt + 1) * 128, :], [xsrc], [x_])
                    for half in range(2):
                        po = pso[ipo % 2]
                        ipo += 1
                        for f in range(NF):
                            c.op("pe", lambda e, po=po, f=f, i=i, half=half: e.matmul(
                                po[:, :], lhsT=aT[:, f, i * 128:(i + 1) * 128],
                                rhs=wfo[:, f, half * 512:(half + 1) * 512], start=(f == 0), stop=(f == NF - 1)),
                                [aT, wfo], [po])
                        c.op("dve", lambda e, po=po, x_=x_, n_=n_, half=half: e.tensor_tensor(
                            out=n_[:, half * 512:(half + 1) * 512], in0=po[:, :], in1=x_[:, half * 512:(half + 1) * 512],
                            op=ALU.add), [po, x_], [n_])
                    self.ld(xdst[tt * 128:(tt + 1) * 128, :], n_[:], [n_], [xdst], q="pool")
                    if g + 1 < NG:
                        prep_tile(g + 1, i)
        c.barrier()

    def phase_final(self, xsrc, fing, out):
        c = self.c
        S = self.S
        NT = S // 128
        with contextlib.ExitStack() as st:
            epsc = c.sbuf("epsc", [128, 1], F32, st)
            c.op("dve", lambda e: e.memset(epsc[:], EPS), [], [epsc])
            fg = c.sbuf("fg", [128, D], F32, st)
            self.ld(fg[:], fing[:], [fing], [fg])
            xt = [c.sbuf("xtz", [128, D], F32, st) for _ in range(3)]
            sq = c.sbuf("sqz", [128, D], BF16, st)
            ss = [c.sbuf("ssz", [128, 4], F32, st) for _ in range(3)]
            yo = [c.sbuf("yo", [128, D], F32, st) for _ in range(3)]
            for tt in range(NT):
                x_ = xt[tt % 3]
                s_ = ss[tt % 3]
                y_ = yo[tt % 3]
                self.ld(x_[:], xsrc[tt * 128:(tt + 1) * 128, :], [xsrc], [x_])
                c.op("act", lambda e, x_=x_, s_=s_: e.activation(out=sq[:], in_=x_[:], func=AF.Square,
                                                                 accum_out=s_[:, 0:1]), [x_], [sq, s_])
                c.op("act", lambda e, s_=s_: e.activation(out=s_[:, 1:2], in_=s_[:, 0:1], func=AF.Sqrt,
                                                          scale=float(1.0 / D), bias=epsc[:, 0:1]), [s_, epsc], [s_])
                c.op("dve", lambda e, s_=s_: e.reciprocal(out=s_[:, 2:3], in_=s_[:, 1:2]), [s_], [s_])
                c.op("dve", lambda e, x_=x_, s_=s_, y_=y_: e.scalar_tensor_tensor(
                    out=y_[:], in0=x_[:], scalar=s_[:, 2:3], in1=fg[:], op0=ALU.mult, op1=ALU.mult),
                    [x_, s_, fg], [y_])
                self.ld(out[tt * 128:(tt + 1) * 128, :], y_[:], [y_], [out], q="pool")
        c.barrier()


def build_program(S, debug=False, stages=99):
    b = Builder(S, debug, stages)
    nc = b.build()
    return nc, b


def prep_inputs(S, x, c, positions, ada_w, ada_b, norm1_g, w_in, b_in, rel_bias, w_branch, w_out,
                norm2_g, w_ffn_in, w_ffn_out, final_g, batches):
    shared = {}
    shared.update(host_consts())
    for l in range(DEPTH):
        d = host_layer_inputs(l, w_in, b_in, ada_w, ada_b, norm1_g, norm2_g, rel_bias, w_branch, w_out,
                              w_ffn_in, w_ffn_out)
        for k, v in d.items():
            shared[f"{k}{l}"] = v
    shared["fing"] = np.ascontiguousarray(np.broadcast_to(np.asarray(final_g, np.float32)[None, :], (128, D)))
    maps = []
    for b in batches:
        m = dict(shared)
        m["x"] = np.ascontiguousarray(np.asarray(x[b], np.float32))
        m["ccol"] = np.ascontiguousarray(np.asarray(c[b], np.float32).reshape(8, 128).T)
        m["pos"] = np.ascontiguousarray(np.broadcast_to(np.asarray(positions[b], np.int32)[None, :], (128, S)))
        maps.append(m)
    return maps


_CACHE = {}


def kernel(**inputs):
    x = np.asarray(inputs["x"])
    B, S, _ = x.shape
    if S not in _CACHE:
        _CACHE[S] = build_program(S)
    nc, b = _CACHE[S]
    batches = [i % B for i in range(8)]
    maps = prep_inputs(S, batches=batches, **inputs)
    res = run_bass_kernel_spmd(nc, maps, core_ids=list(range(8)))
    outs = [np.asarray(res.results[i]["out"]) for i in range(B)]
    return np.stack(outs, 0).astype(np.float32)
```

```python
import contextlib
import numpy as np
import concourse.bass as bass
import concourse.mybir as mybir
from concourse.bass_utils import run_bass_kernel_spmd

F32 = mybir.dt.float32
BF16 = mybir.dt.bfloat16
I32 = mybir.dt.int32
AF = mybir.ActivationFunctionType
ALU = mybir.AluOpType
AX = mybir.AxisListType

ENGS = ("pe", "act", "dve", "pool", "sp")
DMA_K = 24
EPOCH = 12000

D = 1024
DEPTH = 2
HD = 64
NHA, NHB, NHC = 6, 5, 5
WA, WB_, WC = NHA * HD, NHB * HD, NHC * HD
IDXH = 8
TOPK = 256
FFN = 2816
EPS = 1e-6
SPLIT = (WA, WA, WA, IDXH * HD, HD, IDXH, 3 * WB_, 3 * WC, NHC, 3 * D)
INW = sum(SPLIT)
OFF = np.cumsum((0,) + SPLIT)
O_QA, O_KA, O_VA, O_QI, O_KI, O_WI, O_QKVB, O_QKVC, O_FC, O_GATE = [int(v) for v in OFF[:10]]
BIG = 30000.0
NEG = -1.0e30
NITER = 13
TWO_PI = 2.0 * np.pi
CW1 = 6.28125
CW2 = float(TWO_PI - 6.28125)
MAGIC = 12582912.0

R_QA, R_KA, R_QI, R_KI = 0, 384, 768, 1280
R_QB, R_KB, R_QC, R_KC = 1344, 1664, 1984, 2304
R_G = 2624
FM_ROWS = R_G + 3 * D


class Buf:
    def __init__(self, name, t):
        self.name = name
        self.t = t
        self.last_writer = None
        self.readers = []

    def __getitem__(self, idx):
        return self.t[idx]


class Rec:
    __slots__ = ("eng", "fn", "deps", "marked", "count", "epoch", "is_dma", "dsem", "dval",
                 "prewait")

    def __init__(self, eng, fn, is_dma=False):
        self.eng = eng
        self.fn = fn
        self.deps = []
        self.marked = False
        self.count = None
        self.epoch = None
        self.is_dma = is_dma
        self.dsem = None
        self.dval = None
        self.prewait = None


class Ctx:
    def __init__(self, nc):
        self.nc = nc
        self.stack = contextlib.ExitStack()
        self.recs = []
        self.last = {e: None for e in ENGS}
        self.dma_recs = {e: [] for e in ENGS}
        self.nbuf = 0

    def sbuf(self, name, shape, dtype, stack=None):
        st = stack or self.stack
        self.nbuf += 1
        t = st.enter_context(self.nc.sbuf_tensor(f"{name}_{self.nbuf}", list(shape), dtype))
        return Buf(name, t)

    def psum(self, name, shape, dtype, stack=None):
        st = stack or self.stack
        self.nbuf += 1
        t = st.enter_context(self.nc.psum_tensor(f"{name}_{self.nbuf}", list(shape), dtype))
        return Buf(name, t)

    def dram(self, name, shape, dtype, kind="Internal"):
        t = self.nc.dram_tensor(name, list(shape), dtype, kind=kind)
        return Buf(name, t.ap())

    def _track(self, rec, reads, writes):
        for b in reads:
            w = b.last_writer
            if w is not None and w is not rec:
                if not (w.eng == rec.eng and rec.eng == "pe" and not w.is_dma and not rec.is_dma):
                    rec.deps.append(w)
        for b in writes:
            w = b.last_writer
            if w is not None and w is not rec:
                if w.is_dma or rec.is_dma or w.eng != rec.eng or rec.eng != "pe":
                    rec.deps.append(w)
            for r in b.readers:
                if r is rec:
                    continue
                if r.is_dma or rec.is_dma or r.eng != rec.eng or rec.eng != "pe":
                    rec.deps.append(r)
        for b in reads:
            b.readers.append(rec)
        for b in writes:
            b.last_writer = rec
            b.readers = []

    def op(self, eng, fn, reads=(), writes=()):
        rec = Rec(eng, fn)
        self._track(rec, reads, writes)
        self.recs.append(rec)
        self.last[eng] = rec
        return rec

    def dma(self, eng, fn, reads=(), writes=()):
        rec = Rec(eng, fn, is_dma=True)
        self._track(rec, reads, writes)
        self.recs.append(rec)
        self.dma_recs[eng].append(rec)
        return rec

    def barrier(self):
        pend = [self.last[e] for e in ENGS if self.last[e] is not None]
        dmas = []
        for e in ENGS:
            dmas += self.dma_recs[e][-DMA_K:]
        for e in ENGS:
            rec = Rec(e, None)
            rec.deps = [p for p in pend if p.eng != e] + dmas
            self.recs.append(rec)

    def finalize(self):
        nc = self.nc
        for r in self.recs:
            for d in r.deps:
                if not d.is_dma:
                    d.marked = True
        cnt = {e: 0 for e in ENGS}
        for r in self.recs:
            if r.is_dma or r.fn is None:
                continue
            if r.marked:
                cnt[r.eng] += 1
                r.epoch = (cnt[r.eng] - 1) // EPOCH
                r.count = (cnt[r.eng] - 1) % EPOCH + 1
        self.esem = {}
        for e in ENGS:
            for ep in range((cnt[e] + EPOCH - 1) // EPOCH):
                self.esem[(e, ep)] = self.stack.enter_context(nc.semaphore(f"s_{e}_{ep}"))
        self.dsem = {}
        for e in ENGS:
            if self.dma_recs[e]:
                for k in range(DMA_K):
                    self.dsem[(e, k)] = self.stack.enter_context(nc.semaphore(f"d_{e}_{k}"))
            for j, r in enumerate(self.dma_recs[e]):
                r.dsem = self.dsem[(e, j % DMA_K)]
                r.dval = 16 * (j // DMA_K + 1)
                if j >= DMA_K:
                    r.prewait = (r.dsem, 16 * (j // DMA_K))

    def replay(self):
        nc = self.nc
        by_eng = {e: [] for e in ENGS}
        for r in self.recs:
            by_eng[r.eng].append(r)
        self.nwaits = 0
        self.ninstr = {e: len(by_eng[e]) for e in ENGS}
        with nc.Block() as block:
            deco = {"pe": block.tensor, "act": block.scalar, "dve": block.vector,
                    "pool": block.gpsimd, "sp": block.sync}

            def make(e):
                def body(eng):
                    seen = {}
                    for r in by_eng[e]:
                        waits = []
                        for d in r.deps:
                            if d.is_dma:
                                waits.append((d.dsem, d.dval))
                            else:
                                waits.append((self.esem[(d.eng, d.epoch)], d.count))
                        if r.prewait is not None:
                            waits.append(r.prewait)
                        best = {}
                        for s, v in waits:
                            k = id(s)
                            if seen.get(k, 0) >= v:
                                continue
                            if k not in best or best[k][1] < v:
                                best[k] = (s, v)
                        for k, (s, v) in best.items():
                            eng.wait_ge(s, v)
                            seen[k] = v
                            self.nwaits += 1
                        if r.fn is None:
                            continue
                        ins = r.fn(eng)
                        if r.is_dma:
                            ins.then_inc(r.dsem, 16)
                        elif r.marked:
                            ins.then_inc(self.esem[(r.eng, r.epoch)], 1)
                return body

            for e in ENGS:
                if by_eng[e]:
                    deco[e](make(e))


def make_plan():
    roped = []
    for h in range(NHA):
        roped.append(("qa", h, O_QA + h * HD, R_QA + h * HD))
    for h in range(NHA):
        roped.append(("ka", h, O_KA + h * HD, R_KA + h * HD))
    for h in range(IDXH):
        roped.append(("qi", h, O_QI + h * HD, R_QI + h * HD))
    roped.append(("ki", 0, O_KI, R_KI))
    tiles = []

    def newtile(kind):
        t = dict(cols=np.full(128, -1, np.int64), scale=np.ones(128, np.float32), kind=kind, segs=[])
        tiles.append(t)
        return t

    for g0 in range(0, len(roped), 8):
        grp = roped[g0:g0 + 8]
        tR = newtile("ropeR")
        tS = newtile("ropeS")
        for j, (nm, h, cb, rb) in enumerate(grp):
            sc = 0.125 if nm == "qa" else 1.0
            for i in range(16):
                tR["cols"][16 * j + i] = cb + i
                tS["cols"][16 * j + i] = cb + ((i + 8) % 16)
                tR["scale"][16 * j + i] = sc
                tS["scale"][16 * j + i] = sc
            tR["segs"].append((16 * j, 16, rb))
    npass = len(roped) * 48
    ptiles = [newtile("plain") for _ in range((npass + 127) // 128)]
    for hi, (nm, h, cb, rb) in enumerate(roped):
        sc = 0.125 if nm == "qa" else 1.0
        for d in range(48):
            rg = hi * 48 + d
            t = ptiles[rg // 128]
            t["cols"][rg % 128] = cb + 16 + d
            t["scale"][rg % 128] = sc
        r0 = hi * 48
        r1 = r0 + 48
        while r0 < r1:
            ti = r0 // 128
            n = min(r1, (ti + 1) * 128) - r0
            ptiles[ti]["segs"].append((r0 % 128, n, rb + 16 + (r0 - hi * 48)))
            r0 += n
    last = ptiles[-1]
    assert npass % 128 <= 112
    for h in range(NHC):
        last["cols"][112 + h] = O_FC + h
    last["kind"] = "plain_fc"
    plain = []
    for h in range(NHB):
        plain.append((O_QKVB + h * HD, R_QB + h * HD, 0.125))
    for h in range(NHB):
        plain.append((O_QKVB + WB_ + h * HD, R_KB + h * HD, 1.0))
    for h in range(NHC):
        plain.append((O_QKVC + h * HD, R_QC + h * HD, 0.125))
    for h in range(NHC):
        plain.append((O_QKVC + WC + h * HD, R_KC + h * HD, 1.0))
    for i in range(0, len(plain), 2):
        t = newtile("plain")
        for j, (cb, rb, sc) in enumerate(plain[i:i + 2]):
            t["cols"][64 * j:64 * j + 64] = np.arange(cb, cb + 64)
            t["scale"][64 * j:64 * j + 64] = sc
            t["segs"].append((64 * j, 64, rb))
    for j in range(24):
        t = newtile("gate")
        t["cols"][:] = np.arange(O_GATE + j * 128, O_GATE + (j + 1) * 128)
        t["segs"].append((0, 128, R_G + j * 128))
    tm_cols = np.concatenate([
        np.arange(O_VA, O_VA + WA),
        np.arange(O_QKVB + 2 * WB_, O_QKVB + 3 * WB_),
        np.arange(O_QKVC + 2 * WC, O_QKVC + 3 * WC),
        np.arange(O_WI, O_WI + IDXH)])
    return dict(tiles=tiles, tm_cols=tm_cols)


PLAN = make_plan()
NFM = len(PLAN["tiles"])
NTM = len(PLAN["tm_cols"])


def host_layer_inputs(l, w_in, b_in, ada_w, ada_b, norm1_g, norm2_g, rel_bias, w_branch, w_out,
                      w_ffn_in, w_ffn_out):
    out = {}
    W = np.asarray(w_in[l], np.float32)
    B = np.asarray(b_in[l], np.float32)
    wfm = np.zeros((D, NFM * 128), np.float32)
    bfm = np.zeros((128, NFM), np.float32)
    for j, t in enumerate(PLAN["tiles"]):
        m = t["cols"] >= 0
        wfm[:, j * 128:(j + 1) * 128][:, m] = W[:, t["cols"][m]]
        bfm[m, j] = B[t["cols"][m]]
    out["wfm"] = wfm
    out["bfm"] = bfm
    out["wtm"] = np.ascontiguousarray(W[:, PLAN["tm_cols"]])
    out["btm"] = np.ascontiguousarray(np.broadcast_to(B[PLAN["tm_cols"]][None, :], (128, NTM)))
    out["adaw"] = np.asarray(ada_w[l], np.float32)
    ab = np.asarray(ada_b[l], np.float32)
    out["adab_col"] = np.ascontiguousarray(ab.reshape(48, 128).T)
    grow = np.concatenate([ab[2 * D:3 * D], ab[5 * D:6 * D]])
    out["adab_grow"] = np.ascontiguousarray(np.broadcast_to(grow[None, :], (128, 2 * D)))
    out["n1g"] = np.ascontiguousarray(np.asarray(norm1_g[l], np.float32).reshape(8, 128).T)
    out["n2g"] = np.ascontiguousarray(np.asarray(norm2_g[l], np.float32).reshape(8, 128).T)
    rb = np.asarray(rel_bias[l], np.float32)
    q = np.arange(512)[None, :]
    bt = np.zeros((NHB, 8, 128, 512), np.float32)
    for r in range(8):
        k = (-512 + 128 * r + np.arange(128))[:, None]
        dist = np.clip(q - k, -128, 128) + 128
        bt[:, r] = rb[:, dist]
    out["bt"] = bt
    out["wbr"] = np.asarray(w_branch[l], np.float32)
    out["wout"] = np.asarray(w_out[l], np.float32)
    out["wfi"] = np.asarray(w_ffn_in[l], np.float32)
    out["wfo"] = np.asarray(w_ffn_out[l], np.float32)
    return out


def host_consts():
    c = {}
    c["ident"] = np.eye(128, dtype=np.float32)
    c["bigi"] = (BIG * np.eye(128)).astype(np.float32)
    c["negones"] = -np.ones((128, 128), np.float32)
    c["ones"] = np.ones((128, 128), np.float32)
    sc = np.zeros((128, NFM), np.float32)
    for j, t in enumerate(PLAN["tiles"]):
        sc[:, j] = t["scale"]
    c["sctab"] = sc
    i = np.arange(128) % 16
    inv = (500000.0 ** (-(np.arange(0, 16, 2, dtype=np.float32)) / 16.0)).astype(np.float32)
    c["ropec"] = np.stack([inv[i % 8], np.where(i < 8, -1.0, 1.0).astype(np.float32)], 1).astype(np.float32)
    q = np.arange(512)[None, :]
    mb = np.zeros((8, 128, 512), np.float32)
    for r in range(8):
        k = (-512 + 128 * r + np.arange(128))[:, None]
        qc = q // 64
        kc = np.floor_divide(k, 64)
        valid = (kc >= qc - 8) & (kc <= qc)
        mb[r] = np.where(valid, 0.0, -BIG)
    c["maskb"] = mb
    cb = np.zeros((4, 128, 512), np.float32)
    for j in range(4):
        k = (128 * j + np.arange(128))[:, None]
        cb[j] = np.where(k <= q, 0.0, -BIG)
    c["maskc"] = cb
    oh = np.zeros((NHC, NHC, 128), np.float32)
    for h in range(NHC):
        oh[h, h, :] = 1.0
    c["onehot"] = np.ascontiguousarray(oh.transpose(1, 0, 2))
    c["pw"] = np.ascontiguousarray(np.broadcast_to((2.0 ** -np.arange(NITER + 1))[None, :], (128, NITER + 1))).astype(np.float32)
    return c


class Builder:
    def __init__(self, S, debug=False, stages=99):
        self.S = S
        self.debug = debug
        self.stages = stages
        self.nc = bass.Bass("TRN2", target_bir_lowering=False)
        self.c = Ctx(self.nc)
        self.inputs = {}
        self.outputs = {}
        self.qrr = 0

    def inp(self, name, shape, dtype=F32):
        b = self.c.dram(name, shape, dtype, kind="ExternalInput")
        self.inputs[name] = b
        return b

    def outp(self, name, shape, dtype=F32):
        b = self.c.dram(name, shape, dtype, kind="ExternalOutput")
        self.outputs[name] = b
        return b

    def scratch(self, name, shape, dtype):
        if self.debug:
            return self.outp(name, shape, dtype)
        return self.c.dram(name, shape, dtype, kind="Internal")

    def ld(self, out_ap, in_ap, reads, writes, q=None):
        if q is None:
            q = "sp"
        return self.c.dma(q, lambda e: e.dma_start(out=out_ap, in_=in_ap), reads=reads, writes=writes)

    def ldcast(self, out_ap, in_ap, reads, writes):
        return self.c.dma("pool", lambda e: e.dma_start(out=out_ap, in_=in_ap), reads=reads, writes=writes)

    def build(self):
        S = self.S
        c = self.c
        NT = S // 128
        NG = S // 512
        x_in = self.inp("x", [S, D])
        ccol = self.inp("ccol", [128, 8])
        pos = self.inp("pos", [128, S], I32)
        fing = self.inp("fing", [128, D])
        K = {}
        for nm, shp in [("ident", [128, 128]), ("bigi", [128, 128]), ("negones", [128, 128]),
                        ("ones", [128, 128]), ("sctab", [128, NFM]), ("ropec", [128, 2]),
                        ("maskb", [8, 128, 512]), ("maskc", [4, 128, 512]), ("onehot", [NHC, NHC, 128]),
                        ("pw", [128, NITER + 1])]:
            K[nm] = self.inp(nm, shp)
        L = []
        for l in range(DEPTH):
            d = {}
            for nm, shp in [("wfm", [D, NFM * 128]), ("bfm", [128, NFM]), ("wtm", [D, NTM]),
                            ("btm", [128, NTM]), ("adaw", [D, 6 * D]), ("adab_col", [128, 48]),
                            ("adab_grow", [128, 2 * D]), ("n1g", [128, 8]), ("n2g", [128, 8]),
                            ("bt", [NHB, 8, 128, 512]), ("wbr", [D, D]), ("wout", [D, D]),
                            ("wfi", [D, 2 * FFN]), ("wfo", [FFN, D])]:
                d[nm] = self.inp(f"{nm}{l}", shp)
            L.append(d)
        out = self.outp("out", [S, D])
        self.fm = self.scratch("fm", [FM_ROWS, S], BF16)
        self.vO = self.scratch("vO", [S, 16 * 65], BF16)
        self.fcT = self.scratch("fcT", [NHC, S], F32)
        self.FT = self.scratch("FT", [NHC, S], F32)
        self.ctab = self.scratch("ctab", [128, S], F32)
        self.stab = self.scratch("stab", [128, S], F32)
        self.mbd = self.scratch("mbd", [NT, 128, S], BF16)
        self.oT = self.scratch("oT", [D, S], BF16)
        self.xa = self.scratch("xa", [S, D], F32)
        self.xb = self.scratch("xb", [S, D], F32)
        self.wtokd = self.scratch("wtokd", [S, IDXH], F32)
        self.K = K
        self.ident = c.sbuf("ident", [128, 128], F32)
        self.identb = c.sbuf("identb", [128, 128], BF16)
        self.bigi = c.sbuf("bigi", [128, 128], BF16)
        self.negones = c.sbuf("negones", [128, 128], BF16)
        self.onesf = c.sbuf("onesf", [128, 128], F32)
        self.sctab = c.sbuf("sctab", [128, NFM], F32)
        self.ropec = c.sbuf("ropec", [128, 2], F32)
        self.ld(self.ident[:], K["ident"][:], [K["ident"]], [self.ident])
        self.ld(self.onesf[:], K["ones"][:], [K["ones"]], [self.onesf])
        self.ld(self.sctab[:], K["sctab"][:], [K["sctab"]], [self.sctab])
        self.ld(self.ropec[:], K["ropec"][:], [K["ropec"]], [self.ropec])
        self.ldcast(self.identb[:], K["ident"][:], [K["ident"]], [self.identb])
        self.ldcast(self.bigi[:], K["bigi"][:], [K["bigi"]], [self.bigi])
        self.ldcast(self.negones[:], K["negones"][:], [K["negones"]], [self.negones])
        self.modc = c.sbuf("modc", [128, 48], F32)
        self.g1c = c.sbuf("g1c", [128, 8], F32)
        self.g2c = c.sbuf("g2c", [128, 8], F32)
        self.bfe = c.sbuf("bfe", [128, NFM], F32)
        self.grow = c.sbuf("grow", [128, 2 * D], F32)
        self.cond2 = c.sbuf("cond2", [128, 8, 2], F32)
        self.condrep = c.sbuf("condrep", [128, 8, 128], F32)
        self.negF = c.sbuf("negF", [128, NT, NHC], F32)
        self.fgbc = c.sbuf("fgbc", [128, NHC, max(NG, 2)], F32)

        self.phase_setup(pos, ccol)
        xcur = x_in
        for l in range(DEPTH):
            if self.stages < 1:
                break
            self.phase_mod(L[l])
            self.phase_inproj(L[l], xcur)
            if self.stages < 2:
                break
            self.phase_F()
            self.phase_A1()
            if self.stages < 3:
                break
            self.phase_A2()
            self.phase_B(L[l])
            self.phase_C()
            if self.stages < 4:
                break
            self.phase_merge(L[l], xcur, self.xa)
            self.phase_ffn(L[l], self.xa, self.xb)
            xcur = self.xb
            if self.stages < 5:
                break
        if self.stages >= 5:
            self.phase_final(xcur, fing, out)
        else:
            pass
        c.barrier()
        c.finalize()
        c.replay()
        return self.nc

    def phase_setup(self, pos, ccol):
        c = self.c
        S = self.S
        with contextlib.ExitStack() as st:
            ct = c.sbuf("ct", [128, 8], F32, st)
            self.ld(ct[:], ccol[:], [ccol], [ct])
            cs = c.sbuf("cs", [128, 8], F32, st)
            c.op("act", lambda e: e.activation(out=cs[:], in_=ct[:], func=AF.Silu), [ct], [cs])
            c.op("dve", lambda e: e.tensor_copy(out=self.cond2[:, :, 0], in_=cs[:]), [cs], [self.cond2])
            c.op("dve", lambda e: e.tensor_copy(out=self.cond2[:, :, 1], in_=cs[:]), [cs], [self.cond2])
            for k in range(8):
                c.op("dve", lambda e, k=k: e.tensor_scalar(out=self.condrep[:, k, :], in0=self.onesf[:],
                                                           scalar1=cs[:, k:k + 1], scalar2=None, op0=ALU.mult),
                     [self.onesf, cs], [self.condrep])
            NB = 2
            pi_ = [c.sbuf("pi", [128, 512], I32, st) for _ in range(NB)]
            ang = [c.sbuf("ang", [128, 512], F32, st) for _ in range(NB)]
            t1 = [c.sbuf("t1", [128, 512], F32, st) for _ in range(NB)]
            t2 = [c.sbuf("t2", [128, 512], F32, st) for _ in range(NB)]
            res = [[c.sbuf("res", [128, 512], F32, st) for _ in range(2)] for _ in range(NB)]
            for g in range(S // 512):
                b = g % NB
                sl = slice(g * 512, (g + 1) * 512)
                self.ld(pi_[b][:], pos[:, sl], [pos], [pi_[b]])
                c.op("dve", lambda e, b=b: e.tensor_copy(out=ang[b][:], in_=pi_[b][:]), [pi_[b]], [ang[b]])
                c.op("dve", lambda e, b=b: e.tensor_scalar(out=ang[b][:], in0=ang[b][:], scalar1=self.ropec[:, 0:1],
                                                           scalar2=None, op0=ALU.mult), [ang[b], self.ropec], [ang[b]])
                for which in range(2):
                    shift = (np.pi / 2) if which == 0 else 0.0
                    if which == 0:
                        c.op("dve", lambda e, b=b: e.tensor_scalar(
                            out=t1[b][:], in0=ang[b][:], scalar1=float(1.0 / TWO_PI), scalar2=0.25,
                            op0=ALU.mult, op1=ALU.add), [ang[b]], [t1[b]])
                        c.op("dve", lambda e, b=b: e.tensor_scalar(
                            out=t1[b][:], in0=t1[b][:], scalar1=float(MAGIC), scalar2=None,
                            op0=ALU.add), [t1[b]], [t1[b]])
                    else:
                        c.op("dve", lambda e, b=b: e.tensor_scalar(
                            out=t1[b][:], in0=ang[b][:], scalar1=float(1.0 / TWO_PI), scalar2=float(MAGIC),
                            op0=ALU.mult, op1=ALU.add), [ang[b]], [t1[b]])
                    c.op("dve", lambda e, b=b: e.tensor_scalar(out=t1[b][:], in0=t1[b][:], scalar1=float(-MAGIC),
                                                               scalar2=None, op0=ALU.add), [t1[b]], [t1[b]])
                    c.op("dve", lambda e, b=b: e.scalar_tensor_tensor(out=t2[b][:], in0=t1[b][:], scalar=float(-CW1),
                                                                      in1=ang[b][:], op0=ALU.mult, op1=ALU.add),
                         [t1[b], ang[b]], [t2[b]])
                    c.op("dve", lambda e, b=b: e.scalar_tensor_tensor(out=t2[b][:], in0=t1[b][:], scalar=float(-CW2),
                                                                      in1=t2[b][:], op0=ALU.mult, op1=ALU.add),
                         [t1[b], t2[b]], [t2[b]])
                    c.op("dve", lambda e, b=b, shift=shift: e.tensor_scalar(
                        out=t2[b][:], in0=t2[b][:], scalar1=float(shift), scalar2=float(3.1415925),
                        op0=ALU.add, op1=ALU.min), [t2[b]], [t2[b]])
                    c.op("dve", lambda e, b=b: e.tensor_scalar(out=t2[b][:], in0=t2[b][:], scalar1=float(-3.1415925),
                                                               scalar2=None, op0=ALU.max), [t2[b]], [t2[b]])
                    if which == 0:
                        c.op("act", lambda e, b=b: e.activation(out=res[b][0][:], in_=t2[b][:], func=AF.Sin),
                             [t2[b]], [res[b][0]])
                        self.ld(self.ctab[:, sl], res[b][0][:], [res[b][0]], [self.ctab])
                    else:
                        c.op("act", lambda e, b=b: e.activation(out=res[b][1][:], in_=t2[b][:], func=AF.Sin,
                                                                scale=self.ropec[:, 1:2]),
                             [t2[b], self.ropec], [res[b][1]])
                        self.ld(self.stab[:, sl], res[b][1][:], [res[b][1]], [self.stab])
        c.barrier()

    def phase_mod(self, Ld):
        c = self.c
        with contextlib.ExitStack() as st:
            aw = [c.sbuf("aw", [128, 8, 512], F32, st) for _ in range(2)]
            pcol = c.psum("pcol", [128, 512], F32, st)
            prow = [c.psum("prow", [128, 512], F32, st) for _ in range(2)]
            abc = c.sbuf("abc", [128, 48], F32, st)
            abg = c.sbuf("abg", [128, 2 * D], F32, st)
            n1 = c.sbuf("n1", [128, 8], F32, st)
            n2 = c.sbuf("n2", [128, 8], F32, st)
            bfm = c.sbuf("bfm", [128, NFM], F32, st)
            self.ld(abc[:], Ld["adab_col"][:], [Ld["adab_col"]], [abc])
            self.ld(abg[:], Ld["adab_grow"][:], [Ld["adab_grow"]], [abg])
            self.ld(n1[:], Ld["n1g"][:], [Ld["n1g"]], [n1])
            self.ld(n2[:], Ld["n2g"][:], [Ld["n2g"]], [n2])
            self.ld(bfm[:], Ld["bfm"][:], [Ld["bfm"]], [bfm])
            c.op("dve", lambda e: e.tensor_tensor(out=self.bfe[:], in0=bfm[:], in1=self.sctab[:], op=ALU.mult),
                 [bfm, self.sctab], [self.bfe])
            adaw = Ld["adaw"]
            for j in range(12):
                b = j % 2
                src = adaw[:, j * 512:(j + 1) * 512].rearrange("(k p) n -> p k n", p=128)
                self.ld(aw[b][:], src, [adaw], [aw[b]], q=("sp" if j % 2 == 0 else "act"))
                for jj in range(4):
                    col = j * 4 + jj
                    for k in range(8):
                        c.op("pe", lambda e, b=b, jj=jj, k=k, col=col: e.matmul(
                            pcol[:, 2 * col:2 * col + 2], lhsT=aw[b][:, k, jj * 128:(jj + 1) * 128],
                            rhs=self.cond2[:, k, :], start=(k == 0), stop=(k == 7)),
                            [aw[b], self.cond2], [pcol])
                gi = {4: 0, 5: 1, 10: 2, 11: 3}.get(j)
                if gi is not None:
                    pr = prow[gi % 2]
                    for k in range(8):
                        c.op("pe", lambda e, b=b, k=k, pr=pr: e.matmul(
                            pr[:, :], lhsT=self.condrep[:, k, :], rhs=aw[b][:, k, :], start=(k == 0), stop=(k == 7)),
                            [aw[b], self.condrep], [pr])
                    c.op("dve", lambda e, pr=pr, gi=gi: e.tensor_tensor(
                        out=self.grow[:, gi * 512:(gi + 1) * 512], in0=pr[:, :], in1=abg[:, gi * 512:(gi + 1) * 512],
                        op=ALU.add), [pr, abg], [self.grow])
            pv = pcol[:, 0:96].rearrange("p (c t) -> p c t", t=2)[:, :, 0]
            c.op("dve", lambda e: e.tensor_tensor(out=self.modc[:], in0=pv, in1=abc[:], op=ALU.add),
                 [pcol, abc], [self.modc])
            c.op("dve", lambda e: e.scalar_tensor_tensor(out=self.g1c[:], in0=self.modc[:, 8:16], scalar=1.0,
                                                         in1=n1[:], op0=ALU.add, op1=ALU.mult),
                 [self.modc, n1], [self.g1c])
            c.op("dve", lambda e: e.scalar_tensor_tensor(out=self.g2c[:], in0=self.modc[:, 32:40], scalar=1.0,
                                                         in1=n2[:], op0=ALU.add, op1=ALU.mult),
                 [self.modc, n2], [self.g2c])
        c.barrier()

    def norm_transpose(self, st, xsrc, t0, hT, hslot, gcol, bcol0, xt, sq, ss, xs, tp):
        c = self.c
        self.ld(xt[:], xsrc[t0:t0 + 128, :], [xsrc], [xt])
        c.op("act", lambda e: e.activation(out=sq[:], in_=xt[:], func=AF.Square, accum_out=ss[:, 0:1]),
             [xt], [sq, ss])
        c.op("act", lambda e: e.activation(out=ss[:, 1:2], in_=ss[:, 0:1], func=AF.Sqrt, scale=float(1.0 / D),
                                           bias=self.epsc[:, 0:1]), [ss, self.epsc], [ss])
        c.op("dve", lambda e: e.reciprocal(out=ss[:, 2:3], in_=ss[:, 1:2]), [ss], [ss])
        c.op("dve", lambda e: e.tensor_scalar(out=xs[:], in0=xt[:], scalar1=ss[:, 2:3], scalar2=None, op0=ALU.mult),
             [xt, ss], [xs])
        for k in range(8):
            c.op("pe", lambda e, k=k: e.transpose(out=tp[k // 4][:, (k % 4) * 128:(k % 4 + 1) * 128],
                                                  in_=xs[:, k * 128:(k + 1) * 128], identity=self.ident[:]),
                 [xs, self.ident], [tp[k // 4]])
        for k in range(8):
            src = tp[k // 4][:, (k % 4) * 128:(k % 4 + 1) * 128]
            dst = hT[:, k, hslot * 128:(hslot + 1) * 128]
            if k % 2 == 0:
                c.op("act", lambda e, src=src, dst=dst, k=k: e.activation(
                    out=dst, in_=src, func=AF.Identity, scale=gcol[:, k:k + 1],
                    bias=self.modc[:, bcol0 + k:bcol0 + k + 1]), [tp[k // 4], gcol, self.modc], [hT])
            else:
                c.op("dve", lambda e, src=src, dst=dst, k=k: e.tensor_scalar(
                    out=dst, in0=src, scalar1=gcol[:, k:k + 1], scalar2=self.modc[:, bcol0 + k:bcol0 + k + 1],
                    op0=ALU.mult, op1=ALU.add), [tp[k // 4], gcol, self.modc], [hT])

    def phase_inproj(self, Ld, xsrc):
        c = self.c
        S = self.S
        NG = S // 512
        with contextlib.ExitStack() as st:
            self.epsc = c.sbuf("epsc", [128, 1], F32, st)
            c.op("dve", lambda e: e.memset(self.epsc[:], EPS), [], [self.epsc])
            wfm = c.sbuf("wfm", [128, 8, NFM * 128], BF16, st)
            wtm = c.sbuf("wtm", [128, 8, NTM], BF16, st)
            btm = c.sbuf("btm", [128, NTM], F32, st)
            self.ld(btm[:], Ld["btm"][:], [Ld["btm"]], [btm])
            for k in range(8):
                for j0 in range(0, NFM * 128, 2048):
                    j1 = min(NFM * 128, j0 + 2048)
                    self.ldcast(wfm[:, k, j0:j1], Ld["wfm"][k * 128:(k + 1) * 128, j0:j1], [Ld["wfm"]], [wfm])
                self.ldcast(wtm[:, k, :], Ld["wtm"][k * 128:(k + 1) * 128, :], [Ld["wtm"]], [wtm])
            hT = [c.sbuf("hT", [128, 8, 512], BF16, st) for _ in range(2)]
            xt = [c.sbuf("xt", [128, D], F32, st) for _ in range(2)]
            sq = c.sbuf("sq", [128, D], BF16, st)
            xs = [c.sbuf("xs", [128, D], F32, st) for _ in range(2)]
            ss = [c.sbuf("ss", [128, 4], F32, st) for _ in range(2)]
            tp = [[c.psum("tp", [128, 512], F32, st) for _ in range(2)] for _ in range(1)]
            pm = [c.psum("pm", [128, 512], F32, st) for _ in range(4)]
            ptm = [c.psum("ptm", [128, 512], F32, st) for _ in range(2)]
            NE = 10
            ev = [c.sbuf("ev", [128, 512], BF16, st) for _ in range(NE)]
            evf = [c.sbuf("evf", [128, 512], F32, st) for _ in range(2)]
            rR = [c.sbuf("rR", [128, 512], F32, st) for _ in range(2)]
            rS = [c.sbuf("rS", [128, 512], F32, st) for _ in range(2)]
            ctb = [c.sbuf("ctb", [128, 512], F32, st) for _ in range(2)]
            stb = [c.sbuf("stb", [128, 512], F32, st) for _ in range(2)]
            vo = [c.sbuf("vo", [128, 16, 65], BF16, st) for _ in range(2)]
            wt = [c.sbuf("wt", [128, IDXH], F32, st) for _ in range(2)]
            for b in range(2):
                c.op("pool", lambda e, b=b: e.memset(vo[b][:], 1.0), [], [vo[b]])
            tiles = PLAN["tiles"]
            iev = 0
            ipm = 0
            def prep(g):
                sl_ = slice(g * 512, (g + 1) * 512)
                self.ld(ctb[g % 2][:], self.ctab[:, sl_], [self.ctab], [ctb[g % 2]], q="sp")
                self.ld(stb[g % 2][:], self.stab[:, sl_], [self.stab], [stb[g % 2]], q="sp")
                for i in range(4):
                    tt = g * 4 + i
                    self.norm_transpose(st, xsrc, tt * 128, hT[g % 2], i, self.g1c, 0, xt[tt % 2], sq, ss[tt % 2],
                                        xs[tt % 2], tp[0])

            prep(0)
            for g in range(NG):
                hb = hT[g % 2]
                sl = slice(g * 512, (g + 1) * 512)
                for i in range(4):
                    tt = g * 4 + i
                    vb_ = vo[tt % 2]
                    for ci, (c0, c1, h0, nh) in enumerate([(0, 384, 0, 6), (384, 704, 6, 5), (704, 1024, 11, 5),
                                                            (1024, 1032, 0, 0)]):
                        pt = ptm[ci % 2]
                        n = c1 - c0
                        for k in range(8):
                            c.op("pe", lambda e, k=k, pt=pt, n=n, c0=c0, c1=c1, i=i, hb=hb: e.matmul(
                                pt[:, 0:n], lhsT=hb[:, k, i * 128:(i + 1) * 128], rhs=wtm[:, k, c0:c1],
                                start=(k == 0), stop=(k == 7)), [hb, wtm], [pt])
                        if nh > 0:
                            c.op("dve", lambda e, pt=pt, n=n, c0=c0, c1=c1, h0=h0, nh=nh, vb_=vb_: e.tensor_tensor(
                                out=vb_[:, h0:h0 + nh, 0:64], in0=pt[:, 0:n].rearrange("p (h d) -> p h d", d=64),
                                in1=btm[:, c0:c1].rearrange("p (h d) -> p h d", d=64), op=ALU.add),
                                [pt, btm], [vb_])
                        else:
                            wb_ = wt[tt % 2]
                            c.op("dve", lambda e, pt=pt, c0=c0, c1=c1, wb_=wb_: e.tensor_tensor(
                                out=wb_[:], in0=pt[:, 0:IDXH], in1=btm[:, c0:c1], op=ALU.add), [pt, btm], [wb_])
                            self.ld(self.wtokd[tt * 128:(tt + 1) * 128, :], wb_[:], [wb_], [self.wtokd])
                    self.ld(self.vO[tt * 128:(tt + 1) * 128, :], vb_[:].rearrange("p h d -> p (h d)"),
                            [vb_], [self.vO], q="pool")
                for j, t in enumerate(tiles):
                    if j == 28 and g + 1 < NG:
                        prep(g + 1)
                    ps = pm[ipm % 4]
                    ipm += 1
                    for k in range(8):
                        c.op("pe", lambda e, k=k, ps=ps, j=j, hb=hb: e.matmul(
                            ps[:, :], lhsT=wfm[:, k, j * 128:(j + 1) * 128], rhs=hb[:, k, :],
                            start=(k == 0), stop=(k == 7)), [wfm, hb], [ps])
                    kind = t["kind"]
                    if kind in ("ropeR", "ropeS"):
                        dst = (rR if kind == "ropeR" else rS)[(j // 2) % 2]
                        c.op("act", lambda e, ps=ps, dst=dst, j=j: e.activation(
                            out=dst[:], in_=ps[:, :], func=AF.Identity, scale=self.sctab[:, j:j + 1],
                            bias=self.bfe[:, j:j + 1]), [ps, self.sctab, self.bfe], [dst])
                        if kind == "ropeS":
                            a = rR[(j // 2) % 2]
                            b2 = rS[(j // 2) % 2]
                            o = ev[iev % NE]
                            iev += 1
                            c.op("dve", lambda e, a=a, g=g: e.tensor_tensor(out=a[:], in0=a[:], in1=ctb[g % 2][:],
                                                                            op=ALU.mult), [a, ctb[g % 2]], [a])
                            c.op("pool", lambda e, b2=b2, g=g: e.tensor_tensor(out=b2[:], in0=b2[:], in1=stb[g % 2][:],
                                                                              op=ALU.mult), [b2, stb[g % 2]], [b2])
                            c.op("dve", lambda e, a=a, b2=b2, o=o: e.tensor_tensor(out=o[:], in0=a[:], in1=b2[:],
                                                                                   op=ALU.add), [a, b2], [o])
                            for (r0, n, fr) in tiles[j - 1]["segs"]:
                                self.ld(self.fm[fr:fr + n, sl], o[r0:r0 + n, :], [o], [self.fm],
                                        q=("sp" if (r0 // 16) % 2 == 0 else "pool"))
                    else:
                        o = ev[iev % NE]
                        iev += 1
                        func = AF.Sigmoid if kind == "gate" else AF.Identity
                        c.op("act", lambda e, ps=ps, o=o, j=j, func=func: e.activation(
                            out=o[:], in_=ps[:, :], func=func, scale=self.sctab[:, j:j + 1],
                            bias=self.bfe[:, j:j + 1]), [ps, self.sctab, self.bfe], [o])
                        if kind == "plain_fc":
                            of = evf[g % 2]
                            c.op("dve", lambda e, ps=ps, of=of, j=j: e.tensor_scalar(
                                out=of[:], in0=ps[:, :], scalar1=self.bfe[:, j:j + 1], scalar2=None, op0=ALU.add),
                                [ps, self.bfe], [of])
                            self.ld(self.fcT[:, sl], of[112:112 + NHC, :], [of], [self.fcT])
                        for si, (r0, n, fr) in enumerate(t["segs"]):
                            self.ld(self.fm[fr:fr + n, sl], o[r0:r0 + n, :], [o], [self.fm],
                                    q=("sp" if si % 2 == 0 else "pool"))
        c.barrier()

    def phase_F(self):
        c = self.c
        S = self.S
        NT = S // 128
        NG = S // 512
        with contextlib.ExitStack() as st:
            fc = c.sbuf("fc", [NHC, S], F32, st)
            e1 = c.sbuf("e1", [NHC, S], F32, st)
            on = c.sbuf("on", [NHC, S], F32, st)
            G = c.sbuf("G", [NHC, S], F32, st)
            oh = c.sbuf("oh", [NHC, NHC, 128], F32, st)
            gs = c.sbuf("gs", [NHC, NG], F32, st)
            onec = c.sbuf("onec", [NHC, 1], F32, st)
            psT = c.psum("psT", [128, 512], F32, st)
            psb = c.psum("psb", [128, 512], F32, st)
            self.ld(fc[:], self.fcT[:], [self.fcT], [fc])
            self.ld(oh[:], self.K["onehot"][:], [self.K["onehot"]], [oh])
            c.op("pool", lambda e: e.memset(on[:], 1.0), [], [on])
            c.op("dve", lambda e: e.memset(onec[:], 1.0), [], [onec])
            c.op("act", lambda e: e.activation(out=e1[:], in_=fc[:], func=AF.Exp, scale=-1.0), [fc], [e1])
            c.op("act", lambda e: e.activation(out=e1[:], in_=e1[:], func=AF.Ln, bias=onec[:, 0:1]), [e1, onec], [e1])
            c.op("dve", lambda e: e.tensor_tensor_scan(out=G[:], data0=on[:], data1=e1[:], initial=0.0,
                                                       op0=ALU.mult, op1=ALU.add), [on, e1], [G])
            self.ld(self.FT[:], G[:], [G], [self.FT])
            for tt in range(NT):
                c.op("pe", lambda e, tt=tt: e.transpose(out=psT[:, tt * NHC:(tt + 1) * NHC],
                                                        in_=G[:, tt * 128:(tt + 1) * 128],
                                                        identity=self.ident[0:NHC, 0:NHC]), [G, self.ident], [psT])
            c.op("dve", lambda e: e.tensor_copy(out=self.negF[:].rearrange("p n h -> p (n h)"),
                                                in_=psT[:, 0:NT * NHC]), [psT], [self.negF])
            gview = G[:].rearrange("h (g t) -> h g t", t=512)[:, :, 511]
            c.op("dve", lambda e: e.tensor_copy(out=gs[:], in_=gview), [G], [gs])
            for h in range(NHC):
                c.op("pe", lambda e, h=h: e.matmul(psb[:, h * NG:(h + 1) * NG], lhsT=oh[:, h, :], rhs=gs[:],
                                                   start=True, stop=True), [oh, gs], [psb])
            c.op("dve", lambda e: e.tensor_copy(out=self.fgbc[:, :, 0:NG],
                                                in_=psb[:, 0:NHC * NG].rearrange("p (h g) -> p h g", g=NG)),
                 [psb], [self.fgbc])
        c.barrier()

    def phase_A1(self):
        c = self.c
        S = self.S
        NT = S // 128
        FA = 0.0
        with contextlib.ExitStack() as st:
            kiT = c.sbuf("kiT", [64, S], BF16, st)
            self.ld(kiT[:], self.fm[R_KI:R_KI + 64, :], [self.fm], [kiT])
            pw = c.sbuf("pw", [128, NITER + 1], F32, st)
            self.ld(pw[:], self.K["pw"][:], [self.K["pw"]], [pw])
            qib = [c.sbuf("qib", [64, IDXH, 128], BF16, st) for _ in range(2)]
            wtb = [c.sbuf("wtb", [128, IDXH], F32, st) for _ in range(2)]
            Dm = [c.sbuf("Dm", [128, IDXH, 128], BF16, st) for _ in range(2)]
            sc = [c.sbuf("sc", [128, S], F32, st) for _ in range(2)]
            junk = c.sbuf("junk", [128, S], BF16, st)
            junkA = c.sbuf("junkA", [128, S], BF16, st)
            mbq = [c.sbuf("mbq", [128, S], BF16, st) for _ in range(2)]
            Rb = [c.sbuf("Rb", [128, 512], BF16, st) for _ in range(4)]
            sm = [c.sbuf("sm", [128, 8], F32, st) for _ in range(2)]
            sa = [c.sbuf("sa", [128, 2], F32, st) for _ in range(2)]
            WT = [c.sbuf("WT", [128, NITER + 1], F32, st) for _ in range(2)]
            psz = [c.psum("psz", [128, 512], F32, st) for _ in range(4)]
            pss = [c.psum("pss", [128, 512], F32, st) for _ in range(2)]
            cnt = dict(iss=0, iz=0)
            wz = c.sbuf("wz", [128, 512], BF16, st)
            c.op("pool", lambda e: e.memset(wz[:], 0.0), [], [wz])

            def indexer(qb):
                p = qb % 2
                if qb % 2 == 0:
                    self.warm(psz[cnt["iz"] % 4], wz, 12)
                t0 = qb * 128
                nk = (qb + 1) * 128
                src = self.fm[R_QI:R_QI + IDXH * 64, t0:t0 + 128].rearrange("(h d) t -> d h t", d=64)
                self.ld(qib[p][:], src, [self.fm], [qib[p]])
                self.ld(wtb[p][:], self.wtokd[t0:t0 + 128, :], [self.wtokd], [wtb[p]])
                for h in range(IDXH):
                    c.op("pool", lambda e, h=h, p=p: e.tensor_scalar(
                        out=Dm[p][:, h, :], in0=self.identb[:], scalar1=wtb[p][:, h:h + 1], scalar2=None,
                        op0=ALU.mult), [self.identb, wtb[p]], [Dm[p]])
                scb = sc[p]
                for ks in range((nk + 511) // 512):
                    n = min(512, nk - ks * 512)
                    k0 = ks * 512
                    pacc = pss[cnt["iss"] % 2]
                    cnt["iss"] += 1

                    def zmm(h, n=n, k0=k0, p=p):
                        pz = psz[cnt["iz"] % 4]
                        rb = Rb[cnt["iz"] % 4]
                        cnt["iz"] += 1
                        c.op("pe", lambda e: e.matmul(pz[:, 0:n], lhsT=qib[p][:, h, :], rhs=kiT[:, k0:k0 + n],
                                                      start=True, stop=True), [qib[p], kiT], [pz])
                        c.op("act", lambda e: e.activation(out=rb[:, 0:n], in_=pz[:, 0:n], func=AF.Relu), [pz], [rb])
                        return rb

                    def wsm(h, rb, n=n, p=p, pacc=pacc):
                        c.op("pe", lambda e: e.matmul(pacc[:, 0:n], lhsT=Dm[p][:, h, :], rhs=rb[:, 0:n],
                                                      start=(h == 0), stop=(h == IDXH - 1)), [Dm[p], rb], [pacc])

                    rbs = {}
                    for h in range(IDXH + 2):
                        if h < IDXH:
                            rbs[h] = zmm(h)
                        if h >= 2:
                            wsm(h - 2, rbs[h - 2])
                    c.op("act", lambda e, pacc=pacc, scb=scb, n=n, k0=k0: e.copy(out=scb[:, k0:k0 + n],
                                                                                  in_=pacc[:, 0:n]), [pacc], [scb])

            def bisect(qb):
                p = qb % 2
                nk = (qb + 1) * 128
                scb = sc[p]
                s_ = sm[p]
                a_ = sa[p]
                wt_ = WT[p]
                nA = int(nk * FA) // 64 * 64 if nk >= 1024 else 0
                n1 = nk - nA
                c.op("dve", lambda e: e.tensor_reduce(out=s_[:, 5:6], in_=scb[:, 0:nk], axis=AX.X, op=ALU.max),
                     [scb], [s_])
                c.op("dve", lambda e: e.tensor_reduce(out=s_[:, 6:7], in_=scb[:, 0:nk], axis=AX.X, op=ALU.min),
                     [scb], [s_])
                c.op("dve", lambda e: e.memset(scb[0:64, nk - 64:nk], NEG), [], [scb])
                c.op("dve", lambda e: e.tensor_scalar(out=s_[:, 0:1], in0=s_[:, 5:6], scalar1=s_[:, 6:7],
                                                      scalar2=0.5, op0=ALU.add, op1=ALU.mult), [s_], [s_])
                c.op("dve", lambda e: e.tensor_scalar(out=s_[:, 1:2], in0=s_[:, 5:6], scalar1=s_[:, 6:7],
                                                      scalar2=0.5005, op0=ALU.subtract, op1=ALU.mult), [s_], [s_])
                c.op("dve", lambda e: e.tensor_scalar(out=s_[:, 1:2], in0=s_[:, 1:2], scalar1=1e-20,
                                                      scalar2=None, op0=ALU.add), [s_], [s_])
                c.op("dve", lambda e: e.tensor_scalar(out=wt_[:], in0=pw[:], scalar1=s_[:, 1:2],
                                                      scalar2=None, op0=ALU.mult), [pw, s_], [wt_])
                for it in range(NITER):
                    if nA > 0:
                        c.op("dve", lambda e: e.tensor_scalar(out=a_[:, 0:1], in0=s_[:, 0:1], scalar1=-1.0,
                                                              scalar2=None, op0=ALU.mult), [s_], [a_])
                        c.op("act", lambda e: e.activation(out=junkA[:, n1:nk], in_=scb[:, n1:nk], func=AF.Sign,
                                                           bias=a_[:, 0:1], accum_out=a_[:, 1:2]),
                             [scb, a_], [junkA, a_])
                    c.op("dve", lambda e, it=it: e.tensor_tensor(
                        out=s_[:, 2:3], in0=s_[:, 0:1], in1=wt_[:, it + 1:it + 2], op=ALU.subtract), [s_, wt_], [s_])
                    c.op("dve", lambda e: e.tensor_scalar(
                        out=junk[:, 0:n1], in0=scb[:, 0:n1], scalar1=s_[:, 0:1], scalar2=None, op0=ALU.is_ge,
                        op1=ALU.add, accum_out=s_[:, 3:4]), [scb, s_], [junk, s_])
                    if nA > 0:
                        c.op("dve", lambda e: e.scalar_tensor_tensor(
                            out=s_[:, 3:4], in0=a_[:, 1:2], scalar=0.5, in1=s_[:, 3:4], op0=ALU.mult, op1=ALU.add),
                            [a_, s_], [s_])
                    c.op("dve", lambda e: e.tensor_scalar(out=s_[:, 4:5], in0=s_[:, 3:4],
                                                          scalar1=float(TOPK - 0.5 * nA), scalar2=None,
                                                          op0=ALU.is_ge), [s_], [s_])
                    c.op("dve", lambda e, it=it: e.scalar_tensor_tensor(
                        out=s_[:, 0:1], in0=s_[:, 4:5], scalar=wt_[:, it:it + 1], in1=s_[:, 2:3],
                        op0=ALU.mult, op1=ALU.add), [s_, wt_], [s_])
                c.op("dve", lambda e: e.tensor_tensor(
                    out=s_[:, 7:8], in0=s_[:, 0:1], in1=wt_[:, NITER:NITER + 1], op=ALU.subtract), [s_, wt_], [s_])
                mq = mbq[p]
                c.op("dve", lambda e: e.tensor_scalar(
                    out=mq[:, 0:nk], in0=scb[:, 0:nk], scalar1=s_[:, 7:8], scalar2=-1.0, op0=ALU.is_ge, op1=ALU.add),
                    [scb, s_], [mq])
                self.ld(self.mbd[qb, :, 0:nk], mq[:, 0:nk], [mq], [self.mbd], q="sp")

            for qb in range(NT + 1):
                if qb < NT:
                    indexer(qb)
                if qb >= 1:
                    bisect(qb - 1)
        c.barrier()

    def normalize_store(self, oacc, pbc, osb, rec, on_, row0, g):
        c = self.c
        c.op("act", lambda e: e.copy(out=osb[0:65, :], in_=oacc[0:65, :]), [oacc], [osb])
        c.op("dve", lambda e: e.reciprocal(out=rec[64:65, :], in_=osb[64:65, :]), [osb], [rec])
        c.op("pe", lambda e: e.matmul(pbc[0:64, :], lhsT=self.onesf[64:65, 0:64], rhs=rec[64:65, :],
                                      start=True, stop=True), [self.onesf, rec], [pbc])
        c.op("dve", lambda e: e.tensor_tensor(out=on_[0:64, :], in0=osb[0:64, :], in1=pbc[0:64, :], op=ALU.mult),
             [osb, pbc], [on_])
        self.ld(self.oT[row0:row0 + 64, g * 512:(g + 1) * 512], on_[0:64, :], [on_], [self.oT])

    def load_kT(self, st, row0, nheads, name):
        c = self.c
        S = self.S
        kT = c.sbuf(name, [128, (nheads + 1) // 2, S], BF16, st)
        for pr in range((nheads + 1) // 2):
            n = min(128, nheads * 64 - pr * 128)
            self.ld(kT[0:n, pr, :], self.fm[row0 + pr * 128:row0 + pr * 128 + n, :], [self.fm], [kT],
                    q=("sp" if pr % 2 == 0 else "act"))
        return kT

    def load_q(self, qg, row0, nheads, g):
        for pr in range((nheads + 1) // 2):
            n = min(128, nheads * 64 - pr * 128)
            self.ld(qg[0:n, pr, :], self.fm[row0 + pr * 128:row0 + pr * 128 + n, g * 512:(g + 1) * 512],
                    [self.fm], [qg])

    def warm(self, ps, wz, n=20):
        c = self.c
        for _ in range(n):
            c.op("pe", lambda e: e.matmul(ps[:, :], lhsT=self.identb[:], rhs=wz[:], start=True, stop=True),
                 [self.identb, wz], [ps])

    @staticmethod
    def pipeline(blocks, first, rest, la):
        n = len(blocks)
        for idx in range(n + la):
            if idx < n:
                first(idx, blocks[idx])
            if idx - la >= 0:
                rest(idx - la, blocks[idx - la])

    def phase_A2(self):
        c = self.c
        S = self.S
        NT = S // 128
        NG = S // 512
        with contextlib.ExitStack() as st:
            kT = self.load_kT(st, R_KA, NHA, "kTa")
            vO = c.sbuf("vOa", [128, NT, NHA * 65], BF16, st)
            self.ld(vO[:], self.vO[:, 0:NHA * 65].rearrange("(n p) c -> p n c", p=128), [self.vO], [vO], q="act")
            onec = c.sbuf("onecA", [128, 1], F32, st)
            c.op("dve", lambda e: e.memset(onec[:], 1.0), [], [onec])
            wzA = c.sbuf("wzA", [128, 512], BF16, st)
            c.op("pool", lambda e: e.memset(wzA[:], 0.0), [], [wzA])
            qg = [c.sbuf("qga", [128, 3, 512], BF16, st) for _ in range(2)]
            mbc = [c.sbuf("mbc", [128, 4, 512], BF16, st) for _ in range(2)]
            m01 = [c.sbuf("m01", [128, 512], BF16, st) for _ in range(4)]
            pt = [c.sbuf("pta", [128, 512], BF16, st) for _ in range(3)]
            pm = [c.sbuf("ptm", [128, 512], BF16, st) for _ in range(3)]
            osb = [c.sbuf("osb", [128, 512], F32, st) for _ in range(2)]
            rec = [c.sbuf("rec", [128, 512], F32, st) for _ in range(2)]
            on_ = [c.sbuf("on_", [64, 512], BF16, st) for _ in range(2)]
            oacc = [c.psum("oacc", [128, 512], F32, st) for _ in range(3)]
            pst = [c.psum("pst", [128, 512], F32, st) for _ in range(3)]
            pmk = [c.psum("pmk", [128, 512], F32, st) for _ in range(2)]
            blocks = []
            for g in range(NG):
                for hp in range(2):
                    for kta in range(4 * (g + 1)):
                        for hh in range(3):
                            blocks.append((g, hp, kta, hh))
            state = dict(imb=0, imk=0, mb_=None, q_=None)
            ctx = {}
            masks = {}

            def build_mask(g, hp, kta):
                ksup, kt = kta // 4, kta % 4
                if kt == 0:
                    state["mb_"] = mbc[state["imb"] % 2]
                    state["imb"] += 1
                    self.ld(state["mb_"][:], self.mbd[4 * g:4 * g + 4, :, ksup * 512:(ksup + 1) * 512].rearrange(
                        "i p k -> p i k"), [self.mbd], [state["mb_"]], q="sp")
                mb_ = state["mb_"]
                pk = pmk[state["imk"] % 2]
                m_ = m01[state["imk"] % 4]
                state["imk"] += 1
                for i in range(4):
                    vis = kta <= 4 * g + i
                    lhs = mb_[:, i, kt * 128:(kt + 1) * 128] if vis else self.negones[:]
                    rd = [mb_, self.identb] if vis else [self.negones, self.identb]
                    c.op("pe", lambda e, pk=pk, lhs=lhs, i=i: e.matmul(
                        pk[:, i * 128:(i + 1) * 128], lhsT=lhs, rhs=self.identb[:], start=True, stop=True),
                        rd, [pk])
                c.op("dve", lambda e, pk=pk, m_=m_: e.tensor_scalar(out=m_[:], in0=pk[:, :], scalar1=1.0,
                                                                    scalar2=None, op0=ALU.add), [pk], [m_])
                masks[(g, hp, kta)] = m_

            def first(idx, blk):
                g, hp, kta, hh = blk
                h = hp * 3 + hh
                nkt = 4 * (g + 1)
                if kta == 0 and hh == 0 and hp == 0:
                    state["q_"] = qg[g % 2]
                    self.load_q(state["q_"], R_QA, NHA, g)
                if kta == 0 and hh == 0:
                    self.warm(pst[idx % 3], wzA, 16)
                if hh == 0:
                    if kta == 0:
                        build_mask(g, hp, 0)
                    if kta + 1 < nkt:
                        build_mask(g, hp, kta + 1)
                q_ = state["q_"]
                ps = pst[idx % 3]
                pb = 64 * (h % 2)
                k0 = kta * 128
                c.op("pe", lambda e: e.matmul(ps[:, :], lhsT=kT[pb:pb + 64, h // 2, k0:k0 + 128],
                                              rhs=q_[pb:pb + 64, h // 2, :], start=True, stop=True), [kT, q_], [ps])
                ctx[idx] = (ps, masks[(g, hp, kta)])

            def rest(idx, blk):
                g, hp, kta, hh = blk
                h = hp * 3 + hh
                ps, m_ = ctx.pop(idx)
                pt_ = pt[idx % 3]
                pm_ = pm[idx % 3]
                nkt = 4 * (g + 1)
                c.op("act", lambda e: e.activation(out=pt_[:], in_=ps[:, :], func=AF.Exp), [ps], [pt_])
                c.op("dve", lambda e: e.tensor_tensor(out=pm_[:], in0=pt_[:], in1=m_[:], op=ALU.mult), [pt_, m_], [pm_])
                c.op("pe", lambda e: e.matmul(oacc[hh][0:65, :], lhsT=vO[:, kta, h * 65:(h + 1) * 65], rhs=pm_[:],
                                              start=(kta == 0), stop=(kta == nkt - 1)), [vO, pm_], [oacc[hh]])
                if kta == nkt - 1:
                    self.normalize_store(oacc[hh], pmk[hh % 2], osb[h % 2], rec[h % 2], on_[h % 2], h * 64, g)

            self.pipeline(blocks, first, rest, 2)
        c.barrier()

    def phase_B(self, Ld):
        c = self.c
        S = self.S
        NT = S // 128
        NG = S // 512
        with contextlib.ExitStack() as st:
            kT = self.load_kT(st, R_KB, NHB, "kTb")
            vO = c.sbuf("vOb", [128, NT, NHB * 65], BF16, st)
            self.ld(vO[:], self.vO[:, NHA * 65:(NHA + NHB) * 65].rearrange("(n p) c -> p n c", p=128),
                    [self.vO], [vO], q="act")
            btm = c.sbuf("btmB", [128, NHB, 8, 512], BF16, st)
            mk = c.sbuf("mk", [128, 8, 512], F32, st)
            btf = [c.sbuf("btf", [128, 512], F32, st) for _ in range(2)]
            self.ld(mk[:], self.K["maskb"][:].rearrange("r p q -> p r q"), [self.K["maskb"]], [mk])
            for h in range(NHB):
                for r in range(8):
                    bf = btf[(h * 8 + r) % 2]
                    self.ld(bf[:], Ld["bt"][h, r], [Ld["bt"]], [bf])
                    c.op("pool", lambda e, bf=bf, h=h, r=r: e.tensor_tensor(out=btm[:, h, r, :], in0=bf[:],
                                                                          in1=mk[:, r, :], op=ALU.add),
                         [bf, mk], [btm])
            qg = [c.sbuf("qgb", [128, 3, 512], BF16, st) for _ in range(2)]
            pt = [c.sbuf("ptb", [128, 512], BF16, st) for _ in range(3)]
            osb = [c.sbuf("osb", [128, 512], F32, st) for _ in range(2)]
            rec = [c.sbuf("rec", [128, 512], F32, st) for _ in range(2)]
            on_ = [c.sbuf("on_", [64, 512], BF16, st) for _ in range(2)]
            oacc = [c.psum("oacc", [128, 512], F32, st) for _ in range(NHB)]
            pst = [c.psum("pst", [128, 512], F32, st) for _ in range(3)]
            blocks = []
            for g in range(NG):
                rs = [r for r in range(8) if 4 * g - 4 + r >= 0]
                for r in rs:
                    for h in range(NHB):
                        blocks.append((g, r, h, rs[0], rs[-1]))
            state = dict(q_=None)
            ctx = {}

            def first(idx, blk):
                g, r, h, r0, r1 = blk
                if r == r0 and h == 0:
                    state["q_"] = qg[g % 2]
                    self.load_q(state["q_"], R_QB, NHB, g)
                q_ = state["q_"]
                ps = pst[idx % 3]
                pb = 64 * (h % 2)
                k0 = (4 * g - 4 + r) * 128
                c.op("pe", lambda e: e.matmul(ps[:, :], lhsT=kT[pb:pb + 64, h // 2, k0:k0 + 128],
                                              rhs=q_[pb:pb + 64, h // 2, :], start=True, stop=False), [kT, q_], [ps])
                c.op("pe", lambda e: e.matmul(ps[:, :], lhsT=self.identb[:], rhs=btm[:, h, r, :], start=False,
                                              stop=True), [self.identb, btm], [ps])
                ctx[idx] = ps

            def rest(idx, blk):
                g, r, h, r0, r1 = blk
                ps = ctx.pop(idx)
                pt_ = pt[idx % 3]
                kta = 4 * g - 4 + r
                c.op("act", lambda e: e.activation(out=pt_[:], in_=ps[:, :], func=AF.Exp), [ps], [pt_])
                c.op("pe", lambda e: e.matmul(oacc[h][0:65, :], lhsT=vO[:, kta, h * 65:(h + 1) * 65], rhs=pt_[:],
                                              start=(r == r0), stop=(r == r1)), [vO, pt_], [oacc[h]])
                if r == r1:
                    self.normalize_store(oacc[h], pst[(idx + 2) % 3], osb[h % 2], rec[h % 2], on_[h % 2],
                                         WA + h * 64, g)

            self.pipeline(blocks, first, rest, 1)
        c.barrier()

    def phase_C(self):
        c = self.c
        S = self.S
        NT = S // 128
        NG = S // 512
        with contextlib.ExitStack() as st:
            kA = c.sbuf("kA", [65, NHC, S], BF16, st)
            for h in range(NHC):
                self.ld(kA[0:64, h, :], self.fm[R_KC + h * 64:R_KC + (h + 1) * 64, :], [self.fm], [kA],
                        q=("sp" if h % 2 == 0 else "act"))
            c.op("dve", lambda e: e.memset(kA[64:65, :, :], 1.0), [], [kA])
            vO = c.sbuf("vOc", [128, NT, NHC * 65], BF16, st)
            self.ld(vO[:], self.vO[:, (NHA + NHB) * 65:16 * 65].rearrange("(n p) c -> p n c", p=128),
                    [self.vO], [vO], q="act")
            cbm = c.sbuf("cbm", [128, 4, 512], BF16, st)
            self.ldcast(cbm[:], self.K["maskc"][:].rearrange("r p q -> p r q"), [self.K["maskc"]], [cbm])
            qa_ = [c.sbuf("qaug", [65, NHC, 512], BF16, st) for _ in range(2)]
            gq = [c.sbuf("gq", [65, NHC, 512], F32, st) for _ in range(2)]
            kb = [c.sbuf("kb", [128, NHC, NT], F32, st) for _ in range(2)]
            pt = [c.sbuf("ptc", [128, 512], BF16, st) for _ in range(3)]
            osb = [c.sbuf("osb", [128, 512], F32, st) for _ in range(2)]
            rec = [c.sbuf("rec", [128, 512], F32, st) for _ in range(2)]
            on_ = [c.sbuf("on_", [64, 512], BF16, st) for _ in range(2)]
            oacc = [c.psum("oacc", [128, 512], F32, st) for _ in range(NHC)]
            pst = [c.psum("pst", [128, 512], F32, st) for _ in range(3)]
            blocks = []
            for g in range(NG):
                for kta in range(4 * (g + 1)):
                    for h in range(NHC):
                        blocks.append((g, kta, h))
            state = dict(q_=None, kb_=None)
            ctx = {}

            wzC = c.sbuf("wzC", [128, 512], BF16, st)
            c.op("pool", lambda e: e.memset(wzC[:], 0.0), [], [wzC])

            def first(idx, blk):
                g, kta, h = blk
                nkt = 4 * (g + 1)
                if kta == 0 and h == 0:
                    self.warm(pst[idx % 3], wzC, 16)
                    q_ = qa_[g % 2]
                    gq_ = gq[g % 2]
                    kb_ = kb[g % 2]
                    state["q_"] = q_
                    state["kb_"] = kb_
                    for hh in range(NHC):
                        self.ld(q_[0:64, hh, :], self.fm[R_QC + hh * 64:R_QC + (hh + 1) * 64, g * 512:(g + 1) * 512],
                                [self.fm], [q_])
                    self.ld(gq_[64:65, :, :], self.FT[:, g * 512:(g + 1) * 512].rearrange("(o h) t -> o h t", o=1),
                            [self.FT], [gq_], q="sp")
                    for hh in range(NHC):
                        c.op("dve", lambda e, hh=hh: e.tensor_scalar(
                            out=q_[64:65, hh, :], in0=gq_[64:65, hh, :], scalar1=self.fgbc[64:65, hh, g:g + 1],
                            scalar2=-1.0, op0=ALU.subtract, op1=ALU.mult), [gq_, self.fgbc], [q_])
                        c.op("dve", lambda e, hh=hh: e.tensor_scalar(
                            out=kb_[:, hh, 0:nkt], in0=self.negF[:, 0:nkt, hh], scalar1=self.fgbc[:, hh, g:g + 1],
                            scalar2=None, op0=ALU.subtract), [self.negF, self.fgbc], [kb_])
                q_ = state["q_"]
                ps = pst[idx % 3]
                k0 = kta * 128
                diag = kta >= 4 * g
                c.op("pe", lambda e: e.matmul(ps[:, :], lhsT=kA[0:65, h, k0:k0 + 128], rhs=q_[0:65, h, :],
                                              start=True, stop=(not diag)), [kA, q_], [ps])
                if diag:
                    c.op("pe", lambda e: e.matmul(ps[:, :], lhsT=self.identb[:], rhs=cbm[:, kta - 4 * g, :],
                                                  start=False, stop=True), [self.identb, cbm], [ps])
                ctx[idx] = (ps, state["kb_"])

            def rest(idx, blk):
                g, kta, h = blk
                nkt = 4 * (g + 1)
                ps, kb_ = ctx.pop(idx)
                pt_ = pt[idx % 3]
                c.op("act", lambda e: e.activation(out=pt_[:], in_=ps[:, :], func=AF.Exp,
                                                   bias=kb_[:, h, kta:kta + 1]), [ps, kb_], [pt_])
                c.op("pe", lambda e: e.matmul(oacc[h][0:65, :], lhsT=vO[:, kta, h * 65:(h + 1) * 65], rhs=pt_[:],
                                              start=(kta == 0), stop=(kta == nkt - 1)), [vO, pt_], [oacc[h]])
                if kta == nkt - 1:
                    self.normalize_store(oacc[h], pst[(idx + 2) % 3], osb[h % 2], rec[h % 2], on_[h % 2],
                                         WA + WB_ + h * 64, g)

            self.pipeline(blocks, first, rest, 1)
        c.barrier()

    def load_folded(self, st, wsrc, nk, gcol0, name):
        c = self.c
        w = c.sbuf(name, [128, nk, D], BF16, st)
        with contextlib.ExitStack() as st2:
            stg = [c.sbuf("stg", [128, D], F32, st2) for _ in range(2)]
            for k in range(nk):
                sg = stg[k % 2]
                self.ld(sg[:], wsrc[k * 128:(k + 1) * 128, :], [wsrc], [sg], q=("sp" if k % 2 == 0 else "act"))
                eng = "dve" if k % 2 == 0 else "pool"
                c.op(eng, lambda e, sg=sg, k=k: e.tensor_tensor(out=w[:, k, :], in0=sg[:],
                                                               in1=self.grow[:, gcol0:gcol0 + D], op=ALU.mult),
                     [sg, self.grow], [w])
            c.barrier()
        return w

    def phase_merge(self, Ld, xsrc, xdst):
        c = self.c
        S = self.S
        NG = S // 512
        with contextlib.ExitStack() as st:
            wbr = c.sbuf("wbr", [128, 8, D], BF16, st)
            for k in range(8):
                self.ldcast(wbr[:, k, :], Ld["wbr"][k * 128:(k + 1) * 128, :], [Ld["wbr"]], [wbr])
            wout = self.load_folded(st, Ld["wout"], 8, 0, "wout")
            oTs = [c.sbuf("oTs", [128, 8, 512], BF16, st) for _ in range(2)]
            gts = [c.sbuf("gts", [128, 24, 512], BF16, st) for _ in range(2)]
            yT = [c.sbuf("yT", [128, 8, 512], BF16, st) for _ in range(2)]
            ta = [c.sbuf("ta", [128, 512], F32, st) for _ in range(2)]
            tb = [c.sbuf("tb", [128, 512], F32, st) for _ in range(2)]
            tc = [c.sbuf("tc", [128, 512], F32, st) for _ in range(2)]
            xt = [c.sbuf("xtm", [128, D], F32, st) for _ in range(2)]
            xn = [c.sbuf("xnm", [128, D], F32, st) for _ in range(2)]
            pbr = [[c.psum("pbr", [128, 512], F32, st) for _ in range(3)] for _ in range(2)]
            pso = [c.psum("pso", [128, 512], F32, st) for _ in range(2)]
            pieces = [[(0, 0, 128), (1, 0, 128), (2, 0, 128)],
                      [(3, 0, 128), (4, 0, 128), (5, 0, 64)],
                      [(5, 64, 128), (6, 0, 128), (7, 0, 128)]]
            ipo = 0
            for g in range(NG):
                sl = slice(g * 512, (g + 1) * 512)
                o_ = oTs[g % 2]
                g_ = gts[g % 2]
                y_ = yT[g % 2]
                self.ld(o_[:], self.oT[:, sl].rearrange("(k p) t -> p k t", p=128), [self.oT], [o_])
                self.ld(g_[:], self.fm[R_G:R_G + 3 * D, sl].rearrange("(j p) t -> p j t", p=128), [self.fm], [g_],
                        q="pool")
                for dc in range(8):
                    pb = pbr[dc % 2]
                    for br in range(3):
                        for pi, (k, p0, p1) in enumerate(pieces[br]):
                            c.op("pe", lambda e, pb=pb, br=br, k=k, p0=p0, p1=p1, dc=dc, pi=pi, o_=o_: e.matmul(
                                pb[br][:, :], lhsT=wbr[p0:p1, k, dc * 128:(dc + 1) * 128], rhs=o_[p0:p1, k, :],
                                start=(pi == 0), stop=(pi == 2)), [wbr, o_], [pb[br]])
                    a_, b_, c_ = ta[dc % 2], tb[dc % 2], tc[dc % 2]
                    for br, dst in enumerate([a_, b_, c_]):
                        c.op("dve", lambda e, pb=pb, br=br, dst=dst, g_=g_, dc=dc: e.tensor_tensor(
                            out=dst[:], in0=pb[br][:, :], in1=g_[:, br * 8 + dc, :], op=ALU.mult), [pb[br], g_], [dst])
                    c.op("pool", lambda e, a_=a_, b_=b_: e.tensor_tensor(out=a_[:], in0=a_[:], in1=b_[:], op=ALU.add),
                         [a_, b_], [a_])
                    c.op("pool", lambda e, a_=a_, c_=c_, y_=y_, dc=dc: e.tensor_tensor(out=y_[:, dc, :], in0=a_[:],
                                                                                       in1=c_[:], op=ALU.add),
                         [a_, c_], [y_])
                for i in range(4):
                    tt = g * 4 + i
                    x_ = xt[tt % 2]
                    n_ = xn[tt % 2]
                    self.ld(x_[:], xsrc[tt * 128:(tt + 1) * 128, :], [xsrc], [x_])
                    for half in range(2):
                        po = pso[ipo % 2]
                        ipo += 1
                        for k in range(8):
                            c.op("pe", lambda e, po=po, k=k, i=i, half=half, y_=y_: e.matmul(
                                po[:, :], lhsT=y_[:, k, i * 128:(i + 1) * 128], rhs=wout[:, k, half * 512:(half + 1) * 512],
                                start=(k == 0), stop=(k == 7)), [y_, wout], [po])
                        c.op("dve", lambda e, po=po, x_=x_, n_=n_, half=half: e.tensor_tensor(
                            out=n_[:, half * 512:(half + 1) * 512], in0=po[:, :], in1=x_[:, half * 512:(half + 1) * 512],
                            op=ALU.add), [po, x_], [n_])
                    self.ld(xdst[tt * 128:(tt + 1) * 128, :], n_[:], [n_], [xdst], q="pool")
        c.barrier()

    def phase_ffn(self, Ld, xsrc, xdst):
        c = self.c
        S = self.S
        NG = S // 512
        NF = FFN // 128
        with contextlib.ExitStack() as st:
            self.epsc = c.sbuf("epsc", [128, 1], F32, st)
            c.op("dve", lambda e: e.memset(self.epsc[:], EPS), [], [self.epsc])
            wfi = c.sbuf("wfi", [128, 8, 2 * FFN], BF16, st)
            for k in range(8):
                for j0 in range(0, 2 * FFN, 2048):
                    j1 = min(2 * FFN, j0 + 2048)
                    self.ldcast(wfi[:, k, j0:j1], Ld["wfi"][k * 128:(k + 1) * 128, j0:j1], [Ld["wfi"]], [wfi])
            wfo = self.load_folded(st, Ld["wfo"], NF, D, "wfo")
            hT = [c.sbuf("hTf", [128, 8, 512], BF16, st)]
            aT = c.sbuf("aT", [128, NF, 512], BF16, st)
            xt = [c.sbuf("xtf", [128, D], F32, st) for _ in range(2)]
            sq = c.sbuf("sqf", [128, D], BF16, st)
            xs = [c.sbuf("xsf", [128, D], F32, st)]
            ss = [c.sbuf("ssf", [128, 4], F32, st) for _ in range(2)]
            sg = [c.sbuf("sgf", [128, 512], F32, st) for _ in range(2)]
            xn = [c.sbuf("xnf", [128, D], F32, st)]
            tp = [c.psum("tpf", [128, 512], F32, st) for _ in range(2)]
            pg = [c.psum("pg", [128, 512], F32, st) for _ in range(2)]
            pu = [c.psum("pu", [128, 512], F32, st) for _ in range(2)]
            pso = [c.psum("psof", [128, 512], F32, st) for _ in range(2)]

            def prep_tile(g, i):
                tt = g * 4 + i
                self.norm_transpose(st, xsrc, tt * 128, hT[0], i, self.g2c, 24, xt[tt % 2], sq, ss[tt % 2],
                                    xs[0], tp)

            for i in range(4):
                prep_tile(0, i)
            ipo = 0
            for g in range(NG):
                hb = hT[0]
                for f in range(NF):
                    pg_ = pg[f % 2]
                    pu_ = pu[f % 2]
                    for k in range(8):
                        c.op("pe", lambda e, pg_=pg_, k=k, f=f: e.matmul(
                            pg_[:, :], lhsT=wfi[:, k, f * 128:(f + 1) * 128], rhs=hb[:, k, :], start=(k == 0),
                            stop=(k == 7)), [wfi, hb], [pg_])
                    for k in range(8):
                        c.op("pe", lambda e, pu_=pu_, k=k, f=f: e.matmul(
                            pu_[:, :], lhsT=wfi[:, k, FFN + f * 128:FFN + (f + 1) * 128], rhs=hb[:, k, :],
                            start=(k == 0), stop=(k == 7)), [wfi, hb], [pu_])
                    s_ = sg[f % 2]
                    c.op("act", lambda e, pg_=pg_, s_=s_: e.activation(out=s_[:], in_=pg_[:, :], func=AF.Silu),
                         [pg_], [s_])
                    c.op("dve", lambda e, pu_=pu_, s_=s_, f=f: e.tensor_tensor(out=aT[:, f, :], in0=pu_[:, :],
                                                                              in1=s_[:], op=ALU.mult),
                         [pu_, s_], [aT])
                for i in range(4):
                    tt = g * 4 + i
                    n_ = xn[0]
                    x_ = xt[tt % 2]
                    self.ld(x_[:], xsrc[tt * 128:(tt + 1) * 128, :], [xsrc], [x_])
                    for half in range(2):
                        po = pso[ipo % 2]
                        ipo += 1
                        for f in range(NF):
                            c.op("pe", lambda e, po=po, f=f, i=i, half=half: e.matmul(
                                po[:, :], lhsT=aT[:, f, i * 128:(i + 1) * 128],
                                rhs=wfo[:, f, half * 512:(half + 1) * 512], start=(f == 0), stop=(f == NF - 1)),
                                [aT, wfo], [po])
                        c.op("dve", lambda e, po=po, x_=x_, n_=n_, half=half: e.tensor_tensor(
                            out=n_[:, half * 512:(half + 1) * 512], in0=po[:, :], in1=x_[:, half * 512:(half + 1) * 512],
                            op=ALU.add), [po, x_], [n_])
                    self.ld(xdst[tt * 128:(tt + 1) * 128, :], n_[:], [n_], [xdst], q="pool")
                    if g + 1 < NG:
                        prep_tile(g + 1, i)
        c.barrier()

    def phase_final(self, xsrc, fing, out):
        c = self.c
        S = self.S
        NT = S // 128
        with contextlib.ExitStack() as st:
            epsc = c.sbuf("epsc", [128, 1], F32, st)
            c.op("dve", lambda e: e.memset(epsc[:], EPS), [], [epsc])
            fg = c.sbuf("fg", [128, D], F32, st)
            self.ld(fg[:], fing[:], [fing], [fg])
            xt = [c.sbuf("xtz", [128, D], F32, st) for _ in range(3)]
            sq = c.sbuf("sqz", [128, D], BF16, st)
            ss = [c.sbuf("ssz", [128, 4], F32, st) for _ in range(3)]
            yo = [c.sbuf("yo", [128, D], F32, st) for _ in range(3)]
            for tt in range(NT):
                x_ = xt[tt % 3]
                s_ = ss[tt % 3]
                y_ = yo[tt % 3]
                self.ld(x_[:], xsrc[tt * 128:(tt + 1) * 128, :], [xsrc], [x_])
                c.op("act", lambda e, x_=x_, s_=s_: e.activation(out=sq[:], in_=x_[:], func=AF.Square,
                                                                 accum_out=s_[:, 0:1]), [x_], [sq, s_])
                c.op("act", lambda e, s_=s_: e.activation(out=s_[:, 1:2], in_=s_[:, 0:1], func=AF.Sqrt,
                                                          scale=float(1.0 / D), bias=epsc[:, 0:1]), [s_, epsc], [s_])
                c.op("dve", lambda e, s_=s_: e.reciprocal(out=s_[:, 2:3], in_=s_[:, 1:2]), [s_], [s_])
                c.op("dve", lambda e, x_=x_, s_=s_, y_=y_: e.scalar_tensor_tensor(
                    out=y_[:], in0=x_[:], scalar=s_[:, 2:3], in1=fg[:], op0=ALU.mult, op1=ALU.mult),
                    [x_, s_, fg], [y_])
                self.ld(out[tt * 128:(tt + 1) * 128, :], y_[:], [y_], [out], q="pool")
        c.barrier()


def build_program(S, debug=False, stages=99):
    b = Builder(S, debug, stages)
    nc = b.build()
    return nc, b


def prep_inputs(S, x, c, positions, ada_w, ada_b, norm1_g, w_in, b_in, rel_bias, w_branch, w_out,
                norm2_g, w_ffn_in, w_ffn_out, final_g, batches):
    shared = {}
    shared.update(host_consts())
    for l in range(DEPTH):
        d = host_layer_inputs(l, w_in, b_in, ada_w, ada_b, norm1_g, norm2_g, rel_bias, w_branch, w_out,
                              w_ffn_in, w_ffn_out)
        for k, v in d.items():
            shared[f"{k}{l}"] = v
    shared["fing"] = np.ascontiguousarray(np.broadcast_to(np.asarray(final_g, np.float32)[None, :], (128, D)))
    maps = []
    for b in batches:
        m = dict(shared)
        m["x"] = np.ascontiguousarray(np.asarray(x[b], np.float32))
        m["ccol"] = np.ascontiguousarray(np.asarray(c[b], np.float32).reshape(8, 128).T)
        m["pos"] = np.ascontiguousarray(np.broadcast_to(np.asarray(positions[b], np.int32)[None, :], (128, S)))
        maps.append(m)
    return maps


_CACHE = {}


def kernel(**inputs):
    x = np.asarray(inputs["x"])
    B, S, _ = x.shape
    if S not in _CACHE:
        _CACHE[S] = build_program(S)
    nc, b = _CACHE[S]
    batches = [i % B for i in range(8)]
    maps = prep_inputs(S, batches=batches, **inputs)
    res = run_bass_kernel_spmd(nc, maps, core_ids=list(range(8)))
    outs = [np.asarray(res.results[i]["out"]) for i in range(B)]
    return np.stack(outs, 0).astype(np.float32)
```

```python
import contextlib
import numpy as np
import concourse.bass as bass
import concourse.mybir as mybir
from concourse.bass_utils import run_bass_kernel_spmd

F32 = mybir.dt.float32
BF16 = mybir.dt.bfloat16
I32 = mybir.dt.int32
AF = mybir.ActivationFunctionType
ALU = mybir.AluOpType
AX = mybir.AxisListType

ENGS = ("pe", "act", "dve", "pool", "sp")
DMA_K = 24
EPOCH = 12000

D = 1024
DEPTH = 2
HD = 64
NHA, NHB, NHC = 6, 5, 5
WA, WB_, WC = NHA * HD, NHB * HD, NHC * HD
IDXH = 8
TOPK = 256
FFN = 2816
EPS = 1e-6
SPLIT = (WA, WA, WA, IDXH * HD, HD, IDXH, 3 * WB_, 3 * WC, NHC, 3 * D)
INW = sum(SPLIT)
OFF = np.cumsum((0,) + SPLIT)
O_QA, O_KA, O_VA, O_QI, O_KI, O_WI, O_QKVB, O_QKVC, O_FC, O_GATE = [int(v) for v in OFF[:10]]
BIG = 30000.0
NEG = -1.0e30
NITER = 13
TWO_PI = 2.0 * np.pi
CW1 = 6.28125
CW2 = float(TWO_PI - 6.28125)
MAGIC = 12582912.0

R_QA, R_KA, R_QI, R_KI = 0, 384, 768, 1280
R_QB, R_KB, R_QC, R_KC = 1344, 1664, 1984, 2304
R_G = 2624
FM_ROWS = R_G + 3 * D


class Buf:
    def __init__(self, name, t):
        self.name = name
        self.t = t
        self.last_writer = None
        self.readers = []

    def __getitem__(self, idx):
        return self.t[idx]


class Rec:
    __slots__ = ("eng", "fn", "deps", "marked", "count", "epoch", "is_dma", "dsem", "dval",
                 "prewait")

    def __init__(self, eng, fn, is_dma=False):
        self.eng = eng
        self.fn = fn
        self.deps = []
        self.marked = False
        self.count = None
        self.epoch = None
        self.is_dma = is_dma
        self.dsem = None
        self.dval = None
        self.prewait = None


class Ctx:
    def __init__(self, nc):
        self.nc = nc
        self.stack = contextlib.ExitStack()
        self.recs = []
        self.last = {e: None for e in ENGS}
        self.dma_recs = {e: [] for e in ENGS}
        self.nbuf = 0

    def sbuf(self, name, shape, dtype, stack=None):
        st = stack or self.stack
        self.nbuf += 1
        t = st.enter_context(self.nc.sbuf_tensor(f"{name}_{self.nbuf}", list(shape), dtype))
        return Buf(name, t)

    def psum(self, name, shape, dtype, stack=None):
        st = stack or self.stack
        self.nbuf += 1
        t = st.enter_context(self.nc.psum_tensor(f"{name}_{self.nbuf}", list(shape), dtype))
        return Buf(name, t)

    def dram(self, name, shape, dtype, kind="Internal"):
        t = self.nc.dram_tensor(name, list(shape), dtype, kind=kind)
        return Buf(name, t.ap())

    def _track(self, rec, reads, writes):
        for b in reads:
            w = b.last_writer
            if w is not None and w is not rec:
                if not (w.eng == rec.eng and rec.eng == "pe" and not w.is_dma and not rec.is_dma):
                    rec.deps.append(w)
        for b in writes:
            w = b.last_writer
            if w is not None and w is not rec:
                if w.is_dma or rec.is_dma or w.eng != rec.eng or rec.eng != "pe":
                    rec.deps.append(w)
            for r in b.readers:
                if r is rec:
                    continue
                if r.is_dma or rec.is_dma or r.eng != rec.eng or rec.eng != "pe":
                    rec.deps.append(r)
        for b in reads:
            b.readers.append(rec)
        for b in writes:
            b.last_writer = rec
            b.readers = []

    def op(self, eng, fn, reads=(), writes=()):
        rec = Rec(eng, fn)
        self._track(rec, reads, writes)
        self.recs.append(rec)
        self.last[eng] = rec
        return rec

    def dma(self, eng, fn, reads=(), writes=()):
        rec = Rec(eng, fn, is_dma=True)
        self._track(rec, reads, writes)
        self.recs.append(rec)
        self.dma_recs[eng].append(rec)
        return rec

    def barrier(self):
        pend = [self.last[e] for e in ENGS if self.last[e] is not None]
        dmas = []
        for e in ENGS:
            dmas += self.dma_recs[e][-DMA_K:]
        for e in ENGS:
            rec = Rec(e, None)
            rec.deps = [p for p in pend if p.eng != e] + dmas
            self.recs.append(rec)

    def finalize(self):
        nc = self.nc
        for r in self.recs:
            for d in r.deps:
                if not d.is_dma:
                    d.marked = True
        cnt = {e: 0 for e in ENGS}
        for r in self.recs:
            if r.is_dma or r.fn is None:
                continue
            if r.marked:
                cnt[r.eng] += 1
                r.epoch = (cnt[r.eng] - 1) // EPOCH
                r.count = (cnt[r.eng] - 1) % EPOCH + 1
        self.esem = {}
        for e in ENGS:
            for ep in range((cnt[e] + EPOCH - 1) // EPOCH):
                self.esem[(e, ep)] = self.stack.enter_context(nc.semaphore(f"s_{e}_{ep}"))
        self.dsem = {}
        for e in ENGS:
            if self.dma_recs[e]:
                for k in range(DMA_K):
                    self.dsem[(e, k)] = self.stack.enter_context(nc.semaphore(f"d_{e}_{k}"))
            for j, r in enumerate(self.dma_recs[e]):
                r.dsem = self.dsem[(e, j % DMA_K)]
                r.dval = 16 * (j // DMA_K + 1)
                if j >= DMA_K:
                    r.prewait = (r.dsem, 16 * (j // DMA_K))

    def replay(self):
        nc = self.nc
        by_eng = {e: [] for e in ENGS}
        for r in self.recs:
            by_eng[r.eng].append(r)
        self.nwaits = 0
        self.ninstr = {e: len(by_eng[e]) for e in ENGS}
        with nc.Block() as block:
            deco = {"pe": block.tensor, "act": block.scalar, "dve": block.vector,
                    "pool": block.gpsimd, "sp": block.sync}

            def make(e):
                def body(eng):
                    seen = {}
                    for r in by_eng[e]:
                        waits = []
                        for d in r.deps:
                            if d.is_dma:
                                waits.append((d.dsem, d.dval))
                            else:
                                waits.append((self.esem[(d.eng, d.epoch)], d.count))
                        if r.prewait is not None:
                            waits.append(r.prewait)
                        best = {}
                        for s, v in waits:
                            k = id(s)
                            if seen.get(k, 0) >= v:
                                continue
                            if k not in best or best[k][1] < v:
                                best[k] = (s, v)
                        for k, (s, v) in best.items():
                            eng.wait_ge(s, v)
                            seen[k] = v
                            self.nwaits += 1
                        if r.fn is None:
                            continue
                        ins = r.fn(eng)
                        if r.is_dma:
                            ins.then_inc(r.dsem, 16)
                        elif r.marked:
                            ins.then_inc(self.esem[(r.eng, r.epoch)], 1)
                return body

            for e in ENGS:
                if by_eng[e]:
                    deco[e](make(e))


def make_plan():
    roped = []
    for h in range(NHA):
        roped.append(("qa", h, O_QA + h * HD, R_QA + h * HD))
    for h in range(NHA):
        roped.append(("ka", h, O_KA + h * HD, R_KA + h * HD))
    for h in range(IDXH):
        roped.append(("qi", h, O_QI + h * HD, R_QI + h * HD))
    roped.append(("ki", 0, O_KI, R_KI))
    tiles = []

    def newtile(kind):
        t = dict(cols=np.full(128, -1, np.int64), scale=np.ones(128, np.float32), kind=kind, segs=[])
        tiles.append(t)
        return t

    for g0 in range(0, len(roped), 8):
        grp = roped[g0:g0 + 8]
        tR = newtile("ropeR")
        tS = newtile("ropeS")
        for j, (nm, h, cb, rb) in enumerate(grp):
            sc = 0.125 if nm == "qa" else 1.0
            for i in range(16):
                tR["cols"][16 * j + i] = cb + i
                tS["cols"][16 * j + i] = cb + ((i + 8) % 16)
                tR["scale"][16 * j + i] = sc
                tS["scale"][16 * j + i] = sc
            tR["segs"].append((16 * j, 16, rb))
    npass = len(roped) * 48
    ptiles = [newtile("plain") for _ in range((npass + 127) // 128)]
    for hi, (nm, h, cb, rb) in enumerate(roped):
        sc = 0.125 if nm == "qa" else 1.0
        for d in range(48):
            rg = hi * 48 + d
            t = ptiles[rg // 128]
            t["cols"][rg % 128] = cb + 16 + d
            t["scale"][rg % 128] = sc
        r0 = hi * 48
        r1 = r0 + 48
        while r0 < r1:
            ti = r0 // 128
            n = min(r1, (ti + 1) * 128) - r0
            ptiles[ti]["segs"].append((r0 % 128, n, rb + 16 + (r0 - hi * 48)))
            r0 += n
    last = ptiles[-1]
    assert npass % 128 <= 112
    for h in range(NHC):
        last["cols"][112 + h] = O_FC + h
    last["kind"] = "plain_fc"
    plain = []
    for h in range(NHB):
        plain.append((O_QKVB + h * HD, R_QB + h * HD, 0.125))
    for h in range(NHB):
        plain.append((O_QKVB + WB_ + h * HD, R_KB + h * HD, 1.0))
    for h in range(NHC):
        plain.append((O_QKVC + h * HD, R_QC + h * HD, 0.125))
    for h in range(NHC):
        plain.append((O_QKVC + WC + h * HD, R_KC + h * HD, 1.0))
    for i in range(0, len(plain), 2):
        t = newtile("plain")
        for j, (cb, rb, sc) in enumerate(plain[i:i + 2]):
            t["cols"][64 * j:64 * j + 64] = np.arange(cb, cb + 64)
            t["scale"][64 * j:64 * j + 64] = sc
            t["segs"].append((64 * j, 64, rb))
    for j in range(24):
        t = newtile("gate")
        t["cols"][:] = np.arange(O_GATE + j * 128, O_GATE + (j + 1) * 128)
        t["segs"].append((0, 128, R_G + j * 128))
    tm_cols = np.concatenate([
        np.arange(O_VA, O_VA + WA),
        np.arange(O_QKVB + 2 * WB_, O_QKVB + 3 * WB_),
        np.arange(O_QKVC + 2 * WC, O_QKVC + 3 * WC),
        np.arange(O_WI, O_WI + IDXH)])
    return dict(tiles=tiles, tm_cols=tm_cols)


PLAN = make_plan()
NFM = len(PLAN["tiles"])
NTM = len(PLAN["tm_cols"])


def host_layer_inputs(l, w_in, b_in, ada_w, ada_b, norm1_g, norm2_g, rel_bias, w_branch, w_out,
                      w_ffn_in, w_ffn_out):
    out = {}
    W = np.asarray(w_in[l], np.float32)
    B = np.asarray(b_in[l], np.float32)
    wfm = np.zeros((D, NFM * 128), np.float32)
    bfm = np.zeros((128, NFM), np.float32)
    for j, t in enumerate(PLAN["tiles"]):
        m = t["cols"] >= 0
        wfm[:, j * 128:(j + 1) * 128][:, m] = W[:, t["cols"][m]]
        bfm[m, j] = B[t["cols"][m]]
    out["wfm"] = wfm
    out["bfm"] = bfm
    out["wtm"] = np.ascontiguousarray(W[:, PLAN["tm_cols"]])
    out["btm"] = np.ascontiguousarray(np.broadcast_to(B[PLAN["tm_cols"]][None, :], (128, NTM)))
    out["adaw"] = np.asarray(ada_w[l], np.float32)
    ab = np.asarray(ada_b[l], np.float32)
    out["adab_col"] = np.ascontiguousarray(ab.reshape(48, 128).T)
    grow = np.concatenate([ab[2 * D:3 * D], ab[5 * D:6 * D]])
    out["adab_grow"] = np.ascontiguousarray(np.broadcast_to(grow[None, :], (128, 2 * D)))
    out["n1g"] = np.ascontiguousarray(np.asarray(norm1_g[l], np.float32).reshape(8, 128).T)
    out["n2g"] = np.ascontiguousarray(np.asarray(norm2_g[l], np.float32).reshape(8, 128).T)
    rb = np.asarray(rel_bias[l], np.float32)
    q = np.arange(512)[None, :]
    bt = np.zeros((NHB, 8, 128, 512), np.float32)
    for r in range(8):
        k = (-512 + 128 * r + np.arange(128))[:, None]
        dist = np.clip(q - k, -128, 128) + 128
        bt[:, r] = rb[:, dist]
    out["bt"] = bt
    out["wbr"] = np.asarray(w_branch[l], np.float32)
    out["wout"] = np.asarray(w_out[l], np.float32)
    out["wfi"] = np.asarray(w_ffn_in[l], np.float32)
    out["wfo"] = np.asarray(w_ffn_out[l], np.float32)
    return out


def host_consts():
    c = {}
    c["ident"] = np.eye(128, dtype=np.float32)
    c["bigi"] = (BIG * np.eye(128)).astype(np.float32)
    c["negones"] = -np.ones((128, 128), np.float32)
    c["ones"] = np.ones((128, 128), np.float32)
    sc = np.zeros((128, NFM), np.float32)
    for j, t in enumerate(PLAN["tiles"]):
        sc[:, j] = t["scale"]
    c["sctab"] = sc
    i = np.arange(128) % 16
    inv = (500000.0 ** (-(np.arange(0, 16, 2, dtype=np.float32)) / 16.0)).astype(np.float32)
    c["ropec"] = np.stack([inv[i % 8], np.where(i < 8, -1.0, 1.0).astype(np.float32)], 1).astype(np.float32)
    q = np.arange(512)[None, :]
    mb = np.zeros((8, 128, 512), np.float32)
    for r in range(8):
        k = (-512 + 128 * r + np.arange(128))[:, None]
        qc = q // 64
        kc = np.floor_divide(k, 64)
        valid = (kc >= qc - 8) & (kc <= qc)
        mb[r] = np.where(valid, 0.0, -BIG)
    c["maskb"] = mb
    cb = np.zeros((4, 128, 512), np.float32)
    for j in range(4):
        k = (128 * j + np.arange(128))[:, None]
        cb[j] = np.where(k <= q, 0.0, -BIG)
    c["maskc"] = cb
    oh = np.zeros((NHC, NHC, 128), np.float32)
    for h in range(NHC):
        oh[h, h, :] = 1.0
    c["onehot"] = np.ascontiguousarray(oh.transpose(1, 0, 2))
    c["pw"] = np.ascontiguousarray(np.broadcast_to((2.0 ** -np.arange(NITER + 1))[None, :], (128, NITER + 1))).astype(np.float32)
    return c


class Builder:
    def __init__(self, S, debug=False, stages=99):
        self.S = S
        self.debug = debug
        self.stages = stages
        self.nc = bass.Bass("TRN2", target_bir_lowering=False)
        self.c = Ctx(self.nc)
        self.inputs = {}
        self.outputs = {}
        self.qrr = 0

    def inp(self, name, shape, dtype=F32):
        b = self.c.dram(name, shape, dtype, kind="ExternalInput")
        self.inputs[name] = b
        return b

    def outp(self, name, shape, dtype=F32):
        b = self.c.dram(name, shape, dtype, kind="ExternalOutput")
        self.outputs[name] = b
        return b

    def scratch(self, name, shape, dtype):
        if self.debug:
            return self.outp(name, shape, dtype)
        return self.c.dram(name, shape, dtype, kind="Internal")

    def ld(self, out_ap, in_ap, reads, writes, q=None):
        if q is None:
            q = "sp"
        return self.c.dma(q, lambda e: e.dma_start(out=out_ap, in_=in_ap), reads=reads, writes=writes)

    def ldcast(self, out_ap, in_ap, reads, writes):
        return self.c.dma("pool", lambda e: e.dma_start(out=out_ap, in_=in_ap), reads=reads, writes=writes)

    def build(self):
        S = self.S
        c = self.c
        NT = S // 128
        NG = S // 512
        x_in = self.inp("x", [S, D])
        ccol = self.inp("ccol", [128, 8])
        pos = self.inp("pos", [128, S], I32)
        fing = self.inp("fing", [128, D])
        K = {}
        for nm, shp in [("ident", [128, 128]), ("bigi", [128, 128]), ("negones", [128, 128]),
                        ("ones", [128, 128]), ("sctab", [128, NFM]), ("ropec", [128, 2]),
                        ("maskb", [8, 128, 512]), ("maskc", [4, 128, 512]), ("onehot", [NHC, NHC, 128]),
                        ("pw", [128, NITER + 1])]:
            K[nm] = self.inp(nm, shp)
        L = []
        for l in range(DEPTH):
            d = {}
            for nm, shp in [("wfm", [D, NFM * 128]), ("bfm", [128, NFM]), ("wtm", [D, NTM]),
                            ("btm", [128, NTM]), ("adaw", [D, 6 * D]), ("adab_col", [128, 48]),
                            ("adab_grow", [128, 2 * D]), ("n1g", [128, 8]), ("n2g", [128, 8]),
                            ("bt", [NHB, 8, 128, 512]), ("wbr", [D, D]), ("wout", [D, D]),
                            ("wfi", [D, 2 * FFN]), ("wfo", [FFN, D])]:
                d[nm] = self.inp(f"{nm}{l}", shp)
            L.append(d)
        out = self.outp("out", [S, D])
        self.fm = self.scratch("fm", [FM_ROWS, S], BF16)
        self.vO = self.scratch("vO", [S, 16 * 65], BF16)
        self.fcT = self.scratch("fcT", [NHC, S], F32)
        self.FT = self.scratch("FT", [NHC, S], F32)
        self.ctab = self.scratch("ctab", [128, S], F32)
        self.stab = self.scratch("stab", [128, S], F32)
        self.mbd = self.scratch("mbd", [NT, 128, S], BF16)
        self.oT = self.scratch("oT", [D, S], BF16)
        self.xa = self.scratch("xa", [S, D], F32)
        self.xb = self.scratch("xb", [S, D], F32)
        self.wtokd = self.scratch("wtokd", [S, IDXH], F32)
        self.K = K
        self.ident = c.sbuf("ident", [128, 128], F32)
        self.identb = c.sbuf("identb", [128, 128], BF16)
        self.bigi = c.sbuf("bigi", [128, 128], BF16)
        self.negones = c.sbuf("negones", [128, 128], BF16)
        self.onesf = c.sbuf("onesf", [128, 128], F32)
        self.sctab = c.sbuf("sctab", [128, NFM], F32)
        self.ropec = c.sbuf("ropec", [128, 2], F32)
        self.ld(self.ident[:], K["ident"][:], [K["ident"]], [self.ident])
        self.ld(self.onesf[:], K["ones"][:], [K["ones"]], [self.onesf])
        self.ld(self.sctab[:], K["sctab"][:], [K["sctab"]], [self.sctab])
        self.ld(self.ropec[:], K["ropec"][:], [K["ropec"]], [self.ropec])
        self.ldcast(self.identb[:], K["ident"][:], [K["ident"]], [self.identb])
        self.ldcast(self.bigi[:], K["bigi"][:], [K["bigi"]], [self.bigi])
        self.ldcast(self.negones[:], K["negones"][:], [K["negones"]], [self.negones])
        self.modc = c.sbuf("modc", [128, 48], F32)
        self.g1c = c.sbuf("g1c", [128, 8], F32)
        self.g2c = c.sbuf("g2c", [128, 8], F32)
        self.bfe = c.sbuf("bfe", [128, NFM], F32)
        self.grow = c.sbuf("grow", [128, 2 * D], F32)
        self.cond2 = c.sbuf("cond2", [128, 8, 2], F32)
        self.condrep = c.sbuf("condrep", [128, 8, 128], F32)
        self.negF = c.sbuf("negF", [128, NT, NHC], F32)
        self.fgbc = c.sbuf("fgbc", [128, NHC, max(NG, 2)], F32)

        self.phase_setup(pos, ccol)
        xcur = x_in
        for l in range(DEPTH):
            if self.stages < 1:
                break
            self.phase_mod(L[l])
            self.phase_inproj(L[l], xcur)
            if self.stages < 2:
                break
            self.phase_F()
            self.phase_A1()
            if self.stages < 3:
                break
            self.phase_A2()
            self.phase_B(L[l])
            self.phase_C()
            if self.stages < 4:
                break
            self.phase_merge(L[l], xcur, self.xa)
            self.phase_ffn(L[l], self.xa, self.xb)
            xcur = self.xb
            if self.stages < 5:
                break
        if self.stages >= 5:
            self.phase_final(xcur, fing, out)
        else:
            pass
        c.barrier()
        c.finalize()
        c.replay()
        return self.nc

    def phase_setup(self, pos, ccol):
        c = self.c
        S = self.S
        with contextlib.ExitStack() as st:
            ct = c.sbuf("ct", [128, 8], F32, st)
            self.ld(ct[:], ccol[:], [ccol], [ct])
            cs = c.sbuf("cs", [128, 8], F32, st)
            c.op("act", lambda e: e.activation(out=cs[:], in_=ct[:], func=AF.Silu), [ct], [cs])
            c.op("dve", lambda e: e.tensor_copy(out=self.cond2[:, :, 0], in_=cs[:]), [cs], [self.cond2])
            c.op("dve", lambda e: e.tensor_copy(out=self.cond2[:, :, 1], in_=cs[:]), [cs], [self.cond2])
            for k in range(8):
                c.op("dve", lambda e, k=k: e.tensor_scalar(out=self.condrep[:, k, :], in0=self.onesf[:],
                                                           scalar1=cs[:, k:k + 1], scalar2=None, op0=ALU.mult),
                     [self.onesf, cs], [self.condrep])
            NB = 2
            pi_ = [c.sbuf("pi", [128, 512], I32, st) for _ in range(NB)]
            ang = [c.sbuf("ang", [128, 512], F32, st) for _ in range(NB)]
            t1 = [c.sbuf("t1", [128, 512], F32, st) for _ in range(NB)]
            t2 = [c.sbuf("t2", [128, 512], F32, st) for _ in range(NB)]
            res = [[c.sbuf("res", [128, 512], F32, st) for _ in range(2)] for _ in range(NB)]
            for g in range(S // 512):
                b = g % NB
                sl = slice(g * 512, (g + 1) * 512)
                self.ld(pi_[b][:], pos[:, sl], [pos], [pi_[b]])
                c.op("dve", lambda e, b=b: e.tensor_copy(out=ang[b][:], in_=pi_[b][:]), [pi_[b]], [ang[b]])
                c.op("dve", lambda e, b=b: e.tensor_scalar(out=ang[b][:], in0=ang[b][:], scalar1=self.ropec[:, 0:1],
                                                           scalar2=None, op0=ALU.mult), [ang[b], self.ropec], [ang[b]])
                for which in range(2):
                    shift = (np.pi / 2) if which == 0 else 0.0
                    if which == 0:
                        c.op("dve", lambda e, b=b: e.tensor_scalar(
                            out=t1[b][:], in0=ang[b][:], scalar1=float(1.0 / TWO_PI), scalar2=0.25,
                            op0=ALU.mult, op1=ALU.add), [ang[b]], [t1[b]])
                        c.op("dve", lambda e, b=b: e.tensor_scalar(
                            out=t1[b][:], in0=t1[b][:], scalar1=float(MAGIC), scalar2=None,
                            op0=ALU.add), [t1[b]], [t1[b]])
                    else:
                        c.op("dve", lambda e, b=b: e.tensor_scalar(
                            out=t1[b][:], in0=ang[b][:], scalar1=float(1.0 / TWO_PI), scalar2=float(MAGIC),
                            op0=ALU.mult, op1=ALU.add), [ang[b]], [t1[b]])
                    c.op("dve", lambda e, b=b: e.tensor_scalar(out=t1[b][:], in0=t1[b][:], scalar1=float(-MAGIC),
                                                               scalar2=None, op0=ALU.add), [t1[b]], [t1[b]])
                    c.op("dve", lambda e, b=b: e.scalar_tensor_tensor(out=t2[b][:], in0=t1[b][:], scalar=float(-CW1),
                                                                      in1=ang[b][:], op0=ALU.mult, op1=ALU.add),
                         [t1[b], ang[b]], [t2[b]])
                    c.op("dve", lambda e, b=b: e.scalar_tensor_tensor(out=t2[b][:], in0=t1[b][:], scalar=float(-CW2),
                                                                      in1=t2[b][:], op0=ALU.mult, op1=ALU.add),
                         [t1[b], t2[b]], [t2[b]])
                    c.op("dve", lambda e, b=b, shift=shift: e.tensor_scalar(
                        out=t2[b][:], in0=t2[b][:], scalar1=float(shift), scalar2=float(3.1415925),
                        op0=ALU.add, op1=ALU.min), [t2[b]], [t2[b]])
                    c.op("dve", lambda e, b=b: e.tensor_scalar(out=t2[b][:], in0=t2[b][:], scalar1=float(-3.1415925),
                                                               scalar2=None, op0=ALU.max), [t2[b]], [t2[b]])
                    if which == 0:
                        c.op("act", lambda e, b=b: e.activation(out=res[b][0][:], in_=t2[b][:], func=AF.Sin),
                             [t2[b]], [res[b][0]])
                        self.ld(self.ctab[:, sl], res[b][0][:], [res[b][0]], [self.ctab])
                    else:
                        c.op("act", lambda e, b=b: e.activation(out=res[b][1][:], in_=t2[b][:], func=AF.Sin,
                                                                scale=self.ropec[:, 1:2]),
                             [t2[b], self.ropec], [res[b][1]])
                        self.ld(self.stab[:, sl], res[b][1][:], [res[b][1]], [self.stab])
        c.barrier()

    def phase_mod(self, Ld):
        c = self.c
        with contextlib.ExitStack() as st:
            aw = [c.sbuf("aw", [128, 8, 512], F32, st) for _ in range(2)]
            pcol = c.psum("pcol", [128, 512], F32, st)
            prow = [c.psum("prow", [128, 512], F32, st) for _ in range(2)]
            abc = c.sbuf("abc", [128, 48], F32, st)
            abg = c.sbuf("abg", [128, 2 * D], F32, st)
            n1 = c.sbuf("n1", [128, 8], F32, st)
            n2 = c.sbuf("n2", [128, 8], F32, st)
            bfm = c.sbuf("bfm", [128, NFM], F32, st)
            self.ld(abc[:], Ld["adab_col"][:], [Ld["adab_col"]], [abc])
            self.ld(abg[:], Ld["adab_grow"][:], [Ld["adab_grow"]], [abg])
            self.ld(n1[:], Ld["n1g"][:], [Ld["n1g"]], [n1])
            self.ld(n2[:], Ld["n2g"][:], [Ld["n2g"]], [n2])
            self.ld(bfm[:], Ld["bfm"][:], [Ld["bfm"]], [bfm])
            c.op("dve", lambda e: e.tensor_tensor(out=self.bfe[:], in0=bfm[:], in1=self.sctab[:], op=ALU.mult),
                 [bfm, self.sctab], [self.bfe])
            adaw = Ld["adaw"]
            for j in range(12):
                b = j % 2
                src = adaw[:, j * 512:(j + 1) * 512].rearrange("(k p) n -> p k n", p=128)
                self.ld(aw[b][:], src, [adaw], [aw[b]], q=("sp" if j % 2 == 0 else "act"))
                for jj in range(4):
                    col = j * 4 + jj
                    for k in range(8):
                        c.op("pe", lambda e, b=b, jj=jj, k=k, col=col: e.matmul(
                            pcol[:, 2 * col:2 * col + 2], lhsT=aw[b][:, k, jj * 128:(jj + 1) * 128],
                            rhs=self.cond2[:, k, :], start=(k == 0), stop=(k == 7)),
                            [aw[b], self.cond2], [pcol])
                gi = {4: 0, 5: 1, 10: 2, 11: 3}.get(j)
                if gi is not None:
                    pr = prow[gi % 2]
                    for k in range(8):
                        c.op("pe", lambda e, b=b, k=k, pr=pr: e.matmul(
                            pr[:, :], lhsT=self.condrep[:, k, :], rhs=aw[b][:, k, :], start=(k == 0), stop=(k == 7)),
                            [aw[b], self.condrep], [pr])
                    c.op("dve", lambda e, pr=pr, gi=gi: e.tensor_tensor(
                        out=self.grow[:, gi * 512:(gi + 1) * 512], in0=pr[:, :], in1=abg[:, gi * 512:(gi + 1) * 512],
                        op=ALU.add), [pr, abg], [self.grow])
            pv = pcol[:, 0:96].rearrange("p (c t) -> p c t", t=2)[:, :, 0]
            c.op("dve", lambda e: e.tensor_tensor(out=self.modc[:], in0=pv, in1=abc[:], op=ALU.add),
                 [pcol, abc], [self.modc])
            c.op("dve", lambda e: e.scalar_tensor_tensor(out=self.g1c[:], in0=self.modc[:, 8:16], scalar=1.0,
                                                         in1=n1[:], op0=ALU.add, op1=ALU.mult),
                 [self.modc, n1], [self.g1c])
            c.op("dve", lambda e: e.scalar_tensor_tensor(out=self.g2c[:], in0=self.modc[:, 32:40], scalar=1.0,
                                                         in1=n2[:], op0=ALU.add, op1=ALU.mult),
                 [self.modc, n2], [self.g2c])
        c.barrier()

    def norm_transpose(self, st, xsrc, t0, hT, hslot, gcol, bcol0, xt, sq, ss, xs, tp):
        c = self.c
        self.ld(xt[:], xsrc[t0:t0 + 128, :], [xsrc], [xt])
        c.op("act", lambda e: e.activation(out=sq[:], in_=xt[:], func=AF.Square, accum_out=ss[:, 0:1]),
             [xt], [sq, ss])
        c.op("act", lambda e: e.activation(out=ss[:, 1:2], in_=ss[:, 0:1], func=AF.Sqrt, scale=float(1.0 / D),
                                           bias=self.epsc[:, 0:1]), [ss, self.epsc], [ss])
        c.op("dve", lambda e: e.reciprocal(out=ss[:, 2:3], in_=ss[:, 1:2]), [ss], [ss])
        c.op("dve", lambda e: e.tensor_scalar(out=xs[:], in0=xt[:], scalar1=ss[:, 2:3], scalar2=None, op0=ALU.mult),
             [xt, ss], [xs])
        for k in range(8):
            c.op("pe", lambda e, k=k: e.transpose(out=tp[k // 4][:, (k % 4) * 128:(k % 4 + 1) * 128],
                                                  in_=xs[:, k * 128:(k + 1) * 128], identity=self.ident[:]),
                 [xs, self.ident], [tp[k // 4]])
        for k in range(8):
            src = tp[k // 4][:, (k % 4) * 128:(k % 4 + 1) * 128]
            dst = hT[:, k, hslot * 128:(hslot + 1) * 128]
            if k % 2 == 0:
                c.op("act", lambda e, src=src, dst=dst, k=k: e.activation(
                    out=dst, in_=src, func=AF.Identity, scale=gcol[:, k:k + 1],
                    bias=self.modc[:, bcol0 + k:bcol0 + k + 1]), [tp[k // 4], gcol, self.modc], [hT])
            else:
                c.op("dve", lambda e, src=src, dst=dst, k=k: e.tensor_scalar(
                    out=dst, in0=src, scalar1=gcol[:, k:k + 1], scalar2=self.modc[:, bcol0 + k:bcol0 + k + 1],
                    op0=ALU.mult, op1=ALU.add), [tp[k // 4], gcol, self.modc], [hT])

    def phase_inproj(self, Ld, xsrc):
        c = self.c
        S = self.S
        NG = S // 512
        with contextlib.ExitStack() as st:
            self.epsc = c.sbuf("epsc", [128, 1], F32, st)
            c.op("dve", lambda e: e.memset(self.epsc[:], EPS), [], [self.epsc])
            wfm = c.sbuf("wfm", [128, 8, NFM * 128], BF16, st)
            wtm = c.sbuf("wtm", [128, 8, NTM], BF16, st)
            btm = c.sbuf("btm", [128, NTM], F32, st)
            self.ld(btm[:], Ld["btm"][:], [Ld["btm"]], [btm])
            for k in range(8):
                for j0 in range(0, NFM * 128, 2048):
                    j1 = min(NFM * 128, j0 + 2048)
                    self.ldcast(wfm[:, k, j0:j1], Ld["wfm"][k * 128:(k + 1) * 128, j0:j1], [Ld["wfm"]], [wfm])
                self.ldcast(wtm[:, k, :], Ld["wtm"][k * 128:(k + 1) * 128, :], [Ld["wtm"]], [wtm])
            hT = [c.sbuf("hT", [128, 8, 512], BF16, st) for _ in range(2)]
            xt = [c.sbuf("xt", [128, D], F32, st) for _ in range(2)]
            sq = c.sbuf("sq", [128, D], F32, st)
            xs = [c.sbuf("xs", [128, D], F32, st) for _ in range(2)]
            ss = [c.sbuf("ss", [128, 4], F32, st) for _ in range(2)]
            tp = [[c.psum("tp", [128, 512], F32, st) for _ in range(2)] for _ in range(1)]
            pm = [c.psum("pm", [128, 512], F32, st) for _ in range(4)]
            ptm = [c.psum("ptm", [128, 512], F32, st) for _ in range(2)]
            NE = 4
            ev = [c.sbuf("ev", [128, 512], BF16, st) for _ in range(NE)]
            evf = [c.sbuf("evf", [128, 512], F32, st) for _ in range(2)]
            rR = [c.sbuf("rR", [128, 512], F32, st) for _ in range(2)]
            rS = [c.sbuf("rS", [128, 512], F32, st) for _ in range(2)]
            ctb = [c.sbuf("ctb", [128, 512], F32, st) for _ in range(2)]
            stb = [c.sbuf("stb", [128, 512], F32, st) for _ in range(2)]
            vo = [c.sbuf("vo", [128, 16, 65], BF16, st) for _ in range(2)]
            wt = [c.sbuf("wt", [128, IDXH], F32, st) for _ in range(2)]
            for b in range(2):
                c.op("pool", lambda e, b=b: e.memset(vo[b][:], 1.0), [], [vo[b]])
            tiles = PLAN["tiles"]
            iev = 0
            ipm = 0
            def prep(g):
                sl_ = slice(g * 512, (g + 1) * 512)
                self.ld(ctb[g % 2][:], self.ctab[:, sl_], [self.ctab], [ctb[g % 2]], q="act")
                self.ld(stb[g % 2][:], self.stab[:, sl_], [self.stab], [stb[g % 2]], q="act")
                for i in range(4):
                    tt = g * 4 + i
                    self.norm_transpose(st, xsrc, tt * 128, hT[g % 2], i, self.g1c, 0, xt[tt % 2], sq, ss[tt % 2],
                                        xs[tt % 2], tp[0])

            prep(0)
            for g in range(NG):
                hb = hT[g % 2]
                sl = slice(g * 512, (g + 1) * 512)
                for i in range(4):
                    tt = g * 4 + i
                    vb_ = vo[tt % 2]
                    for ci, (c0, c1, h0, nh) in enumerate([(0, 384, 0, 6), (384, 704, 6, 5), (704, 1024, 11, 5),
                                                            (1024, 1032, 0, 0)]):
                        pt = ptm[ci % 2]
                        n = c1 - c0
                        for k in range(8):
                            c.op("pe", lambda e, k=k, pt=pt, n=n, c0=c0, c1=c1, i=i, hb=hb: e.matmul(
                                pt[:, 0:n], lhsT=hb[:, k, i * 128:(i + 1) * 128], rhs=wtm[:, k, c0:c1],
                                start=(k == 0), stop=(k == 7)), [hb, wtm], [pt])
                        if nh > 0:
                            c.op("dve", lambda e, pt=pt, n=n, c0=c0, c1=c1, h0=h0, nh=nh, vb_=vb_: e.tensor_tensor(
                                out=vb_[:, h0:h0 + nh, 0:64], in0=pt[:, 0:n].rearrange("p (h d) -> p h d", d=64),
                                in1=btm[:, c0:c1].rearrange("p (h d) -> p h d", d=64), op=ALU.add),
                                [pt, btm], [vb_])
                        else:
                            wb_ = wt[tt % 2]
                            c.op("dve", lambda e, pt=pt, c0=c0, c1=c1, wb_=wb_: e.tensor_tensor(
                                out=wb_[:], in0=pt[:, 0:IDXH], in1=btm[:, c0:c1], op=ALU.add), [pt, btm], [wb_])
                            self.ld(self.wtokd[tt * 128:(tt + 1) * 128, :], wb_[:], [wb_], [self.wtokd])
                    self.ld(self.vO[tt * 128:(tt + 1) * 128, :], vb_[:].rearrange("p h d -> p (h d)"),
                            [vb_], [self.vO], q="act")
                for j, t in enumerate(tiles):
                    if j == 28 and g + 1 < NG:
                        prep(g + 1)
                    ps = pm[ipm % 4]
                    ipm += 1
                    for k in range(8):
                        c.op("pe", lambda e, k=k, ps=ps, j=j, hb=hb: e.matmul(
                            ps[:, :], lhsT=wfm[:, k, j * 128:(j + 1) * 128], rhs=hb[:, k, :],
                            start=(k == 0), stop=(k == 7)), [wfm, hb], [ps])
                    kind = t["kind"]
                    if kind in ("ropeR", "ropeS"):
                        dst = (rR if kind == "ropeR" else rS)[(j // 2) % 2]
                        c.op("act", lambda e, ps=ps, dst=dst, j=j: e.activation(
                            out=dst[:], in_=ps[:, :], func=AF.Identity, scale=self.sctab[:, j:j + 1],
                            bias=self.bfe[:, j:j + 1]), [ps, self.sctab, self.bfe], [dst])
                        if kind == "ropeS":
                            a = rR[(j // 2) % 2]
                            b2 = rS[(j // 2) % 2]
                            o = ev[iev % NE]
                            iev += 1
                            c.op("dve", lambda e, a=a, g=g: e.tensor_tensor(out=a[:], in0=a[:], in1=ctb[g % 2][:],
                                                                            op=ALU.mult), [a, ctb[g % 2]], [a])
                            c.op("pool", lambda e, b2=b2, g=g: e.tensor_tensor(out=b2[:], in0=b2[:], in1=stb[g % 2][:],
                                                                              op=ALU.mult), [b2, stb[g % 2]], [b2])
                            c.op("dve", lambda e, a=a, b2=b2, o=o: e.tensor_tensor(out=o[:], in0=a[:], in1=b2[:],
                                                                                   op=ALU.add), [a, b2], [o])
                            for (r0, n, fr) in tiles[j - 1]["segs"]:
                                self.ld(self.fm[fr:fr + n, sl], o[r0:r0 + n, :], [o], [self.fm],
                                        q=("sp" if (r0 // 16) % 2 == 0 else "act"))
                    else:
                        o = ev[iev % NE]
                        iev += 1
                        func = AF.Sigmoid if kind == "gate" else AF.Identity
                        c.op("act", lambda e, ps=ps, o=o, j=j, func=func: e.activation(
                            out=o[:], in_=ps[:, :], func=func, scale=self.sctab[:, j:j + 1],
                            bias=self.bfe[:, j:j + 1]), [ps, self.sctab, self.bfe], [o])
                        if kind == "plain_fc":
                            of = evf[g % 2]
                            c.op("dve", lambda e, ps=ps, of=of, j=j: e.tensor_scalar(
                                out=of[:], in0=ps[:, :], scalar1=self.bfe[:, j:j + 1], scalar2=None, op0=ALU.add),
                                [ps, self.bfe], [of])
                            self.ld(self.fcT[:, sl], of[112:112 + NHC, :], [of], [self.fcT])
                        for si, (r0, n, fr) in enumerate(t["segs"]):
                            self.ld(self.fm[fr:fr + n, sl], o[r0:r0 + n, :], [o], [self.fm],
                                    q=("sp" if si % 2 == 0 else "act"))
        c.barrier()

    def phase_F(self):
        c = self.c
        S = self.S
        NT = S // 128
        NG = S // 512
        with contextlib.ExitStack() as st:
            fc = c.sbuf("fc", [NHC, S], F32, st)
            e1 = c.sbuf("e1", [NHC, S], F32, st)
            on = c.sbuf("on", [NHC, S], F32, st)
            G = c.sbuf("G", [NHC, S], F32, st)
            oh = c.sbuf("oh", [NHC, NHC, 128], F32, st)
            gs = c.sbuf("gs", [NHC, NG], F32, st)
            onec = c.sbuf("onec", [NHC, 1], F32, st)
            psT = c.psum("psT", [128, 512], F32, st)
            psb = c.psum("psb", [128, 512], F32, st)
            self.ld(fc[:], self.fcT[:], [self.fcT], [fc])
            self.ld(oh[:], self.K["onehot"][:], [self.K["onehot"]], [oh])
            c.op("pool", lambda e: e.memset(on[:], 1.0), [], [on])
            c.op("dve", lambda e: e.memset(onec[:], 1.0), [], [onec])
            c.op("act", lambda e: e.activation(out=e1[:], in_=fc[:], func=AF.Exp, scale=-1.0), [fc], [e1])
            c.op("act", lambda e: e.activation(out=e1[:], in_=e1[:], func=AF.Ln, bias=onec[:, 0:1]), [e1, onec], [e1])
            c.op("dve", lambda e: e.tensor_tensor_scan(out=G[:], data0=on[:], data1=e1[:], initial=0.0,
                                                       op0=ALU.mult, op1=ALU.add), [on, e1], [G])
            self.ld(self.FT[:], G[:], [G], [self.FT])
            for tt in range(NT):
                c.op("pe", lambda e, tt=tt: e.transpose(out=psT[:, tt * NHC:(tt + 1) * NHC],
                                                        in_=G[:, tt * 128:(tt + 1) * 128],
                                                        identity=self.ident[0:NHC, 0:NHC]), [G, self.ident], [psT])
            c.op("dve", lambda e: e.tensor_copy(out=self.negF[:].rearrange("p n h -> p (n h)"),
                                                in_=psT[:, 0:NT * NHC]), [psT], [self.negF])
            gview = G[:].rearrange("h (g t) -> h g t", t=512)[:, :, 511]
            c.op("dve", lambda e: e.tensor_copy(out=gs[:], in_=gview), [G], [gs])
            for h in range(NHC):
                c.op("pe", lambda e, h=h: e.matmul(psb[:, h * NG:(h + 1) * NG], lhsT=oh[:, h, :], rhs=gs[:],
                                                   start=True, stop=True), [oh, gs], [psb])
            c.op("dve", lambda e: e.tensor_copy(out=self.fgbc[:, :, 0:NG],
                                                in_=psb[:, 0:NHC * NG].rearrange("p (h g) -> p h g", g=NG)),
                 [psb], [self.fgbc])
        c.barrier()

    def phase_A1(self):
        c = self.c
        S = self.S
        NT = S // 128
        FA = 0.0
        with contextlib.ExitStack() as st:
            kiT = c.sbuf("kiT", [64, S], BF16, st)
            self.ld(kiT[:], self.fm[R_KI:R_KI + 64, :], [self.fm], [kiT])
            pw = c.sbuf("pw", [128, NITER + 1], F32, st)
            self.ld(pw[:], self.K["pw"][:], [self.K["pw"]], [pw])
            qib = [c.sbuf("qib", [64, IDXH, 128], BF16, st) for _ in range(2)]
            wtb = [c.sbuf("wtb", [128, IDXH], F32, st) for _ in range(2)]
            Dm = [c.sbuf("Dm", [128, IDXH, 128], BF16, st) for _ in range(2)]
            sc = [c.sbuf("sc", [128, S], F32, st) for _ in range(2)]
            junk = c.sbuf("junk", [128, S], BF16, st)
            junkA = c.sbuf("junkA", [128, S], BF16, st)
            mbq = [c.sbuf("mbq", [128, S], BF16, st) for _ in range(2)]
            Rb = [c.sbuf("Rb", [128, 512], BF16, st) for _ in range(4)]
            sm = [c.sbuf("sm", [128, 8], F32, st) for _ in range(2)]
            sa = [c.sbuf("sa", [128, 2], F32, st) for _ in range(2)]
            WT = [c.sbuf("WT", [128, NITER + 1], F32, st) for _ in range(2)]
            psz = [c.psum("psz", [128, 512], F32, st) for _ in range(4)]
            pss = [c.psum("pss", [128, 512], F32, st) for _ in range(2)]
            cnt = dict(iss=0, iz=0)

            def indexer(qb):
                p = qb % 2
                t0 = qb * 128
                nk = (qb + 1) * 128
                src = self.fm[R_QI:R_QI + IDXH * 64, t0:t0 + 128].rearrange("(h d) t -> d h t", d=64)
                self.ld(qib[p][:], src, [self.fm], [qib[p]])
                self.ld(wtb[p][:], self.wtokd[t0:t0 + 128, :], [self.wtokd], [wtb[p]])
                for h in range(IDXH):
                    c.op("pool", lambda e, h=h, p=p: e.tensor_scalar(
                        out=Dm[p][:, h, :], in0=self.identb[:], scalar1=wtb[p][:, h:h + 1], scalar2=None,
                        op0=ALU.mult), [self.identb, wtb[p]], [Dm[p]])
                scb = sc[p]
                for ks in range((nk + 511) // 512):
                    n = min(512, nk - ks * 512)
                    k0 = ks * 512
                    pacc = pss[cnt["iss"] % 2]
                    cnt["iss"] += 1

                    def zmm(h, n=n, k0=k0, p=p):
                        pz = psz[cnt["iz"] % 4]
                        rb = Rb[cnt["iz"] % 4]
                        cnt["iz"] += 1
                        c.op("pe", lambda e: e.matmul(pz[:, 0:n], lhsT=qib[p][:, h, :], rhs=kiT[:, k0:k0 + n],
                                                      start=True, stop=True), [qib[p], kiT], [pz])
                        c.op("act", lambda e: e.activation(out=rb[:, 0:n], in_=pz[:, 0:n], func=AF.Relu), [pz], [rb])
                        return rb

                    def wsm(h, rb, n=n, p=p, pacc=pacc):
                        c.op("pe", lambda e: e.matmul(pacc[:, 0:n], lhsT=Dm[p][:, h, :], rhs=rb[:, 0:n],
                                                      start=(h == 0), stop=(h == IDXH - 1)), [Dm[p], rb], [pacc])

                    rbs = {}
                    for h in range(IDXH + 2):
                        if h < IDXH:
                            rbs[h] = zmm(h)
                        if h >= 2:
                            wsm(h - 2, rbs[h - 2])
                    c.op("act", lambda e, pacc=pacc, scb=scb, n=n, k0=k0: e.copy(out=scb[:, k0:k0 + n],
                                                                                  in_=pacc[:, 0:n]), [pacc], [scb])

            def bisect(qb):
                p = qb % 2
                nk = (qb + 1) * 128
                scb = sc[p]
                s_ = sm[p]
                a_ = sa[p]
                wt_ = WT[p]
                nA = int(nk * FA) // 64 * 64 if nk >= 1024 else 0
                n1 = nk - nA
                c.op("dve", lambda e: e.tensor_reduce(out=s_[:, 5:6], in_=scb[:, 0:nk], axis=AX.X, op=ALU.max),
                     [scb], [s_])
                c.op("dve", lambda e: e.tensor_reduce(out=s_[:, 6:7], in_=scb[:, 0:nk], axis=AX.X, op=ALU.min),
                     [scb], [s_])
                c.op("dve", lambda e: e.memset(scb[0:64, nk - 64:nk], NEG), [], [scb])
                c.op("dve", lambda e: e.tensor_scalar(out=s_[:, 0:1], in0=s_[:, 5:6], scalar1=s_[:, 6:7],
                                                      scalar2=0.5, op0=ALU.add, op1=ALU.mult), [s_], [s_])
                c.op("dve", lambda e: e.tensor_scalar(out=s_[:, 1:2], in0=s_[:, 5:6], scalar1=s_[:, 6:7],
                                                      scalar2=0.5005, op0=ALU.subtract, op1=ALU.mult), [s_], [s_])
                c.op("dve", lambda e: e.tensor_scalar(out=s_[:, 1:2], in0=s_[:, 1:2], scalar1=1e-20,
                                                      scalar2=None, op0=ALU.add), [s_], [s_])
                c.op("dve", lambda e: e.tensor_scalar(out=wt_[:], in0=pw[:], scalar1=s_[:, 1:2],
                                                      scalar2=None, op0=ALU.mult), [pw, s_], [wt_])
                for it in range(NITER):
                    if nA > 0:
                        c.op("dve", lambda e: e.tensor_scalar(out=a_[:, 0:1], in0=s_[:, 0:1], scalar1=-1.0,
                                                              scalar2=None, op0=ALU.mult), [s_], [a_])
                        c.op("act", lambda e: e.activation(out=junkA[:, n1:nk], in_=scb[:, n1:nk], func=AF.Sign,
                                                           bias=a_[:, 0:1], accum_out=a_[:, 1:2]),
                             [scb, a_], [junkA, a_])
                    c.op("dve", lambda e, it=it: e.tensor_tensor(
                        out=s_[:, 2:3], in0=s_[:, 0:1], in1=wt_[:, it + 1:it + 2], op=ALU.subtract), [s_, wt_], [s_])
                    c.op("dve", lambda e: e.tensor_scalar(
                        out=junk[:, 0:n1], in0=scb[:, 0:n1], scalar1=s_[:, 0:1], scalar2=None, op0=ALU.is_ge,
                        op1=ALU.add, accum_out=s_[:, 3:4]), [scb, s_], [junk, s_])
                    if nA > 0:
                        c.op("dve", lambda e: e.scalar_tensor_tensor(
                            out=s_[:, 3:4], in0=a_[:, 1:2], scalar=0.5, in1=s_[:, 3:4], op0=ALU.mult, op1=ALU.add),
                            [a_, s_], [s_])
                    c.op("dve", lambda e: e.tensor_scalar(out=s_[:, 4:5], in0=s_[:, 3:4],
                                                          scalar1=float(TOPK - 0.5 * nA), scalar2=None,
                                                          op0=ALU.is_ge), [s_], [s_])
                    c.op("dve", lambda e, it=it: e.scalar_tensor_tensor(
                        out=s_[:, 0:1], in0=s_[:, 4:5], scalar=wt_[:, it:it + 1], in1=s_[:, 2:3],
                        op0=ALU.mult, op1=ALU.add), [s_, wt_], [s_])
                c.op("dve", lambda e: e.tensor_tensor(
                    out=s_[:, 7:8], in0=s_[:, 0:1], in1=wt_[:, NITER:NITER + 1], op=ALU.subtract), [s_, wt_], [s_])
                mq = mbq[p]
                c.op("dve", lambda e: e.tensor_scalar(
                    out=mq[:, 0:nk], in0=scb[:, 0:nk], scalar1=s_[:, 7:8], scalar2=-1.0, op0=ALU.is_ge, op1=ALU.add),
                    [scb, s_], [mq])
                self.ld(self.mbd[qb, :, 0:nk], mq[:, 0:nk], [mq], [self.mbd], q="act")

            for qb in range(NT + 1):
                if qb < NT:
                    indexer(qb)
                if qb >= 1:
                    bisect(qb - 1)
        c.barrier()

    def normalize_store(self, oacc, pbc, osb, rec, on_, row0, g):
        c = self.c
        c.op("act", lambda e: e.copy(out=osb[0:65, :], in_=oacc[0:65, :]), [oacc], [osb])
        c.op("dve", lambda e: e.reciprocal(out=rec[64:65, :], in_=osb[64:65, :]), [osb], [rec])
        c.op("pe", lambda e: e.matmul(pbc[0:64, :], lhsT=self.onesf[64:65, 0:64], rhs=rec[64:65, :],
                                      start=True, stop=True), [self.onesf, rec], [pbc])
        c.op("dve", lambda e: e.tensor_tensor(out=on_[0:64, :], in0=osb[0:64, :], in1=pbc[0:64, :], op=ALU.mult),
             [osb, pbc], [on_])
        self.ld(self.oT[row0:row0 + 64, g * 512:(g + 1) * 512], on_[0:64, :], [on_], [self.oT])

    def load_kT(self, st, row0, nheads, name):
        c = self.c
        S = self.S
        kT = c.sbuf(name, [128, (nheads + 1) // 2, S], BF16, st)
        for pr in range((nheads + 1) // 2):
            n = min(128, nheads * 64 - pr * 128)
            self.ld(kT[0:n, pr, :], self.fm[row0 + pr * 128:row0 + pr * 128 + n, :], [self.fm], [kT],
                    q=("sp" if pr % 2 == 0 else "act"))
        return kT

    def load_q(self, qg, row0, nheads, g):
        for pr in range((nheads + 1) // 2):
            n = min(128, nheads * 64 - pr * 128)
            self.ld(qg[0:n, pr, :], self.fm[row0 + pr * 128:row0 + pr * 128 + n, g * 512:(g + 1) * 512],
                    [self.fm], [qg])

    @staticmethod
    def pipeline(blocks, first, rest, la):
        n = len(blocks)
        for idx in range(n + la):
            if idx < n:
                first(idx, blocks[idx])
            if idx - la >= 0:
                rest(idx - la, blocks[idx - la])

    def phase_A2(self):
        c = self.c
        S = self.S
        NT = S // 128
        NG = S // 512
        with contextlib.ExitStack() as st:
            kT = self.load_kT(st, R_KA, NHA, "kTa")
            vO = c.sbuf("vOa", [128, NT, NHA * 65], BF16, st)
            self.ld(vO[:], self.vO[:, 0:NHA * 65].rearrange("(n p) c -> p n c", p=128), [self.vO], [vO], q="act")
            onec = c.sbuf("onecA", [128, 1], F32, st)
            c.op("dve", lambda e: e.memset(onec[:], 1.0), [], [onec])
            qg = [c.sbuf("qga", [128, 3, 512], BF16, st) for _ in range(2)]
            mbc = [c.sbuf("mbc", [128, 4, 512], BF16, st) for _ in range(2)]
            m01 = [c.sbuf("m01", [128, 512], BF16, st) for _ in range(4)]
            pt = [c.sbuf("pta", [128, 512], BF16, st) for _ in range(3)]
            pm = [c.sbuf("ptm", [128, 512], BF16, st) for _ in range(3)]
            osb = [c.sbuf("osb", [128, 512], F32, st) for _ in range(2)]
            rec = [c.sbuf("rec", [128, 512], F32, st) for _ in range(2)]
            on_ = [c.sbuf("on_", [64, 512], BF16, st) for _ in range(2)]
            oacc = [c.psum("oacc", [128, 512], F32, st) for _ in range(3)]
            pst = [c.psum("pst", [128, 512], F32, st) for _ in range(3)]
            pmk = [c.psum("pmk", [128, 512], F32, st) for _ in range(2)]
            blocks = []
            for g in range(NG):
                for hp in range(2):
                    for kta in range(4 * (g + 1)):
                        for hh in range(3):
                            blocks.append((g, hp, kta, hh))
            state = dict(imb=0, imk=0, mb_=None, q_=None)
            ctx = {}
            masks = {}

            def build_mask(g, hp, kta):
                ksup, kt = kta // 4, kta % 4
                if kt == 0:
                    state["mb_"] = mbc[state["imb"] % 2]
                    state["imb"] += 1
                    self.ld(state["mb_"][:], self.mbd[4 * g:4 * g + 4, :, ksup * 512:(ksup + 1) * 512].rearrange(
                        "i p k -> p i k"), [self.mbd], [state["mb_"]], q="act")
                mb_ = state["mb_"]
                pk = pmk[state["imk"] % 2]
                m_ = m01[state["imk"] % 4]
                state["imk"] += 1
                for i in range(4):
                    vis = kta <= 4 * g + i
                    lhs = mb_[:, i, kt * 128:(kt + 1) * 128] if vis else self.negones[:]
                    rd = [mb_, self.identb] if vis else [self.negones, self.identb]
                    c.op("pe", lambda e, pk=pk, lhs=lhs, i=i: e.matmul(
                        pk[:, i * 128:(i + 1) * 128], lhsT=lhs, rhs=self.identb[:], start=True, stop=True),
                        rd, [pk])
                c.op("dve", lambda e, pk=pk, m_=m_: e.tensor_scalar(out=m_[:], in0=pk[:, :], scalar1=1.0,
                                                                    scalar2=None, op0=ALU.add), [pk], [m_])
                masks[(g, hp, kta)] = m_

            def first(idx, blk):
                g, hp, kta, hh = blk
                h = hp * 3 + hh
                nkt = 4 * (g + 1)
                if kta == 0 and hh == 0 and hp == 0:
                    state["q_"] = qg[g % 2]
                    self.load_q(state["q_"], R_QA, NHA, g)
                if hh == 0:
                    if kta == 0:
                        build_mask(g, hp, 0)
                    if kta + 1 < nkt:
                        build_mask(g, hp, kta + 1)
                q_ = state["q_"]
                ps = pst[idx % 3]
                pb = 64 * (h % 2)
                k0 = kta * 128
                c.op("pe", lambda e: e.matmul(ps[:, :], lhsT=kT[pb:pb + 64, h // 2, k0:k0 + 128],
                                              rhs=q_[pb:pb + 64, h // 2, :], start=True, stop=True), [kT, q_], [ps])
                ctx[idx] = (ps, masks[(g, hp, kta)])

            def rest(idx, blk):
                g, hp, kta, hh = blk
                h = hp * 3 + hh
                ps, m_ = ctx.pop(idx)
                pt_ = pt[idx % 3]
                pm_ = pm[idx % 3]
                nkt = 4 * (g + 1)
                c.op("act", lambda e: e.activation(out=pt_[:], in_=ps[:, :], func=AF.Exp), [ps], [pt_])
                c.op("dve", lambda e: e.tensor_tensor(out=pm_[:], in0=pt_[:], in1=m_[:], op=ALU.mult), [pt_, m_], [pm_])
                c.op("pe", lambda e: e.matmul(oacc[hh][0:65, :], lhsT=vO[:, kta, h * 65:(h + 1) * 65], rhs=pm_[:],
                                              start=(kta == 0), stop=(kta == nkt - 1)), [vO, pm_], [oacc[hh]])
                if kta == nkt - 1:
                    self.normalize_store(oacc[hh], pmk[hh % 2], osb[h % 2], rec[h % 2], on_[h % 2], h * 64, g)

            self.pipeline(blocks, first, rest, 2)
        c.barrier()

    def phase_B(self, Ld):
        c = self.c
        S = self.S
        NT = S // 128
        NG = S // 512
        with contextlib.ExitStack() as st:
            kT = self.load_kT(st, R_KB, NHB, "kTb")
            vO = c.sbuf("vOb", [128, NT, NHB * 65], BF16, st)
            self.ld(vO[:], self.vO[:, NHA * 65:(NHA + NHB) * 65].rearrange("(n p) c -> p n c", p=128),
                    [self.vO], [vO], q="act")
            btm = c.sbuf("btmB", [128, NHB, 8, 512], BF16, st)
            mk = c.sbuf("mk", [128, 8, 512], F32, st)
            btf = [c.sbuf("btf", [128, 512], F32, st) for _ in range(2)]
            self.ld(mk[:], self.K["maskb"][:].rearrange("r p q -> p r q"), [self.K["maskb"]], [mk])
            for h in range(NHB):
                for r in range(8):
                    bf = btf[(h * 8 + r) % 2]
                    self.ld(bf[:], Ld["bt"][h, r], [Ld["bt"]], [bf])
                    c.op("pool", lambda e, bf=bf, h=h, r=r: e.tensor_tensor(out=btm[:, h, r, :], in0=bf[:],
                                                                          in1=mk[:, r, :], op=ALU.add),
                         [bf, mk], [btm])
            qg = [c.sbuf("qgb", [128, 3, 512], BF16, st) for _ in range(2)]
            pt = [c.sbuf("ptb", [128, 512], BF16, st) for _ in range(3)]
            osb = [c.sbuf("osb", [128, 512], F32, st) for _ in range(2)]
            rec = [c.sbuf("rec", [128, 512], F32, st) for _ in range(2)]
            on_ = [c.sbuf("on_", [64, 512], BF16, st) for _ in range(2)]
            oacc = [c.psum("oacc", [128, 512], F32, st) for _ in range(NHB)]
            pst = [c.psum("pst", [128, 512], F32, st) for _ in range(3)]
            blocks = []
            for g in range(NG):
                rs = [r for r in range(8) if 4 * g - 4 + r >= 0]
                for r in rs:
                    for h in range(NHB):
                        blocks.append((g, r, h, rs[0], rs[-1]))
            state = dict(q_=None)
            ctx = {}

            def first(idx, blk):
                g, r, h, r0, r1 = blk
                if r == r0 and h == 0:
                    state["q_"] = qg[g % 2]
                    self.load_q(state["q_"], R_QB, NHB, g)
                q_ = state["q_"]
                ps = pst[idx % 3]
                pb = 64 * (h % 2)
                k0 = (4 * g - 4 + r) * 128
                c.op("pe", lambda e: e.matmul(ps[:, :], lhsT=kT[pb:pb + 64, h // 2, k0:k0 + 128],
                                              rhs=q_[pb:pb + 64, h // 2, :], start=True, stop=False), [kT, q_], [ps])
                c.op("pe", lambda e: e.matmul(ps[:, :], lhsT=self.identb[:], rhs=btm[:, h, r, :], start=False,
                                              stop=True), [self.identb, btm], [ps])
                ctx[idx] = ps

            def rest(idx, blk):
                g, r, h, r0, r1 = blk
                ps = ctx.pop(idx)
                pt_ = pt[idx % 3]
                kta = 4 * g - 4 + r
                c.op("act", lambda e: e.activation(out=pt_[:], in_=ps[:, :], func=AF.Exp), [ps], [pt_])
                c.op("pe", lambda e: e.matmul(oacc[h][0:65, :], lhsT=vO[:, kta, h * 65:(h + 1) * 65], rhs=pt_[:],
                                              start=(r == r0), stop=(r == r1)), [vO, pt_], [oacc[h]])
                if r == r1:
                    self.normalize_store(oacc[h], pst[(idx + 2) % 3], osb[h % 2], rec[h % 2], on_[h % 2],
                                         WA + h * 64, g)

            self.pipeline(blocks, first, rest, 1)
        c.barrier()

    def phase_C(self):
        c = self.c
        S = self.S
        NT = S // 128
        NG = S // 512
        with contextlib.ExitStack() as st:
            kA = c.sbuf("kA", [65, NHC, S], BF16, st)
            for h in range(NHC):
                self.ld(kA[0:64, h, :], self.fm[R_KC + h * 64:R_KC + (h + 1) * 64, :], [self.fm], [kA],
                        q=("sp" if h % 2 == 0 else "act"))
            c.op("dve", lambda e: e.memset(kA[64:65, :, :], 1.0), [], [kA])
            vO = c.sbuf("vOc", [128, NT, NHC * 65], BF16, st)
            self.ld(vO[:], self.vO[:, (NHA + NHB) * 65:16 * 65].rearrange("(n p) c -> p n c", p=128),
                    [self.vO], [vO], q="act")
            cbm = c.sbuf("cbm", [128, 4, 512], BF16, st)
            self.ldcast(cbm[:], self.K["maskc"][:].rearrange("r p q -> p r q"), [self.K["maskc"]], [cbm])
            qa_ = [c.sbuf("qaug", [65, NHC, 512], BF16, st) for _ in range(2)]
            gq = [c.sbuf("gq", [65, NHC, 512], F32, st) for _ in range(2)]
            kb = [c.sbuf("kb", [128, NHC, NT], F32, st) for _ in range(2)]
            pt = [c.sbuf("ptc", [128, 512], BF16, st) for _ in range(3)]
            osb = [c.sbuf("osb", [128, 512], F32, st) for _ in range(2)]
            rec = [c.sbuf("rec", [128, 512], F32, st) for _ in range(2)]
            on_ = [c.sbuf("on_", [64, 512], BF16, st) for _ in range(2)]
            oacc = [c.psum("oacc", [128, 512], F32, st) for _ in range(NHC)]
            pst = [c.psum("pst", [128, 512], F32, st) for _ in range(3)]
            blocks = []
            for g in range(NG):
                for kta in range(4 * (g + 1)):
                    for h in range(NHC):
                        blocks.append((g, kta, h))
            state = dict(q_=None, kb_=None)
            ctx = {}

            def first(idx, blk):
                g, kta, h = blk
                nkt = 4 * (g + 1)
                if kta == 0 and h == 0:
                    q_ = qa_[g % 2]
                    gq_ = gq[g % 2]
                    kb_ = kb[g % 2]
                    state["q_"] = q_
                    state["kb_"] = kb_
                    for hh in range(NHC):
                        self.ld(q_[0:64, hh, :], self.fm[R_QC + hh * 64:R_QC + (hh + 1) * 64, g * 512:(g + 1) * 512],
                                [self.fm], [q_])
                    self.ld(gq_[64:65, :, :], self.FT[:, g * 512:(g + 1) * 512].rearrange("(o h) t -> o h t", o=1),
                            [self.FT], [gq_], q="act")
                    for hh in range(NHC):
                        c.op("dve", lambda e, hh=hh: e.tensor_scalar(
                            out=q_[64:65, hh, :], in0=gq_[64:65, hh, :], scalar1=self.fgbc[64:65, hh, g:g + 1],
                            scalar2=-1.0, op0=ALU.subtract, op1=ALU.mult), [gq_, self.fgbc], [q_])
                        c.op("dve", lambda e, hh=hh: e.tensor_scalar(
                            out=kb_[:, hh, 0:nkt], in0=self.negF[:, 0:nkt, hh], scalar1=self.fgbc[:, hh, g:g + 1],
                            scalar2=None, op0=ALU.subtract), [self.negF, self.fgbc], [kb_])
                q_ = state["q_"]
                ps = pst[idx % 3]
                k0 = kta * 128
                diag = kta >= 4 * g
                c.op("pe", lambda e: e.matmul(ps[:, :], lhsT=kA[0:65, h, k0:k0 + 128], rhs=q_[0:65, h, :],
                                              start=True, stop=(not diag)), [kA, q_], [ps])
                if diag:
                    c.op("pe", lambda e: e.matmul(ps[:, :], lhsT=self.identb[:], rhs=cbm[:, kta - 4 * g, :],
                                                  start=False, stop=True), [self.identb, cbm], [ps])
                ctx[idx] = (ps, state["kb_"])

            def rest(idx, blk):
                g, kta, h = blk
                nkt = 4 * (g + 1)
                ps, kb_ = ctx.pop(idx)
                pt_ = pt[idx % 3]
                c.op("act", lambda e: e.activation(out=pt_[:], in_=ps[:, :], func=AF.Exp,
                                                   bias=kb_[:, h, kta:kta + 1]), [ps, kb_], [pt_])
                c.op("pe", lambda e: e.matmul(oacc[h][0:65, :], lhsT=vO[:, kta, h * 65:(h + 1) * 65], rhs=pt_[:],
                                              start=(kta == 0), stop=(kta == nkt - 1)), [vO, pt_], [oacc[h]])
                if kta == nkt - 1:
                    self.normalize_store(oacc[h], pst[(idx + 2) % 3], osb[h % 2], rec[h % 2], on_[h % 2],
                                         WA + WB_ + h * 64, g)

            self.pipeline(blocks, first, rest, 1)
        c.barrier()

    def load_folded(self, st, wsrc, nk, gcol0, name):
        c = self.c
        w = c.sbuf(name, [128, nk, D], BF16, st)
        with contextlib.ExitStack() as st2:
            stg = [c.sbuf("stg", [128, D], F32, st2) for _ in range(2)]
            for k in range(nk):
                sg = stg[k % 2]
                self.ld(sg[:], wsrc[k * 128:(k + 1) * 128, :], [wsrc], [sg], q=("sp" if k % 2 == 0 else "act"))
                eng = "dve" if k % 2 == 0 else "pool"
                c.op(eng, lambda e, sg=sg, k=k: e.tensor_tensor(out=w[:, k, :], in0=sg[:],
                                                               in1=self.grow[:, gcol0:gcol0 + D], op=ALU.mult),
                     [sg, self.grow], [w])
            c.barrier()
        return w

    def phase_merge(self, Ld, xsrc, xdst):
        c = self.c
        S = self.S
        NG = S // 512
        with contextlib.ExitStack() as st:
            wbr = c.sbuf("wbr", [128, 8, D], BF16, st)
            for k in range(8):
                self.ldcast(wbr[:, k, :], Ld["wbr"][k * 128:(k + 1) * 128, :], [Ld["wbr"]], [wbr])
            wout = self.load_folded(st, Ld["wout"], 8, 0, "wout")
            oTs = [c.sbuf("oTs", [128, 8, 512], BF16, st) for _ in range(2)]
            gts = [c.sbuf("gts", [128, 24, 512], BF16, st) for _ in range(2)]
            yT = [c.sbuf("yT", [128, 8, 512], BF16, st) for _ in range(2)]
            ta = [c.sbuf("ta", [128, 512], F32, st) for _ in range(2)]
            tb = [c.sbuf("tb", [128, 512], F32, st) for _ in range(2)]
            tc = [c.sbuf("tc", [128, 512], F32, st) for _ in range(2)]
            xt = [c.sbuf("xtm", [128, D], F32, st) for _ in range(2)]
            xn = [c.sbuf("xnm", [128, D], F32, st) for _ in range(2)]
            pbr = [[c.psum("pbr", [128, 512], F32, st) for _ in range(3)] for _ in range(2)]
            pso = [c.psum("pso", [128, 512], F32, st) for _ in range(2)]
            pieces = [[(0, 0, 128), (1, 0, 128), (2, 0, 128)],
                      [(3, 0, 128), (4, 0, 128), (5, 0, 64)],
                      [(5, 64, 128), (6, 0, 128), (7, 0, 128)]]
            ipo = 0
            for g in range(NG):
                sl = slice(g * 512, (g + 1) * 512)
                o_ = oTs[g % 2]
                g_ = gts[g % 2]
                y_ = yT[g % 2]
                self.ld(o_[:], self.oT[:, sl].rearrange("(k p) t -> p k t", p=128), [self.oT], [o_])
                self.ld(g_[:], self.fm[R_G:R_G + 3 * D, sl].rearrange("(j p) t -> p j t", p=128), [self.fm], [g_],
                        q="act")
                for dc in range(8):
                    pb = pbr[dc % 2]
                    for br in range(3):
                        for pi, (k, p0, p1) in enumerate(pieces[br]):
                            c.op("pe", lambda e, pb=pb, br=br, k=k, p0=p0, p1=p1, dc=dc, pi=pi, o_=o_: e.matmul(
                                pb[br][:, :], lhsT=wbr[p0:p1, k, dc * 128:(dc + 1) * 128], rhs=o_[p0:p1, k, :],
                                start=(pi == 0), stop=(pi == 2)), [wbr, o_], [pb[br]])
                    a_, b_, c_ = ta[dc % 2], tb[dc % 2], tc[dc % 2]
                    for br, dst in enumerate([a_, b_, c_]):
                        c.op("dve", lambda e, pb=pb, br=br, dst=dst, g_=g_, dc=dc: e.tensor_tensor(
                            out=dst[:], in0=pb[br][:, :], in1=g_[:, br * 8 + dc, :], op=ALU.mult), [pb[br], g_], [dst])
                    c.op("pool", lambda e, a_=a_, b_=b_: e.tensor_tensor(out=a_[:], in0=a_[:], in1=b_[:], op=ALU.add),
                         [a_, b_], [a_])
                    c.op("pool", lambda e, a_=a_, c_=c_, y_=y_, dc=dc: e.tensor_tensor(out=y_[:, dc, :], in0=a_[:],
                                                                                       in1=c_[:], op=ALU.add),
                         [a_, c_], [y_])
                for i in range(4):
                    tt = g * 4 + i
                    x_ = xt[tt % 2]
                    n_ = xn[tt % 2]
                    self.ld(x_[:], xsrc[tt * 128:(tt + 1) * 128, :], [xsrc], [x_])
                    for half in range(2):
                        po = pso[ipo % 2]
                        ipo += 1
                        for k in range(8):
                            c.op("pe", lambda e, po=po, k=k, i=i, half=half, y_=y_: e.matmul(
                                po[:, :], lhsT=y_[:, k, i * 128:(i + 1) * 128], rhs=wout[:, k, half * 512:(half + 1) * 512],
                                start=(k == 0), stop=(k == 7)), [y_, wout], [po])
                        c.op("dve", lambda e, po=po, x_=x_, n_=n_, half=half: e.tensor_tensor(
                            out=n_[:, half * 512:(half + 1) * 512], in0=po[:, :], in1=x_[:, half * 512:(half + 1) * 512],
                            op=ALU.add), [po, x_], [n_])
                    self.ld(xdst[tt * 128:(tt + 1) * 128, :], n_[:], [n_], [xdst], q="act")
        c.barrier()

    def phase_ffn(self, Ld, xsrc, xdst):
        c = self.c
        S = self.S
        NG = S // 512
        NF = FFN // 128
        with contextlib.ExitStack() as st:
            self.epsc = c.sbuf("epsc", [128, 1], F32, st)
            c.op("dve", lambda e: e.memset(self.epsc[:], EPS), [], [self.epsc])
            wfi = c.sbuf("wfi", [128, 8, 2 * FFN], BF16, st)
            for k in range(8):
                for j0 in range(0, 2 * FFN, 2048):
                    j1 = min(2 * FFN, j0 + 2048)
                    self.ldcast(wfi[:, k, j0:j1], Ld["wfi"][k * 128:(k + 1) * 128, j0:j1], [Ld["wfi"]], [wfi])
            wfo = self.load_folded(st, Ld["wfo"], NF, D, "wfo")
            hT = [c.sbuf("hTf", [128, 8, 512], BF16, st)]
            aT = c.sbuf("aT", [128, NF, 512], BF16, st)
            xt = [c.sbuf("xtf", [128, D], F32, st) for _ in range(2)]
            sq = c.sbuf("sqf", [128, D], BF16, st)
            xs = [c.sbuf("xsf", [128, D], F32, st)]
            ss = [c.sbuf("ssf", [128, 4], F32, st) for _ in range(2)]
            sg = [c.sbuf("sgf", [128, 512], F32, st) for _ in range(2)]
            xn = [c.sbuf("xnf", [128, D], F32, st)]
            tp = [c.psum("tpf", [128, 512], F32, st) for _ in range(2)]
            pg = [c.psum("pg", [128, 512], F32, st) for _ in range(2)]
            pu = [c.psum("pu", [128, 512], F32, st) for _ in range(2)]
            pso = [c.psum("psof", [128, 512], F32, st) for _ in range(2)]

            def prep_tile(g, i):
                tt = g * 4 + i
                self.norm_transpose(st, xsrc, tt * 128, hT[0], i, self.g2c, 24, xt[tt % 2], sq, ss[tt % 2],
                                    xs[0], tp)

            for i in range(4):
                prep_tile(0, i)
            ipo = 0
            for g in range(NG):
                hb = hT[0]
                for f in range(NF):
                    pg_ = pg[f % 2]
                    pu_ = pu[f % 2]
                    for k in range(8):
                        c.op("pe", lambda e, pg_=pg_, k=k, f=f: e.matmul(
                            pg_[:, :], lhsT=wfi[:, k, f * 128:(f + 1) * 128], rhs=hb[:, k, :], start=(k == 0),
                            stop=(k == 7)), [wfi, hb], [pg_])
                    for k in range(8):
                        c.op("pe", lambda e, pu_=pu_, k=k, f=f: e.matmul(
                            pu_[:, :], lhsT=wfi[:, k, FFN + f * 128:FFN + (f + 1) * 128], rhs=hb[:, k, :],
                            start=(k == 0), stop=(k == 7)), [wfi, hb], [pu_])
                    s_ = sg[f % 2]
                    c.op("act", lambda e, pg_=pg_, s_=s_: e.activation(out=s_[:], in_=pg_[:, :], func=AF.Silu),
                         [pg_], [s_])
                    c.op("dve", lambda e, pu_=pu_, s_=s_, f=f: e.tensor_tensor(out=aT[:, f, :], in0=pu_[:, :],
                                                                              in1=s_[:], op=ALU.mult),
                         [pu_, s_], [aT])
                for i in range(4):
                    tt = g * 4 + i
                    n_ = xn[0]
                    x_ = xt[tt % 2]
                    self.ld(x_[:], xsrc[tt * 128:(tt + 1) * 128, :], [xsrc], [x_])
                    for half in range(2):
                        po = pso[ipo % 2]
                        ipo += 1
                        for f in range(NF):
                            c.op("pe", lambda e, po=po, f=f, i=i, half=half: e.matmul(
                                po[:, :], lhsT=aT[:, f, i * 128:(i + 1) * 128],
                                rhs=wfo[:, f, half * 512:(half + 1) * 512], start=(f == 0), stop=(f == NF - 1)),
                                [aT, wfo], [po])
                        c.op("dve", lambda e, po=po, x_=x_, n_=n_, half=half: e.tensor_tensor(
                            out=n_[:, half * 512:(half + 1) * 512], in0=po[:, :], in1=x_[:, half * 512:(half + 1) * 512],
                            op=ALU.add), [po, x_], [n_])
                    self.ld(xdst[tt * 128:(tt + 1) * 128, :], n_[:], [n_], [xdst], q="act")
                    if g + 1 < NG:
                        prep_tile(g + 1, i)
        c.barrier()

    def phase_final(self, xsrc, fing, out):
        c = self.c
        S = self.S
        NT = S // 128
        with contextlib.ExitStack() as st:
            epsc = c.sbuf("epsc", [128, 1], F32, st)
            c.op("dve", lambda e: e.memset(epsc[:], EPS), [], [epsc])
            fg = c.sbuf("fg", [128, D], F32, st)
            self.ld(fg[:], fing[:], [fing], [fg])
            xt = [c.sbuf("xtz", [128, D], F32, st) for _ in range(3)]
            sq = c.sbuf("sqz", [128, D], BF16, st)
            ss = [c.sbuf("ssz", [128, 4], F32, st) for _ in range(3)]
            yo = [c.sbuf("yo", [128, D], F32, st) for _ in range(3)]
            for tt in range(NT):
                x_ = xt[tt % 3]
                s_ = ss[tt % 3]
                y_ = yo[tt % 3]
                self.ld(x_[:], xsrc[tt * 128:(tt + 1) * 128, :], [xsrc], [x_])
                c.op("act", lambda e, x_=x_, s_=s_: e.activation(out=sq[:], in_=x_[:], func=AF.Square,
                                                                 accum_out=s_[:, 0:1]), [x_], [sq, s_])
                c.op("act", lambda e, s_=s_: e.activation(out=s_[:, 1:2], in_=s_[:, 0:1], func=AF.Sqrt,
                                                          scale=float(1.0 / D), bias=epsc[:, 0:1]), [s_, epsc], [s_])
                c.op("dve", lambda e, s_=s_: e.reciprocal(out=s_[:, 2:3], in_=s_[:, 1:2]), [s_], [s_])
                c.op("dve", lambda e, x_=x_, s_=s_, y_=y_: e.scalar_tensor_tensor(
                    out=y_[:], in0=x_[:], scalar=s_[:, 2:3], in1=fg[:], op0=ALU.mult, op1=ALU.mult),
                    [x_, s_, fg], [y_])
                self.ld(out[tt * 128:(tt + 1) * 128, :], y_[:], [y_], [out], q="act")
        c.barrier()


def build_program(S, debug=False, stages=99):
    b = Builder(S, debug, stages)
    nc = b.build()
    return nc, b


def prep_inputs(S, x, c, positions, ada_w, ada_b, norm1_g, w_in, b_in, rel_bias, w_branch, w_out,
                norm2_g, w_ffn_in, w_ffn_out, final_g, batches):
    shared = {}
    shared.update(host_consts())
    for l in range(DEPTH):
        d = host_layer_inputs(l, w_in, b_in, ada_w, ada_b, norm1_g, norm2_g, rel_bias, w_branch, w_out,
                              w_ffn_in, w_ffn_out)
        for k, v in d.items():
            shared[f"{k}{l}"] = v
    shared["fing"] = np.ascontiguousarray(np.broadcast_to(np.asarray(final_g, np.float32)[None, :], (128, D)))
    maps = []
    for b in batches:
        m = dict(shared)
        m["x"] = np.ascontiguousarray(np.asarray(x[b], np.float32))
        m["ccol"] = np.ascontiguousarray(np.asarray(c[b], np.float32).reshape(8, 128).T)
        m["pos"] = np.ascontiguousarray(np.broadcast_to(np.asarray(positions[b], np.int32)[None, :], (128, S)))
        maps.append(m)
    return maps


_CACHE = {}


def kernel(**inputs):
    x = np.asarray(inputs["x"])
    B, S, _ = x.shape
    if S not in _CACHE:
        _CACHE[S] = build_program(S)
    nc, b = _CACHE[S]
    slots = [0, 1, None, None, 2, 3, None, None]
    real = prep_inputs(S, batches=list(range(B)), **inputs)
    zero = {k: np.zeros_like(v) for k, v in real[0].items()}
    maps = [real[b] if b is not None else zero for b in slots]
    res = run_bass_kernel_spmd(nc, maps, core_ids=list(range(8)))
    outs = [None] * B
    for core, b in enumerate(slots):
        if b is not None:
            outs[b] = np.asarray(res.results[core]["out"])
    return np.stack(outs, 0).astype(np.float32)
```
